# Optimizing a Trainium2 kernel written in Bass

```python
import math
import jax, jax.numpy as jnp
from jax import lax
import numpy as np

D_MODEL = 1024
BATCH = 4
SEQ = 4096
DEPTH = 2

EPS = 1e-6
NEG = -1e30
Q_BLOCK = 128

M_HEADS = 4
M_DQK = 64
M_DV = 128
M_CHUNK = 64
M_CONV = 4
FORGET_BIAS = 3.0

N_HEADS = 8
N_GROUPS = 2
N_DH = 64
CMP_LEN = 32
CMP_STRIDE = 16
CMP_HID = 128
SLC_LEN = 64
SLC_TOPK = 16
WIN = 512
FORCE_BONUS = 1e4

L_HEADS = 8
Q_RANK = 384
KV_RANK = 256
D_NOPE = 64
D_ROPE = 32
D_V = 64
ROPE_BASE = 10000.0

BRANCH_W = 512
D_FF = -(-8 * D_MODEL // (3 * 256)) * 256

IN_SIZES = (2 * M_HEADS * M_DQK, M_HEADS * M_DV, M_HEADS * M_DV, 2 * M_HEADS,
            N_HEADS * N_DH, 6 * N_GROUPS * N_DH, 3 * N_HEADS,
            Q_RANK, KV_RANK + D_ROPE, 3 * D_MODEL)
D_IN = int(sum(IN_SIZES))
IN_SPLITS = tuple(int(s) for s in np.cumsum(IN_SIZES)[:-1])

kernel_name = "hybrid_mlstm_nsa_mla_gated_block"


def rmsnorm(x, g):
    xf = x.astype(jnp.float32)
    y = xf * lax.rsqrt(jnp.mean(xf * xf, axis=-1, keepdims=True) + EPS)
    return (y * g.astype(jnp.float32)).astype(x.dtype)


def masked_softmax(s, mask):
    s = jnp.where(mask, s.astype(jnp.float32), NEG)
    return jax.nn.softmax(s, axis=-1)


def causal_dwconv(x, w):
    return lax.conv_general_dilated(x, w[:, None, :].astype(x.dtype), window_strides=(1,),
                                    padding=[(w.shape[0] - 1, 0)],
                                    dimension_numbers=('NWC', 'WIO', 'NWC'),
                                    feature_group_count=x.shape[-1])


def rope(x, pos):
    half = x.shape[-1] // 2
    freq = ROPE_BASE ** (-jnp.arange(half, dtype=jnp.float32) / half)
    ang = pos.astype(jnp.float32)[..., None] * freq
    cos = jnp.cos(ang)[:, :, None, :]
    sin = jnp.sin(ang)[:, :, None, :]
    xf = x.astype(jnp.float32)
    x1, x2 = xf[..., :half], xf[..., half:]
    return jnp.concatenate([x1 * cos - x2 * sin, x1 * sin + x2 * cos], axis=-1).astype(x.dtype)


def to_chunks(a):
    B, S, H = a.shape[:3]
    a = a.reshape(B, S // M_CHUNK, M_CHUNK, H, *a.shape[3:])
    return jnp.moveaxis(jnp.moveaxis(a, 1, 0), 3, 2)


def mlstm(q, k, v, i_pre, f_pre):
    B, S, H, _ = q.shape
    f32 = jnp.float32
    qc = to_chunks(q.astype(f32) * (M_DQK ** -0.5))
    kc = to_chunks(k.astype(f32))
    vc = to_chunks(v.astype(f32))
    lic = to_chunks(i_pre.astype(f32))
    lfc = to_chunks(jax.nn.log_sigmoid(f_pre.astype(f32)))
    causal = jnp.tril(jnp.ones((M_CHUNK, M_CHUNK), bool))

    def step(carry, xs):
        C, n, m = carry
        q_, k_, v_, li, lf = xs
        b = jnp.cumsum(lf, axis=-1)
        logD = jnp.where(causal, b[..., :, None] - b[..., None, :] + li[..., None, :], -jnp.inf)
        inter = b + m[..., None]
        m_t = jnp.maximum(inter, jnp.max(logD, axis=-1))
        D = jnp.exp(logD - m_t[..., None])
        a_inter = jnp.exp(inter - m_t)
        s = jnp.einsum('bhtd,bhsd->bhts', q_, k_) * D
        num = a_inter[..., None] * jnp.einsum('bhvd,bhtd->bhtv', C, q_) + jnp.einsum('bhts,bhsv->bhtv', s, v_)
        den = a_inter * jnp.einsum('bhd,bhtd->bht', n, q_) + jnp.sum(s, axis=-1)
        h = num / jnp.maximum(jnp.abs(den), jnp.exp(-m_t))[..., None]
        bL = b[..., -1]
        logw = bL[..., None] - b + li
        m_new = jnp.maximum(bL + m, jnp.max(logw, axis=-1))
        w = jnp.exp(logw - m_new[..., None])
        decay = jnp.exp(bL + m - m_new)
        C_new = decay[..., None, None] * C + jnp.einsum('bhs,bhsv,bhsd->bhvd', w, v_, k_)
        n_new = decay[..., None] * n + jnp.einsum('bhs,bhsd->bhd', w, k_)
        return (C_new, n_new, m_new), h

    init = (jnp.zeros((B, H, M_DV, M_DQK), f32), jnp.zeros((B, H, M_DQK), f32), jnp.zeros((B, H), f32))
    _, hs = lax.scan(step, init, (qc, kc, vc, lic, lfc))
    return hs.transpose(1, 0, 3, 2, 4).reshape(B, S, H, M_DV).astype(q.dtype)


def compress_blocks(x, c_idx, pos, w1, w2):
    B = x.shape[0]
    nb = c_idx.shape[0]
    blk = x[:, c_idx] + pos[None, None, :, None, :]
    blk = jnp.swapaxes(blk, 2, 3).reshape(B, nb, N_GROUPS, CMP_LEN * N_DH)
    return jax.nn.silu(blk @ w1) @ w2


def nsa(q, kv, gate_pre, cmp_pos, cmp_w1, cmp_w2):
    B, S, H, dh = q.shape
    G, R = N_GROUPS, N_HEADS // N_GROUPS
    scale = dh ** -0.5
    qg = q.reshape(B, S, G, R, dh)
    k_cr, v_cr, k_slc, v_slc, k_win, v_win = [kv[:, :, i] for i in range(6)]
    t_all = jnp.arange(S)

    nb = (S - CMP_LEN) // CMP_STRIDE + 1
    c_start = np.arange(nb) * CMP_STRIDE
    c_idx = c_start[:, None] + np.arange(CMP_LEN)[None, :]
    k_cmp = compress_blocks(k_cr, c_idx, cmp_pos[0], cmp_w1[0], cmp_w2[0])
    v_cmp = compress_blocks(v_cr, c_idx, cmp_pos[1], cmp_w1[1], cmp_w2[1])
    c_mask = jnp.asarray(c_start + CMP_LEN - 1)[None, :] <= t_all[:, None]
    sc = jnp.einsum('bsgrd,bngd->bgrsn', qg, k_cmp) * scale
    p_cmp = masked_softmax(sc, c_mask) * (t_all >= CMP_LEN - 1)[:, None].astype(jnp.float32)
    o_cmp = jnp.einsum('bgrsn,bngd->bsgrd', p_cmp.astype(q.dtype), v_cmp)

    ns = S // SLC_LEN
    s_start = np.arange(ns) * SLC_LEN
    overlap = ((c_start[:, None] < s_start[None, :] + SLC_LEN) &
               (c_start[:, None] + CMP_LEN > s_start[None, :])).astype(np.float32)
    imp = jnp.einsum('bgrsn,nj->bgsj', p_cmp, jnp.asarray(overlap))
    cur = t_all // SLC_LEN
    jb = jnp.arange(ns)
    valid = jb[None, :] <= cur[:, None]
    forced = (jb[None, :] == 0) | (jb[None, :] == cur[:, None]) | (jb[None, :] == cur[:, None] - 1)
    score = jnp.where(valid, imp + FORCE_BONUS * forced.astype(jnp.float32), -1.0)
    top_val, sel_idx = lax.top_k(score, min(SLC_TOPK, ns))
    sel_ok = top_val > -0.5
    ks_blocks = k_slc.reshape(B, ns, SLC_LEN, G, dh).transpose(0, 3, 1, 2, 4)
    vs_blocks = v_slc.reshape(B, ns, SLC_LEN, G, dh).transpose(0, 3, 1, 2, 4)
    b_ix = jnp.arange(B)[:, None, None, None]
    g_ix = jnp.arange(G)[None, :, None, None]
    kw_pad = jnp.pad(k_win, ((0, 0), (WIN, 0), (0, 0), (0, 0)))
    vw_pad = jnp.pad(v_win, ((0, 0), (WIN, 0), (0, 0), (0, 0)))

    def block(qb):
        s0 = qb * Q_BLOCK
        qblk = lax.dynamic_slice_in_dim(qg, s0, Q_BLOCK, axis=1)
        t_pos = s0 + jnp.arange(Q_BLOCK)
        idx = lax.dynamic_slice_in_dim(sel_idx, s0, Q_BLOCK, axis=2)
        ok = lax.dynamic_slice_in_dim(sel_ok, s0, Q_BLOCK, axis=2)
        kk = idx.shape[-1]
        ksel = ks_blocks[b_ix, g_ix, idx].reshape(B, G, Q_BLOCK, kk * SLC_LEN, dh)
        vsel = vs_blocks[b_ix, g_ix, idx].reshape(B, G, Q_BLOCK, kk * SLC_LEN, dh)
        tok = idx[..., None] * SLC_LEN + jnp.arange(SLC_LEN)
        m_sel = (ok[..., None] & (tok <= t_pos[None, None, :, None, None])).reshape(B, G, Q_BLOCK, kk * SLC_LEN)
        s_sel = jnp.einsum('btgrd,bgtnd->bgtrn', qblk, ksel) * scale
        p_sel = masked_softmax(s_sel, m_sel[:, :, :, None, :])
        o_sel = jnp.einsum('bgtrn,bgtnd->btgrd', p_sel.astype(q.dtype), vsel)
        kwin = lax.dynamic_slice_in_dim(kw_pad, s0, WIN + Q_BLOCK, axis=1)
        vwin = lax.dynamic_slice_in_dim(vw_pad, s0, WIN + Q_BLOCK, axis=1)
        kpos = s0 - WIN + jnp.arange(WIN + Q_BLOCK)
        wm = (kpos[None, :] <= t_pos[:, None]) & (t_pos[:, None] - kpos[None, :] < WIN) & (kpos[None, :] >= 0)
        s_w = jnp.einsum('btgrd,bngd->bgrtn', qblk, kwin) * scale
        p_w = masked_softmax(s_w, wm)
        o_w = jnp.einsum('bgrtn,bngd->btgrd', p_w.astype(q.dtype), vwin)
        return o_sel, o_w

    o_sel, o_w = lax.map(block, jnp.arange(S // Q_BLOCK))
    o_sel = jnp.swapaxes(o_sel, 0, 1).reshape(B, S, G, R, dh)
    o_w = jnp.swapaxes(o_w, 0, 1).reshape(B, S, G, R, dh)
    g = jax.nn.sigmoid(gate_pre).reshape(B, S, G, R, 3)
    o = g[..., 0:1] * o_cmp + g[..., 1:2] * o_sel + g[..., 2:3] * o_w
    return o.reshape(B, S, H * dh)


def causal_attention(q, k, v, scale):
    B, S, H, _ = q.shape
    kpos = jnp.arange(S)

    def blk(qb):
        s0 = qb * Q_BLOCK
        qblk = lax.dynamic_slice_in_dim(q, s0, Q_BLOCK, axis=1)
        sc = jnp.einsum('bthd,bshd->bhts', qblk, k) * scale
        mask = kpos[None, :] <= (s0 + jnp.arange(Q_BLOCK))[:, None]
        p = masked_softmax(sc, mask)
        return jnp.einsum('bhts,bshd->bthd', p.astype(v.dtype), v)

    o = lax.map(blk, jnp.arange(S // Q_BLOCK))
    return jnp.swapaxes(o, 0, 1).reshape(B, S, H, v.shape[-1])


def mla(c_q, c_kv, positions, q_norm, kv_norm, w_uq, w_ukv):
    B, S, _ = c_q.shape
    c_kv, k_rope = jnp.split(c_kv, [KV_RANK], axis=-1)
    q = (rmsnorm(c_q, q_norm) @ w_uq).reshape(B, S, L_HEADS, D_NOPE + D_ROPE)
    kv = (rmsnorm(c_kv, kv_norm) @ w_ukv).reshape(B, S, L_HEADS, D_NOPE + D_V)
    q_nope, q_rope = jnp.split(q, [D_NOPE], axis=-1)
    k_nope, v = jnp.split(kv, [D_NOPE], axis=-1)
    q = jnp.concatenate([q_nope, rope(q_rope, positions)], axis=-1)
    k_rope = jnp.broadcast_to(rope(k_rope[:, :, None, :], positions), (B, S, L_HEADS, D_ROPE))
    k = jnp.concatenate([k_nope, k_rope], axis=-1)
    o = causal_attention(q, k, v, (D_NOPE + D_ROPE) ** -0.5)
    return o.reshape(B, S, L_HEADS * D_V)


def token_mixing(h, positions, w_in, m_conv, m_gate_bias, m_head_norm, cmp_pos, cmp_w1, cmp_w2,
                 q_norm, kv_norm, w_uq, w_ukv, w_branch, w_out):
    B, S, _ = h.shape
    z = h @ w_in
    a_qk, a_v, a_o, a_if, b_q, b_kv, b_g, c_q, c_kv, gates = jnp.split(z, IN_SPLITS, axis=-1)
    a_qk = jax.nn.silu(causal_dwconv(a_qk, m_conv))
    a_q, a_k = jnp.split(a_qk, 2, axis=-1)
    i_pre, f_pre = jnp.split(a_if + m_gate_bias, 2, axis=-1)
    h_a = mlstm(a_q.reshape(B, S, M_HEADS, M_DQK), a_k.reshape(B, S, M_HEADS, M_DQK),
                a_v.reshape(B, S, M_HEADS, M_DV), i_pre, f_pre)
    y_a = rmsnorm(h_a, m_head_norm).reshape(B, S, BRANCH_W) * jax.nn.sigmoid(a_o)
    y_b = nsa(b_q.reshape(B, S, N_HEADS, N_DH), b_kv.reshape(B, S, 6, N_GROUPS, N_DH),
              b_g.reshape(B, S, N_HEADS, 3), cmp_pos, cmp_w1, cmp_w2)
    y_c = mla(c_q, c_kv, positions, q_norm, kv_norm, w_uq, w_ukv)
    g = jax.nn.sigmoid(gates).reshape(B, S, 3, D_MODEL)
    merged = (g[:, :, 0] * (y_a @ w_branch[0]) + g[:, :, 1] * (y_b @ w_branch[1])
              + g[:, :, 2] * (y_c @ w_branch[2]))
    return merged @ w_out


def setup_inputs(seed: int = 0) -> dict:
    key = jax.random.key(seed)
    ks = jax.random.split(key, 20)
    f32 = jnp.float32

    def nrm(k, shape, fan):
        return jax.random.normal(k, shape, f32) * (fan ** -0.5)

    x = jax.random.normal(ks[0], (BATCH, SEQ, D_MODEL), f32)
    positions = (jnp.arange(SEQ, dtype=jnp.int32)[None, :]
                 + jax.random.randint(ks[1], (BATCH, 1), 0, 1024, dtype=jnp.int32))
    norm_gains = 1.0 + 0.05 * jax.random.normal(ks[2], (DEPTH, 4, D_MODEL), f32)
    w_in = nrm(ks[3], (DEPTH, D_MODEL, D_IN), D_MODEL)
    m_conv = nrm(ks[4], (DEPTH, M_CONV, 2 * M_HEADS * M_DQK), M_CONV)
    m_gate_bias = (0.01 * jax.random.normal(ks[5], (DEPTH, 2 * M_HEADS), f32)
                   + jnp.concatenate([jnp.zeros((M_HEADS,), f32), jnp.full((M_HEADS,), FORGET_BIAS, f32)]))
    m_head_norm = 1.0 + 0.05 * jax.random.normal(ks[6], (DEPTH, M_HEADS, M_DV), f32)
    nsa_cmp_pos = 0.02 * jax.random.normal(ks[7], (DEPTH, 2, CMP_LEN, N_DH), f32)
    nsa_cmp_w1 = nrm(ks[8], (DEPTH, 2, CMP_LEN * N_DH, CMP_HID), CMP_LEN * N_DH)
    nsa_cmp_w2 = nrm(ks[9], (DEPTH, 2, CMP_HID, N_DH), CMP_HID)
    mla_q_norm = 1.0 + 0.05 * jax.random.normal(ks[10], (DEPTH, Q_RANK), f32)
    mla_kv_norm = 1.0 + 0.05 * jax.random.normal(ks[11], (DEPTH, KV_RANK), f32)
    mla_w_uq = nrm(ks[12], (DEPTH, Q_RANK, L_HEADS * (D_NOPE + D_ROPE)), Q_RANK)
    mla_w_ukv = nrm(ks[13], (DEPTH, KV_RANK, L_HEADS * (D_NOPE + D_V)), KV_RANK)
    w_branch = nrm(ks[14], (DEPTH, 3, BRANCH_W, D_MODEL), BRANCH_W)
    w_out = nrm(ks[15], (DEPTH, D_MODEL, D_MODEL), D_MODEL)
    w_ffn_gate = nrm(ks[16], (DEPTH, D_MODEL, D_FF), D_MODEL)
    w_ffn_up = nrm(ks[17], (DEPTH, D_MODEL, D_FF), D_MODEL)
    w_ffn_down = nrm(ks[18], (DEPTH, D_FF, D_MODEL), D_FF)
    return {"x": x, "positions": positions, "norm_gains": norm_gains, "w_in": w_in,
            "m_conv": m_conv, "m_gate_bias": m_gate_bias, "m_head_norm": m_head_norm,
            "nsa_cmp_pos": nsa_cmp_pos, "nsa_cmp_w1": nsa_cmp_w1, "nsa_cmp_w2": nsa_cmp_w2,
            "mla_q_norm": mla_q_norm, "mla_kv_norm": mla_kv_norm, "mla_w_uq": mla_w_uq,
            "mla_w_ukv": mla_w_ukv, "w_branch": w_branch, "w_out": w_out,
            "w_ffn_gate": w_ffn_gate, "w_ffn_up": w_ffn_up, "w_ffn_down": w_ffn_down}


def reference(x, positions, norm_gains, w_in, m_conv, m_gate_bias, m_head_norm, nsa_cmp_pos,
              nsa_cmp_w1, nsa_cmp_w2, mla_q_norm, mla_kv_norm, mla_w_uq, mla_w_ukv, w_branch,
              w_out, w_ffn_gate, w_ffn_up, w_ffn_down):
    for l in range(DEPTH):
        h = rmsnorm(x, norm_gains[l, 0])
        mix = token_mixing(h, positions, w_in[l], m_conv[l], m_gate_bias[l], m_head_norm[l],
                           nsa_cmp_pos[l], nsa_cmp_w1[l], nsa_cmp_w2[l], mla_q_norm[l],
                           mla_kv_norm[l], mla_w_uq[l], mla_w_ukv[l], w_branch[l], w_out[l])
        x = x + rmsnorm(mix, norm_gains[l, 1])
        h = rmsnorm(x, norm_gains[l, 2])
        f = (jax.nn.silu(h @ w_ffn_gate[l]) * (h @ w_ffn_up[l])) @ w_ffn_down[l]
        x = x + rmsnorm(f, norm_gains[l, 3])
    return x
```

```python
import math
from contextlib import ExitStack
import numpy as np
import ml_dtypes
import concourse.bass as bass
import concourse.mybir as mybir
from concourse.bass_utils import run_bass_kernel_spmd

F32 = mybir.dt.float32
BF16 = mybir.dt.bfloat16
I32 = mybir.dt.int32
AF = mybir.ActivationFunctionType
ALU = mybir.AluOpType
AX = mybir.AxisListType

S = 4096
D = 1024
NT = 32
NTB = 8
DFF = 2816
NFC = 22
EPS = 1e-6
DEPTH = 2
NCORES = 8

ENGS = ("pe", "act", "dve", "pool", "sp")

FM = []
def _fm(name, c0, w, kind):
    FM.append((name, c0, w, kind))
_fm("aq0", 0, 128, "copy"); _fm("aq1", 128, 128, "copy")
_fm("ak0", 256, 128, "copy"); _fm("ak1", 384, 128, "copy")
for i in range(4): _fm("ao%d" % i, 1024 + 128 * i, 128, "sigmoid")
_fm("ai", 1536, 4, "ai"); _fm("af", 1540, 4, "af")
for i in range(4): _fm("bq%d" % i, 1544 + 128 * i, 128, "scale8")
BKV = 2056
_fm("kcr", BKV + 0, 128, "copy"); _fm("vcr", BKV + 128, 128, "copy")
_fm("kslc", BKV + 256, 128, "copy"); _fm("kwin", BKV + 512, 128, "copy")
_fm("bg", 2824, 24, "sigmoid")
for i in range(3): _fm("cq%d" % i, 2848 + 128 * i, 128, "copy")
for i in range(2): _fm("ckv%d" % i, 3232 + 128 * i, 128, "copy")
_fm("kr", 3488, 32, "copy"); _fm("krp", -1, 32, "copy")
for i in range(24): _fm("gt%d" % i, 3520 + 128 * i, 128, "sigmoid")
NFM = len(FM)
FMI = {f[0]: i for i, f in enumerate(FM)}


class Prog:
    def __init__(self, nc, n_dma_sems=32):
        self.nc = nc
        self.ops = {e: [] for e in ENGS}
        self.seq = {e: 0 for e in ENGS}
        self.last_w = {}
        self.readers = {}
        self.waited = {e: {} for e in ENGS}
        self.n_dma_sems = n_dma_sems
        self.dma_cnt = [0] * n_dma_sems
        self.dma_rr = 0
        self.n_ops = 0

    @staticmethod
    def _norm(rs):
        return [r.split("@")[0] if isinstance(r, str) else r for r in rs]

    def _deps(self, eng, reads, writes):
        deps = {}

        def add(k, v):
            if deps.get(k, 0) < v:
                deps[k] = v
        for r in reads:
            h = self.last_w.get(r)
            if h is not None:
                add(*h)
        for w in writes:
            h = self.last_w.get(w)
            if h is not None:
                add(*h)
            for k, v in self.readers.get(w, {}).items():
                add(k, v)
        out = []
        for k, v in deps.items():
            if k == eng and eng == "pe":
                continue
            if self.waited[eng].get(k, 0) >= v:
                continue
            self.waited[eng][k] = v
            out.append((k, v))
        return out

    def _commit(self, handle, reads, writes):
        k, v = handle
        for r in reads:
            d = self.readers.setdefault(r, {})
            if d.get(k, 0) < v:
                d[k] = v
        for w in writes:
            self.last_w[w] = handle
            self.readers[w] = {}

    def op(self, eng, fn, reads=(), writes=()):
        reads, writes = self._norm(reads), self._norm(writes)
        waits = self._deps(eng, reads, writes)
        self.seq[eng] += 1
        handle = (eng, self.seq[eng])
        self.ops[eng].append((waits, fn, ("eng", eng)))
        self._commit(handle, reads, writes)
        self.n_ops += 1

    def dma(self, eng, fn, reads=(), writes=()):
        s = self.dma_rr
        self.dma_rr = (self.dma_rr + 1) % self.n_dma_sems
        key = ("dma", s)
        reads, writes = self._norm(reads), self._norm(writes)
        waits = self._deps(eng, reads, writes)
        prev = self.dma_cnt[s] * 16
        if prev and self.waited[eng].get(key, 0) < prev:
            self.waited[eng][key] = prev
            waits.append((key, prev))
        self.dma_cnt[s] += 1
        handle = (key, self.dma_cnt[s] * 16)
        self.ops[eng].append((waits, fn, ("dma", s)))
        self._commit(handle, reads, writes)
        self.n_ops += 1

    def barrier(self):
        cur = [(e, self.seq[e]) for e in ENGS if self.seq[e] > 0]
        cur += [(("dma", s), c * 16) for s, c in enumerate(self.dma_cnt) if c > 0]
        for e in ENGS:
            waits = []
            for k, v in cur:
                if k == e:
                    continue
                if self.waited[e].get(k, 0) >= v:
                    continue
                self.waited[e][k] = v
                waits.append((k, v))
            if waits:
                self.ops[e].append((waits, None, None))

    def emit(self):
        nc = self.nc
        with ExitStack() as st:
            W = 16000
            esem = {e: [st.enter_context(nc.semaphore("s_%s%d" % (e, i))) for i in range(self.seq[e] // W + 1)] for e in ENGS}
            dsem = [st.enter_context(nc.semaphore("d%d" % i)) for i in range(self.n_dma_sems)]
            block = st.enter_context(nc.Block())

            def run(eo, lst):
                myseq = 0
                for waits, fn, inc in lst:
                    for k, v in waits:
                        if isinstance(k, tuple):
                            eo.wait_ge(dsem[k[1]], v)
                        else:
                            eo.wait_ge(esem[k][(v - 1) // W], (v - 1) % W + 1)
                    if fn is None:
                        continue
                    ins = fn(eo)
                    if inc[0] == "eng":
                        myseq += 1
                        ins.then_inc(esem[inc[1]][(myseq - 1) // W], 1)
                    else:
                        ins.then_inc(dsem[inc[1]], 16)

            @block.tensor
            def _(e):
                run(e, self.ops["pe"])

            @block.scalar
            def _(e):
                run(e, self.ops["act"])

            @block.vector
            def _(e):
                run(e, self.ops["dve"])

            @block.gpsimd
            def _(e):
                run(e, self.ops["pool"])

            @block.sync
            def _(e):
                run(e, self.ops["sp"])


class K:
    pass


def build(nlayers=DEPTH, taps=(), stop_after=None):
    nc = bass.Bass("TRN2", target_bir_lowering=False)
    k = K()
    k.nc = nc
    k.taps = set(taps)
    P = Prog(nc)
    k.P = P

    def din(name, shape, dt):
        return nc.dram_tensor(name, list(shape), dt, kind="ExternalInput").ap()

    def dscr(name, shape, dt):
        kind = "ExternalOutput" if name in k.taps else "Internal"
        return nc.dram_tensor(name, list(shape), dt, kind=kind).ap()

    k.x = din("x", [S, D], F32)
    k.pos = din("pos", [S], I32)
    k.gains = din("gains", [128, DEPTH * 4 * 8], F32)
    k.gain_rows = din("gain_rows", [DEPTH * 4, D], F32)
    k.w_fm = din("w_fm", [DEPTH, D, NFM * 128], F32)
    k.w_tm = din("w_tm", [DEPTH, D, 768], F32)
    k.mconv = din("mconv", [DEPTH, 128, 16], F32)
    k.gbias = din("gbias", [DEPTH, 4, 2], F32)
    k.hnorm = din("hnorm", [DEPTH, 128, 4], F32)
    k.cpos = din("cpos", [DEPTH, 64, 64], F32)
    k.cw1 = din("cw1", [DEPTH, 2, 64, 32 * 128], F32)
    k.cw2 = din("cw2", [DEPTH, 2, 128, 64], F32)
    k.qn = din("qn", [DEPTH, 128, 3], F32)
    k.kvn = din("kvn", [DEPTH, 128, 2], F32)
    k.wuq = din("wuq", [DEPTH, 384, 8 * 128], F32)
    k.wuk = din("wuk", [DEPTH, 256, 8 * 96], F32)
    k.wuv = din("wuv", [DEPTH, 256, 512], F32)
    k.wbr = din("wbr", [DEPTH, 3, 512, D], F32)
    k.wout = din("wout", [DEPTH, D, D], F32)
    k.wg = din("wg", [DEPTH, D, DFF], F32)
    k.wu = din("wu", [DEPTH, D, DFF], F32)
    k.wd = din("wd", [DEPTH, DFF, D], F32)
    k.c_ident = din("c_ident", [128, 128], BF16)
    k.c_identf = din("c_identf", [128, 128], F32)
    k.c_cmask = din("c_cmask", [128, 8 * 512], BF16)
    k.c_freq = din("c_freq", [32, 4], F32)
    k.c_cm = din("c_cm", [128, 2 * S], BF16)
    k.c_ovl = din("c_ovl", [128, 2 * 64], BF16)
    k.c_valid = din("c_valid", [128, NT * 64], F32)
    k.c_addc = din("c_addc", [128, NT * 64], F32)
    k.c_E = din("c_E", [64, S], BF16)
    k.c_gsel = din("c_gsel", [24, 24 * 128], BF16)
    k.y = nc.dram_tensor("y", [S, D], F32, kind="ExternalOutput").ap()

    k.zT = dscr("zT", [NFM * 128, S], BF16)
    k.vtm = dscr("vtm", [S, 768], BF16)
    k.Bd = dscr("Bd", [4, S], F32)
    k.csd = dscr("csd", [4, S], F32)
    k.yT = dscr("yT", [1536, S], BF16)
    k.qaT = dscr("qaT", [8 * 96, S], BF16)
    k.kaT = dscr("kaT", [8 * 96, S], BF16)
    k.vml = dscr("vml", [S, 512], BF16)
    k.ycmp = dscr("ycmp", [512, S], BF16)
    k.biasT = dscr("biasT", [128, S], BF16)
    k.x1 = dscr("x1", [S, D], F32)
    k.x2 = dscr("x2", [S, D], F32)
    k.kcmpd = dscr("kcmpd", [128, 256], BF16)
    k.impd = dscr("impd", [128, S], F32)
    k.ifd = dscr("ifd", [2, 4, S], F32)
    k.ropeT = dscr("ropeT", [4, 32, S], F32)

    with ExitStack() as gst:
        k.uid = 0

        def sb(name, shape, dt, st=gst):
            k.uid += 1
            return st.enter_context(nc.sbuf_tensor("%s@%d" % (name, k.uid), list(shape), dt))
        k.sb = sb
        k.ps = [gst.enter_context(nc.psum_tensor("ps%d" % i, [128, 512], F32)) for i in range(8)]
        k.psb = k.ps[7][:, :].bitcast(BF16)
        k.ident = sb("ident", [128, 128], BF16)
        k.ones = sb("ones", [128, 128], BF16)
        k.cmask = sb("cmask", [128, 8, 512], BF16)
        k.gn = sb("gn", [128, DEPTH * 4, 8], F32)
        k.epsT = sb("epsT", [128, 1], F32)
        k.zeroT = sb("zeroT", [128, 1], F32)
        P.dma("sp", lambda e: e.dma_start(out=k.ident[:], in_=k.c_ident[:, :]), writes=["ident"])
        P.dma("sp", lambda e: e.dma_start(out=k.cmask[:].rearrange("p a b -> p (a b)"), in_=k.c_cmask[:, :]), writes=["cmask"])
        P.dma("sp", lambda e: e.dma_start(out=k.gn[:].rearrange("p a b -> p (a b)"), in_=k.gains[:, :]), writes=["gn"])
        P.op("pool", lambda e: e.memset(k.ones[:], 1.0), writes=["ones"])
        P.op("pool", lambda e: e.memset(k.epsT[:], EPS), writes=["epsT"])
        P.op("pool", lambda e: e.memset(k.zeroT[:], 0.0), writes=["zeroT"])
        P.barrier()

        xin = k.x
        for l in range(nlayers):
            last = (l == nlayers - 1)
            phase_proj(k, l, xin)
            if stop_after == "proj":
                break
            phase_mlstm(k, l)
            if stop_after == "mlstm":
                break
            phase_mla(k, l)
            if stop_after == "mla":
                break
            phase_nsa(k, l)
            if stop_after == "nsa":
                break
            phase_merge(k, l, xin, k.x1)
            if stop_after == "merge":
                break
            xo = k.y if last else k.x2
            phase_ffn(k, l, k.x1, xo)
            xin = k.x2
        P.barrier()
        P.emit()
    return nc


def norm_tile_T(k, st_tag, xt, ss, rs, xn, n):
    P = k.P
    P.op("pool", lambda e: e.memset(ss[:], 0.0), writes=[ss.name])
    P.op("act", lambda e: e.activation(out=xn[:], in_=xt[:], func=AF.Square, accum_out=ss[:]),
         reads=[xt.name, ss.name], writes=[xn.name, ss.name])
    P.op("act", lambda e: e.activation(out=rs[:], in_=ss[:], func=AF.Sqrt, bias=k.epsT[:], scale=1.0 / D),
         reads=[ss.name, "epsT"], writes=[rs.name])
    P.op("dve", lambda e: e.reciprocal(out=rs[:], in_=rs[:]), reads=[rs.name], writes=[rs.name])
    P.op("dve", lambda e: e.tensor_scalar(out=xn[:], in0=xt[:], scalar1=rs[:, 0:1], scalar2=None, op0=ALU.mult),
         reads=[xt.name, rs.name, xn.name], writes=[xn.name])


def load_cast(k, st, name, dram_ap, rows_p, nk, ncols, dst, dst_res, stage, q="sp", eng="rot", sw=2048):
    P = k.P
    per = max(1, sw // ncols)
    kc = 0
    i = 0
    while kc < nk:
        m = min(per, nk - kc)
        stg = stage[k.stage_i % len(stage)]
        k.stage_i += 1
        src = dram_ap[kc * 128:(kc + m) * 128, :].rearrange("(a p) c -> p a c", p=128)
        sv = stg[:rows_p, 0:m * ncols].rearrange("p (a c) -> p a c", a=m)
        P.dma(q, lambda e, sv=sv, src=src: e.dma_start(out=sv, in_=src), writes=[stg.name])
        dv = dst[:rows_p, kc:kc + m, 0:ncols]
        if eng == "rot":
            eng_i = ("dve", "act", "dve", "pool")[k.stage_i % 4]
        else:
            eng_i = eng
        if eng_i == "act":
            P.op("act", lambda e, dv=dv, sv=sv: e.copy(out=dv, in_=sv), reads=[stg.name], writes=[dst_res])
        else:
            P.op(eng_i, lambda e, dv=dv, sv=sv: e.tensor_copy(out=dv, in_=sv), reads=[stg.name], writes=[dst_res])
        kc += m
        i += 1


def make_hT(k, st, xin, l, gi, hT, tiles=range(NT), tok_off=0):
    P = k.P
    nc = k.nc
    xt2 = [k.sb("mh_x%d" % i, [128, D], F32, st) for i in range(2)]
    xn2 = [k.sb("mh_n%d" % i, [128, D], BF16, st) for i in range(2)]
    ss2 = [k.sb("mh_s%d" % i, [128, 1], F32, st) for i in range(2)]
    rs2 = [k.sb("mh_r%d" % i, [128, 1], F32, st) for i in range(2)]
    gb = k.gn[:, l * 4 + gi, :].unsqueeze(2).broadcast_to([128, 8, 128])
    for j, n in enumerate(tiles):
        xt, xn, ss, rs = xt2[j % 2], xn2[j % 2], ss2[j % 2], rs2[j % 2]
        P.dma("sp", lambda e, xt=xt, n=n: e.dma_start(out=xt[:], in_=xin[n * 128:(n + 1) * 128, :]), reads=[("dram", xin.name)], writes=[xt.name])
        norm_tile_T(k, "mh", xt, ss, rs, xn, n)
        for kc in range(8):
            P.op("pe", lambda e, kc=kc, xn=xn: e.transpose(out=k.psb[:, kc * 128:(kc + 1) * 128], in_=xn[:, kc * 128:(kc + 1) * 128], identity=k.ident[:]),
                 reads=[xn.name, "ident"], writes=["ps7"])
        t0 = (n - tok_off) * 128
        P.op("dve", lambda e, t0=t0: e.tensor_tensor(out=hT[:, :, t0:t0 + 128], in0=k.psb.rearrange("p (a t) -> p a t", a=8), in1=gb, op=ALU.mult),
             reads=["ps7", "gn"], writes=[hT.name])


def phase_proj(k, l, xin):
    P = k.P
    with ExitStack() as st:
        hT = k.sb("hT", [128, 8, S], BF16, st)
        make_hT(k, st, xin, l, 0, hT)
        stage = [k.sb("pj_stg%d" % i, [128, 2048], F32, st) for i in range(2)]
        k.stage_i = 0
        wb2 = [k.sb("pj_wb%d" % i, [128, 8, 512], BF16, st) for i in range(2)]
        ev = [k.sb("pj_ev%d" % i, [128, 512], BF16, st) for i in range(4)]
        gbt = k.sb("pj_gb", [4, 2], F32, st)
        evf = [k.sb("pj_evf%d" % i, [4, 512], F32, st) for i in range(2)]
        P.dma("sp", lambda e: e.dma_start(out=gbt[:], in_=k.gbias[l]), writes=["pj_gb"])
        evi = 0
        psi = 0
        def load_group(g0):
            gw = min(4, NFM - g0)
            wb = wb2[(g0 // 4) % 2]
            load_cast(k, st, "wfm", k.w_fm[l][:, g0 * 128:(g0 + gw) * 128], 128, 8, gw * 128, wb, wb.name, stage, eng="pool")
        load_group(0)
        for g0 in range(0, NFM, 4):
            gw = min(4, NFM - g0)
            wb = wb2[(g0 // 4) % 2]
            if g0 + 4 < NFM:
                load_group(g0 + 4)
            for ci in range(g0, g0 + gw):
                name, c0, w, kind = FM[ci]
                off = (ci - g0) * 128
                for tb in range(NTB):
                    ps = k.ps[psi % 2]
                    psi += 1
                    for kc in range(8):
                        P.op("pe", lambda e, ps=ps, kc=kc, wb=wb, off=off, w=w, tb=tb: e.matmul(ps[0:w, :], lhsT=wb[:, kc, off:off + w], rhs=hT[:, kc, tb * 512:(tb + 1) * 512], start=(kc == 0), stop=(kc == 7)),
                             reads=[wb.name, "hT"], writes=[ps.name])
                    if kind in ("ai", "af"):
                        j = 0 if kind == "ai" else 1
                        dst = evf[evi % 2]
                        evi += 1
                        P.op("dve", lambda e, ps=ps, dst=dst, j=j: e.tensor_scalar(out=dst[:], in0=ps[0:4, :], scalar1=gbt[:, j:j + 1], scalar2=None, op0=ALU.add),
                             reads=[ps.name, "pj_gb"], writes=[dst.name])
                        P.dma("sp", lambda e, dst=dst, j=j, tb=tb: e.dma_start(out=k.ifd[j][:, tb * 512:(tb + 1) * 512], in_=dst[:]), reads=[dst.name], writes=["ifd"])
                        continue
                    e_t = ev[evi % 4]
                    evi += 1
                    if kind == "sigmoid":
                        P.op("act", lambda e, ps=ps, e_t=e_t, w=w: e.activation(out=e_t[0:w, :], in_=ps[0:w, :], func=AF.Sigmoid), reads=[ps.name], writes=[e_t.name])
                    elif kind == "scale8":
                        P.op("dve", lambda e, ps=ps, e_t=e_t, w=w: e.tensor_scalar(out=e_t[0:w, :], in0=ps[0:w, :], scalar1=0.125, scalar2=None, op0=ALU.mult), reads=[ps.name], writes=[e_t.name])
                    else:
                        if evi % 2:
                            P.op("dve", lambda e, ps=ps, e_t=e_t, w=w: e.tensor_copy(out=e_t[0:w, :], in_=ps[0:w, :]), reads=[ps.name], writes=[e_t.name])
                        else:
                            P.op("act", lambda e, ps=ps, e_t=e_t, w=w: e.copy(out=e_t[0:w, :], in_=ps[0:w, :]), reads=[ps.name], writes=[e_t.name])
                    P.dma("sp", lambda e, e_t=e_t, ci=ci, w=w, tb=tb: e.dma_start(out=k.zT[ci * 128:ci * 128 + w, tb * 512:(tb + 1) * 512], in_=e_t[0:w, :]),
                          reads=[e_t.name], writes=[("zT", ci)])
        for (c0, cw) in ((0, 512), (512, 256)):
            wb = wb2[0]
            load_cast(k, st, "wtm", k.w_tm[l][:, c0:c0 + cw], 128, 8, cw, wb, wb.name, stage, eng="pool")
            for n in range(NT):
                ps = k.ps[psi % 2]
                psi += 1
                for kc in range(8):
                    P.op("pe", lambda e, ps=ps, kc=kc, n=n, cw=cw: e.matmul(ps[:, 0:cw], lhsT=hT[:, kc, n * 128:(n + 1) * 128], rhs=wb[:, kc, 0:cw], start=(kc == 0), stop=(kc == 7)),
                         reads=[wb.name, "hT"], writes=[ps.name])
                e_t = ev[evi % 4]
                evi += 1
                P.op("dve", lambda e, ps=ps, e_t=e_t, cw=cw: e.tensor_copy(out=e_t[:, 0:cw], in_=ps[:, 0:cw]), reads=[ps.name], writes=[e_t.name])
                P.dma("sp", lambda e, e_t=e_t, n=n, c0=c0, cw=cw: e.dma_start(out=k.vtm[n * 128:(n + 1) * 128, c0:c0 + cw], in_=e_t[:, 0:cw]),
                      reads=[e_t.name], writes=[("vtm", c0)])
        k.P.barrier()


def attn_tb(k, steps, qk, pf, pv, nb=3):
    n = len(steps)
    sps = [k.ps[i] for i in range(nb)]
    LA = nb - 1
    for j in range(min(LA, n)):
        qk(j, steps[j], sps[j % nb])
    for i in range(n):
        if i + LA < n:
            qk(i + LA, steps[i + LA], sps[(i + LA) % nb])
        pt = k.pts[k.pt_i % 4]
        k.pt_i += 1
        pf(i, steps[i], sps[i % nb], pt)
        pv(i, steps[i], pt, i == 0, i == n - 1)


def attn_tb_g(k, steps, qk, pf, pv, banks):
    n = len(steps)
    sps = list(banks)
    nb = len(sps)
    LA = nb - 1
    for j in range(min(LA, n)):
        qk(j, steps[j], sps[j % nb])
    for i in range(n):
        if i + LA < n:
            qk(i + LA, steps[i + LA], sps[(i + LA) % nb])
        pt = k.pts[k.pt_i % 4]
        k.pt_i += 1
        pf(i, steps[i], sps[i % nb], pt)
        pv(i, steps[i], pt, i == 0, i == n - 1)
        yield


def mm(k, out, lhsT, rhs, start, stop, reads, writes):
    k.P.op("pe", lambda e: e.matmul(out, lhsT=lhsT, rhs=rhs, start=start, stop=stop), reads=reads, writes=writes)


def causal_steps(tb):
    return [(st, (st - 4 * tb) if st >= 4 * tb else None) for st in range(4 * tb + 4)]


def attn_scratch(k, st):
    k.pts = [k.sb("pt%d" % i, [128, 512], BF16, st) for i in range(4)]
    k.pt_i = 0


def std_pf(k, maskres="cmask"):
    P = k.P

    def pf(i, step, ps, pt):
        s_t, mo = step
        P.op("act", lambda e: e.activation(out=pt[:], in_=ps[:], func=AF.Exp), reads=[ps.name], writes=[pt.name])
        if mo is not None:
            P.op("dve", lambda e: e.tensor_tensor(out=pt[:], in0=pt[:], in1=k.cmask[:, mo, :], op=ALU.mult), reads=[pt.name, maskres], writes=[pt.name])
    return pf


def phase_mlstm(k, l):
    P = k.P
    with ExitStack() as st:
        mcv = k.sb("ml_mcv", [128, 16], F32, st)
        P.dma("sp", lambda e: e.dma_start(out=mcv[:], in_=k.mconv[l]), writes=["ml_mcv"])
        with ExitStack() as st2:
            xp = k.sb("ml_xp", [128, 3 + S], BF16, st2)
            acc = k.sb("ml_acc", [128, S], F32, st2)
            ob = k.sb("ml_ob", [128, S], BF16, st2)
            P.op("pool", lambda e: e.memset(xp[:, 0:3], 0.0), writes=["ml_xp"])
            for c in range(4):
                ci = c
                P.dma("sp", lambda e, ci=ci: e.dma_start(out=xp[:, 3:3 + S], in_=k.zT[ci * 128:(ci + 1) * 128, :]), reads=[("zT", ci)], writes=["ml_xp"])
                P.op("dve", lambda e, c=c: e.tensor_scalar(out=acc[:], in0=xp[:, 3:3 + S], scalar1=mcv[:, c * 4 + 3:c * 4 + 4], scalar2=None, op0=ALU.mult), reads=["ml_xp", "ml_mcv"], writes=["ml_acc"])
                for tap in range(3):
                    P.op("dve", lambda e, c=c, tap=tap: e.scalar_tensor_tensor(out=acc[:], in0=xp[:, tap:tap + S], scalar=mcv[:, c * 4 + tap:c * 4 + tap + 1], in1=acc[:], op0=ALU.mult, op1=ALU.add),
                         reads=["ml_xp", "ml_mcv", "ml_acc"], writes=["ml_acc"])
                if c < 2:
                    P.op("act", lambda e: e.activation(out=acc[:], in_=acc[:], func=AF.Silu), reads=["ml_acc"], writes=["ml_acc"])
                    P.op("dve", lambda e: e.tensor_scalar(out=ob[:], in0=acc[:], scalar1=0.125, scalar2=None, op0=ALU.mult), reads=["ml_acc"], writes=["ml_ob"])
                else:
                    P.op("act", lambda e: e.activation(out=ob[:], in_=acc[:], func=AF.Silu), reads=["ml_acc"], writes=["ml_ob"])
                P.dma("sp", lambda e, ci=ci: e.dma_start(out=k.zT[ci * 128:(ci + 1) * 128, :], in_=ob[:]), reads=["ml_ob"], writes=[("zT", ci)])
            t1 = k.sb("ml_t1", [4, S], F32, st2)
            cum = k.sb("ml_cum", [4, S], F32, st2)
            on4 = k.sb("ml_on4", [4, S], F32, st2)
            oneT = k.sb("ml_one", [4, 1], F32, st2)
            li = k.sb("ml_li", [4, S], F32, st2)
            P.op("pool", lambda e: e.memset(oneT[:], 1.0), writes=["ml_one"])
            P.dma("sp", lambda e: e.dma_start(out=cum[:], in_=k.ifd[1]), reads=["ifd"], writes=["ml_cum"])
            P.dma("sp", lambda e: e.dma_start(out=li[:], in_=k.ifd[0]), reads=["ifd"], writes=["ml_li"])
            P.op("pool", lambda e: e.memset(on4[:], 1.0), writes=["ml_on4"])
            P.op("act", lambda e: e.activation(out=t1[:], in_=cum[:], func=AF.Exp, scale=-1.0), reads=["ml_cum"], writes=["ml_t1"])
            P.op("act", lambda e: e.activation(out=t1[:], in_=t1[:], func=AF.Ln, bias=oneT[:], scale=1.0), reads=["ml_t1", "ml_one"], writes=["ml_t1"])
            P.op("dve", lambda e: e.tensor_tensor_scan(out=cum[:], data0=on4[:], data1=t1[:], initial=0.0, op0=ALU.mult, op1=ALU.add), reads=["ml_on4", "ml_t1"], writes=["ml_cum"])
            P.op("dve", lambda e: e.tensor_tensor(out=t1[:], in0=li[:], in1=cum[:], op=ALU.add), reads=["ml_li", "ml_cum", "ml_t1"], writes=["ml_t1"])
            P.op("dve", lambda e: e.tensor_scalar(out=cum[:], in0=cum[:], scalar1=-1.0, scalar2=None, op0=ALU.mult), reads=["ml_cum"], writes=["ml_cum"])
            P.dma("sp", lambda e: e.dma_start(out=k.Bd[:, :], in_=cum[:]), reads=["ml_cum"], writes=["Bd"])
            P.dma("sp", lambda e: e.dma_start(out=k.csd[:, :], in_=t1[:]), reads=["ml_t1"], writes=["csd"])
            P.barrier()
        attn_scratch(k, st)
        cs_tm = k.sb("ml_cs", [128, 4, NT], F32, st)
        hn_g = k.sb("ml_hn", [128, 4], F32, st)
        P.dma("sp", lambda e: e.dma_start(out=hn_g[:], in_=k.hnorm[l]), writes=["ml_hn"])
        for h in range(4):
            P.dma("sp", lambda e, h=h: e.dma_start(out=cs_tm[:, h, :], in_=k.csd[h].rearrange("(n p) -> p n", p=128), allow_slow_non_contiguous=True), reads=["csd"], writes=["ml_cs"])
        KT2 = [k.sb("ml_K%d" % i, [64, S], BF16, st) for i in range(2)]
        QT2 = [k.sb("ml_Q%d" % i, [64, S], BF16, st) for i in range(2)]
        V2 = [k.sb("ml_V%d" % i, [128, NT, 128], BF16, st) for i in range(2)]
        Br2 = [k.sb("ml_B%d" % i, [128, S], F32, st) for i in range(2)]
        Dt2 = [k.sb("ml_D%d" % i, [128, 512], F32, st) for i in range(4)]
        ao2 = [k.sb("ml_ao%d" % i, [128, 512], BF16, st) for i in range(2)]
        hn = k.sb("ml_h", [128, 512], F32, st)
        rr = k.sb("ml_r", [128, 512], F32, st)
        sq = k.sb("ml_sq", [128, 512], BF16, st)
        yo2 = [k.sb("ml_yo%d" % i, [128, 512], BF16, st) for i in range(2)]
        di = [0]
        Cf = k.sb("ml_Cf", [64, 128], F32, st)
        Nf = k.sb("ml_Nf", [64, 128], F32, st)
        Cb2 = [k.sb("ml_Cb%d" % i, [64, 128], BF16, st) for i in range(2)]
        Nb2 = [k.sb("ml_Nb%d" % i, [64, 128], BF16, st) for i in range(2)]
        negE = k.sb("ml_negE", [128, NTB], F32, st)
        gcol = k.sb("ml_gcol", [128, 1], F32, st)
        dec = k.sb("ml_dec", [64, 512], F32, st)
        qd2 = [k.sb("ml_qd%d" % i, [64, 512], BF16, st) for i in range(2)]
        wcol = [k.sb("ml_w%d" % i, [128, 1], F32, st) for i in range(4)]
        kw = [k.sb("ml_kw%d" % i, [128, 64], BF16, st) for i in range(4)]
        psT = k.ps[2][:, :].bitcast(BF16)
        TS = [(Cf, Nf, Cb2, Nb2, negE, gcol, dec, qd2, wcol, kw, hn, rr, sq)]
        TS.append((k.sb("ml_CfB", [64, 128], F32, st), k.sb("ml_NfB", [64, 128], F32, st),
                   [k.sb("ml_CbB%d" % i, [64, 128], BF16, st) for i in range(2)],
                   [k.sb("ml_NbB%d" % i, [64, 128], BF16, st) for i in range(2)],
                   k.sb("ml_negEB", [128, NTB], F32, st), k.sb("ml_gcolB", [128, 1], F32, st),
                   k.sb("ml_decB", [64, 512], F32, st),
                   [k.sb("ml_qdB%d" % i, [64, 512], BF16, st) for i in range(2)],
                   [k.sb("ml_wB%d" % i, [128, 1], F32, st) for i in range(4)],
                   [k.sb("ml_kwB%d" % i, [128, 64], BF16, st) for i in range(4)],
                   k.sb("ml_hB", [128, 512], F32, st), k.sb("ml_rB", [128, 512], F32, st), k.sb("ml_sqB", [128, 512], BF16, st)))

        def load_head(h):
            KT, QT, V, Br = KT2[h % 2], QT2[h % 2], V2[h % 2], Br2[h % 2]
            cq, ck = h // 2, 2 + h // 2
            r0 = (h % 2) * 64
            P.dma("sp", lambda e, KT=KT, ck=ck, r0=r0: e.dma_start(out=KT[:], in_=k.zT[ck * 128 + r0:ck * 128 + r0 + 64, :]), reads=[("zT", ck)], writes=[KT.name])
            P.dma("sp", lambda e, QT=QT, cq=cq, r0=r0: e.dma_start(out=QT[:], in_=k.zT[cq * 128 + r0:cq * 128 + r0 + 64, :]), reads=[("zT", cq)], writes=[QT.name])
            P.dma("sp", lambda e, V=V, h=h: e.dma_start(out=V[:], in_=k.vtm[:, h * 128:(h + 1) * 128].rearrange("(n p) c -> p n c", p=128)), reads=[("vtm", 0)], writes=[V.name])
            P.dma("sp", lambda e, Br=Br, h=h: e.dma_start(out=Br[:], in_=k.Bd[h].partition_broadcast(128)), reads=["Bd"], writes=[Br.name])
        def block(h, tb):
            KT, QT, V, Br = KT2[h % 2], QT2[h % 2], V2[h % 2], Br2[h % 2]
            Cf, Nf, Cb2, Nb2, negE, gcol, dec, qd2, wcol, kw, hn, rr, sq = TS[h % 2]
            sbank = k.ps[0] if h % 2 == 0 else k.ps[2]
            scr = k.ps[1] if h % 2 == 0 else k.ps[7]
            psT = scr[:, :].bitcast(BF16)
            if True:
                if tb == 0:
                    P.op("dve", lambda e: e.tensor_scalar(out=negE[:], in0=Br[:, 511::512], scalar1=-1.0, scalar2=None, op0=ALU.mult), reads=[Br.name], writes=[negE.name])
                tsl = slice(tb * 512, (tb + 1) * 512)
                num, den = (k.ps[3], k.ps[4]) if h % 2 == 0 else (k.ps[5], k.ps[6])
                ao = ao2[h % 2]
                P.dma("sp", lambda e, ao=ao, h=h, tsl=tsl: e.dma_start(out=ao[:], in_=k.zT[(4 + h) * 128:(5 + h) * 128, tsl]), reads=[("zT", 4 + h)], writes=[ao.name])
                Cb, Nb = Cb2[tb % 2], Nb2[tb % 2]
                if tb >= 1:
                    qd = qd2[tb % 2]
                    P.op("act", lambda e, Br=Br, tsl=tsl, tb=tb: e.activation(out=dec[:], in_=Br[0:64, tsl], func=AF.Exp, bias=negE[0:64, tb - 1:tb], scale=1.0), reads=[Br.name, negE.name], writes=[dec.name])
                    P.op("dve", lambda e, QT=QT, tsl=tsl, qd=qd: e.tensor_tensor(out=qd[:], in0=QT[:, tsl], in1=dec[:], op=ALU.mult), reads=[QT.name, dec.name], writes=[qd.name])
                    mm(k, num[:], Cb[:], qd[:], True, False, [Cb.name, qd.name], [num.name])
                    mm(k, den[:], Nb[:], qd[:], True, False, [Nb.name, qd.name], [den.name])

                def qk(i, step, ps, KT=KT, QT=QT, tsl=tsl):
                    s_t = step[0]
                    mm(k, ps[:], KT[:, s_t * 128:(s_t + 1) * 128], QT[:, tsl], True, True, [KT.name, QT.name], [ps.name])

                def pf(i, step, ps, pt, Br=Br, tsl=tsl, h=h):
                    s_t, mo = step
                    Dt = Dt2[di[0] % 4]
                    di[0] += 1
                    P.op("act", lambda e: e.activation(out=Dt[:], in_=Br[:, tsl], func=AF.Exp, bias=cs_tm[:, h, s_t:s_t + 1], scale=1.0), reads=[Br.name, "ml_cs"], writes=[Dt.name])
                    P.op("dve", lambda e: e.tensor_tensor(out=pt[:], in0=ps[:], in1=Dt[:], op=ALU.mult), reads=[ps.name, Dt.name], writes=[pt.name])
                    if mo is not None:
                        P.op("pool" if mo % 2 else "dve", lambda e: e.tensor_tensor(out=pt[:], in0=pt[:], in1=k.cmask[:, mo, :], op=ALU.mult), reads=[pt.name, "cmask"], writes=[pt.name])

                def pv(i, step, pt, first, lastf, V=V, num=num, den=den, tb=tb):
                    s_t = step[0]
                    mm(k, num[:], V[:, s_t, :], pt[:], first and tb == 0, lastf, [V.name, pt.name], [num.name])
                    mm(k, den[:], k.ones[:], pt[:], first and tb == 0, lastf, ["ones", pt.name], [den.name])
                yield from attn_tb_g(k, [(4 * tb + o, o) for o in range(4)], qk, pf, pv, [sbank])
                if tb < NTB - 1:
                    ecol = Br[:, tb * 512 + 511:tb * 512 + 512]
                    dC, dN = scr, scr
                    for o in range(4):
                        s_t = 4 * tb + o
                        P.op("act", lambda e, o=o, s_t=s_t, ecol=ecol, h=h: e.activation(out=wcol[o][:], in_=cs_tm[:, h, s_t:s_t + 1], func=AF.Exp, bias=ecol, scale=1.0), reads=["ml_cs", Br.name], writes=[wcol[o].name])
                        yield
                        P.op("pe", lambda e, o=o, s_t=s_t, KT=KT: e.transpose(out=psT[:, o * 64:(o + 1) * 64], in_=KT[:, s_t * 128:(s_t + 1) * 128], identity=k.ident[0:64, 0:64]), reads=[KT.name, "ident"], writes=[scr.name])
                        yield
                        P.op("dve", lambda e, o=o: e.tensor_scalar(out=kw[o][:], in0=psT[:, o * 64:(o + 1) * 64], scalar1=wcol[o][:, 0:1], scalar2=None, op0=ALU.mult), reads=[scr.name, wcol[o].name], writes=[kw[o].name])
                        yield
                    for o in range(4):
                        mm(k, dC[0:64, 128:256], kw[o][:], V[:, 4 * tb + o, :], o == 0, o == 3, [kw[o].name, V.name], [dC.name])
                        yield
                    for o in range(4):
                        mm(k, dN[0:64, 256:384], kw[o][:], k.ones[:], o == 0, o == 3, [kw[o].name, "ones"], [dN.name])
                        yield
                    Cbn, Nbn = Cb2[(tb + 1) % 2], Nb2[(tb + 1) % 2]
                    if tb == 0:
                        P.op("dve", lambda e, dC=dC: e.tensor_copy(out=Cf[:], in_=dC[0:64, 128:256]), reads=[dC.name], writes=[Cf.name])
                        yield
                        P.op("dve", lambda e, dN=dN: e.tensor_copy(out=Nf[:], in_=dN[0:64, 256:384]), reads=[dN.name], writes=[Nf.name])
                        yield
                    else:
                        P.op("act", lambda e, ecol=ecol, tb=tb: e.activation(out=gcol[0:64, :], in_=ecol[0:64, :], func=AF.Exp, bias=negE[0:64, tb - 1:tb], scale=1.0), reads=[Br.name, negE.name], writes=[gcol.name])
                        yield
                        P.op("dve", lambda e, dC=dC: e.scalar_tensor_tensor(out=Cf[:], in0=Cf[:], scalar=gcol[0:64, 0:1], in1=dC[0:64, 128:256], op0=ALU.mult, op1=ALU.add), reads=[Cf.name, gcol.name, dC.name], writes=[Cf.name])
                        yield
                        P.op("dve", lambda e, dN=dN: e.scalar_tensor_tensor(out=Nf[:], in0=Nf[:], scalar=gcol[0:64, 0:1], in1=dN[0:64, 256:384], op0=ALU.mult, op1=ALU.add), reads=[Nf.name, gcol.name, dN.name], writes=[Nf.name])
                        yield
                    P.op("act", lambda e, Cbn=Cbn: e.copy(out=Cbn[:], in_=Cf[:]), reads=[Cf.name], writes=[Cbn.name])
                    yield
                    P.op("act", lambda e, Nbn=Nbn: e.copy(out=Nbn[:], in_=Nf[:]), reads=[Nf.name], writes=[Nbn.name])
                    yield
                P.op("dve", lambda e, den=den: e.tensor_scalar(out=rr[:], in0=den[:], scalar1=-1.0, scalar2=None, op0=ALU.mult), reads=[den.name], writes=[rr.name])
                yield
                P.op("dve", lambda e, den=den: e.tensor_tensor(out=rr[:], in0=rr[:], in1=den[:], op=ALU.max), reads=[den.name, rr.name], writes=[rr.name])
                yield
                P.op("dve", lambda e: e.tensor_scalar(out=rr[:], in0=rr[:], scalar1=1.0, scalar2=None, op0=ALU.max), reads=[rr.name], writes=[rr.name])
                yield
                P.op("act", lambda e: e.activation(out=rr[:], in_=rr[:], func=AF.Ln), reads=[rr.name], writes=[rr.name])
                yield
                P.op("act", lambda e: e.activation(out=rr[:], in_=rr[:], func=AF.Exp, scale=-1.0), reads=[rr.name], writes=[rr.name])
                yield
                P.op("dve", lambda e, num=num: e.tensor_tensor(out=hn[:], in0=num[:], in1=rr[:], op=ALU.mult), reads=[num.name, rr.name], writes=[hn.name])
                yield
                P.op("pool", lambda e: e.tensor_tensor(out=sq[:], in0=hn[:], in1=hn[:], op=ALU.mult), reads=[hn.name], writes=[sq.name])
                yield
                ss = sbank
                mm(k, ss[:], k.ones[:], sq[:], True, True, ["ones", sq.name], [ss.name])
                yield
                P.op("act", lambda e, ss=ss: e.activation(out=rr[:], in_=ss[:], func=AF.Ln, bias=k.epsT[:], scale=1.0 / 128), reads=[ss.name, "epsT"], writes=[rr.name])
                yield
                P.op("act", lambda e: e.activation(out=rr[:], in_=rr[:], func=AF.Exp, scale=-0.5), reads=[rr.name], writes=[rr.name])
                yield
                P.op("dve", lambda e, h=h: e.scalar_tensor_tensor(out=hn[:], in0=hn[:], scalar=hn_g[:, h:h + 1], in1=rr[:], op0=ALU.mult, op1=ALU.mult), reads=[hn.name, "ml_hn", rr.name], writes=[hn.name])
                yield
                yo = yo2[h % 2]
                P.op("pool", lambda e, yo=yo, ao=ao: e.tensor_tensor(out=yo[:], in0=hn[:], in1=ao[:], op=ALU.mult), reads=[hn.name, ao.name], writes=[yo.name])
                yield
                P.dma("sp", lambda e, yo=yo, h=h, tsl=tsl: e.dma_start(out=k.yT[h * 128:(h + 1) * 128, tsl], in_=yo[:]), reads=[yo.name], writes=[("yT", h)])
                yield
        for hp in (0, 2):
            load_head(hp)
            load_head(hp + 1)
            for tb in range(NTB):
                gens = [block(hp, tb), block(hp + 1, tb)]
                while gens:
                    for g_ in list(gens):
                        try:
                            next(g_)
                        except StopIteration:
                            gens.remove(g_)
        P.barrier()


def rms_fm(k, src, nchunk, gain, dst, sq, rs, width):
    P = k.P
    P.op("act", lambda e: e.activation(out=sq[:, 0:nchunk, :], in_=src[:, 0:nchunk, :], func=AF.Square), reads=[src.name], writes=[sq.name])
    ss = k.ps[7]
    for a in range(nchunk):
        mm(k, ss[:], k.ones[:], sq[:, a, :], a == 0, a == nchunk - 1, ["ones", sq.name], [ss.name])
    P.op("act", lambda e: e.activation(out=rs[:], in_=ss[:], func=AF.Ln, bias=k.epsT[:], scale=1.0 / width), reads=[ss.name, "epsT"], writes=[rs.name])
    P.op("act", lambda e: e.activation(out=rs[:], in_=rs[:], func=AF.Exp, scale=-0.5), reads=[rs.name], writes=[rs.name])
    for a in range(nchunk):
        P.op("dve", lambda e, a=a: e.scalar_tensor_tensor(out=dst[:, a, :], in0=src[:, a, :], scalar=gain[:, a:a + 1], in1=rs[:], op0=ALU.mult, op1=ALU.mult),
             reads=[src.name, gain.name, rs.name], writes=[dst.name])


def phase_mla(k, l):
    P = k.P
    SC = 96.0 ** -0.5
    with ExitStack() as st:
        with ExitStack() as st2:
            C2 = k.sb("la_C2", [32, S], F32, st2)
            S2 = k.sb("la_S2", [32, S], F32, st2)
            C2q = k.sb("la_C2q", [32, S], F32, st2)
            S2q = k.sb("la_S2q", [32, S], F32, st2)
            fr = k.sb("la_fr", [32, 4], F32, st2)
            if l == 0:
                st3 = ExitStack()
                posi = k.sb("la_posi", [32, S], I32, st3)
                ang = k.sb("la_ang", [32, S], F32, st3)
                tmp = k.sb("la_tmp", [32, S], F32, st3)
                red = k.sb("la_red", [32, S], F32, st3)
                P.dma("sp", lambda e: e.dma_start(out=fr[:], in_=k.c_freq[:, :]), writes=["la_fr"])
                P.dma("sp", lambda e: e.dma_start(out=posi[:], in_=k.pos.partition_broadcast(32)), writes=["la_posi"])
                P.op("dve", lambda e: e.tensor_copy(out=ang[:], in_=posi[:]), reads=["la_posi"], writes=["la_ang"])
                P.op("dve", lambda e: e.tensor_scalar(out=ang[:], in0=ang[:], scalar1=fr[:, 0:1], scalar2=None, op0=ALU.mult), reads=["la_ang", "la_fr"], writes=["la_ang"])
                TWO_PI = 2 * math.pi

                def reduce_to_pi(src_off):
                    P.op("dve", lambda e: e.tensor_scalar(out=tmp[:], in0=ang[:], scalar1=float(src_off), scalar2=None, op0=ALU.add), reads=["la_ang", "la_S2", "la_C2"], writes=["la_tmp"])
                    P.op("dve", lambda e: e.tensor_scalar(out=red[:], in0=tmp[:], scalar1=1.0 / TWO_PI, scalar2=None, op0=ALU.mult), reads=["la_tmp"], writes=["la_red"])
                    P.op("dve", lambda e: e.tensor_copy(out=posi[:], in_=red[:]), reads=["la_red"], writes=["la_posi"])
                    P.op("dve", lambda e: e.tensor_copy(out=red[:], in_=posi[:]), reads=["la_posi"], writes=["la_red"])
                    P.op("dve", lambda e: e.tensor_scalar(out=red[:], in0=red[:], scalar1=-TWO_PI, scalar2=None, op0=ALU.mult), reads=["la_red"], writes=["la_red"])
                    P.op("dve", lambda e: e.tensor_tensor(out=tmp[:], in0=tmp[:], in1=red[:], op=ALU.add), reads=["la_tmp", "la_red"], writes=["la_tmp"])
                    for (cmp_op, thr, corr) in ((ALU.is_gt, math.pi, -TWO_PI), (ALU.is_lt, -math.pi, TWO_PI)):
                        P.op("dve", lambda e, cmp_op=cmp_op, thr=thr: e.tensor_scalar(out=red[:], in0=tmp[:], scalar1=float(thr), scalar2=None, op0=cmp_op), reads=["la_tmp"], writes=["la_red"])
                        P.op("dve", lambda e, corr=corr: e.tensor_scalar(out=red[:], in0=red[:], scalar1=float(corr), scalar2=None, op0=ALU.mult), reads=["la_red"], writes=["la_red"])
                        P.op("dve", lambda e: e.tensor_tensor(out=tmp[:], in0=tmp[:], in1=red[:], op=ALU.add), reads=["la_tmp", "la_red"], writes=["la_tmp"])
                reduce_to_pi(0.0)
                P.op("act", lambda e: e.activation(out=S2[:], in_=tmp[:], func=AF.Sin, scale=fr[:, 1:2]), reads=["la_tmp", "la_fr"], writes=["la_S2"])
                reduce_to_pi(0.5 * math.pi)
                P.op("act", lambda e: e.activation(out=C2[:], in_=tmp[:], func=AF.Sin), reads=["la_tmp"], writes=["la_C2"])
                P.op("pool", lambda e: e.tensor_scalar(out=C2q[:], in0=C2[:], scalar1=SC, scalar2=None, op0=ALU.mult), reads=["la_C2"], writes=["la_C2q"])
                P.op("pool", lambda e: e.tensor_scalar(out=S2q[:], in0=S2[:], scalar1=SC, scalar2=None, op0=ALU.mult), reads=["la_S2"], writes=["la_S2q"])
                P.barrier()
                st3.close()
                for ti, T in enumerate((C2, S2, C2q, S2q)):
                    P.dma("sp", lambda e, ti=ti, T=T: e.dma_start(out=k.ropeT[ti], in_=T[:]), reads=[T.name], writes=["ropeT"])
            else:
                for ti, T in enumerate((C2, S2, C2q, S2q)):
                    P.dma("sp", lambda e, ti=ti, T=T: e.dma_start(out=T[:], in_=k.ropeT[ti]), reads=["ropeT"], writes=[T.name])
            stage = [k.sb("la_stg%d" % i, [128, 2048], F32, st2) for i in range(2)]
            k.stage_i = 0
            Wq = k.sb("la_Wq", [128, 3, 1024], BF16, st2)
            Wk = k.sb("la_Wk", [128, 2, 768], BF16, st2)
            Wv = k.sb("la_Wv", [128, 2, 512], BF16, st2)
            load_cast(k, st2, "wuq", k.wuq[l], 128, 3, 1024, Wq, "la_Wq", stage)
            load_cast(k, st2, "wuk", k.wuk[l], 128, 2, 768, Wk, "la_Wk", stage)
            load_cast(k, st2, "wuv", k.wuv[l], 128, 2, 512, Wv, "la_Wv", stage)
            qng = k.sb("la_qn", [128, 3], F32, st2)
            kvng = k.sb("la_kvn", [128, 2], F32, st2)
            P.dma("sp", lambda e: e.dma_start(out=qng[:], in_=k.qn[l]), writes=["la_qn"])
            P.dma("sp", lambda e: e.dma_start(out=kvng[:], in_=k.kvn[l]), writes=["la_kvn"])
            cq = k.sb("la_cq", [128, 3, 512], BF16, st2)
            ckv = k.sb("la_ckv", [128, 2, 512], BF16, st2)
            cqn = k.sb("la_cqn", [128, 3, 512], BF16, st2)
            ckvn = k.sb("la_ckvn", [128, 2, 512], BF16, st2)
            sq = k.sb("la_sq", [128, 3, 512], BF16, st2)
            rs = k.sb("la_rs", [128, 512], F32, st2)
            krb = k.sb("la_krb", [32, 2, 512], BF16, st2)
            krr = k.sb("la_krr", [32, 512], F32, st2)
            t1 = k.sb("la_t1", [32, 512], F32, st2)
            t2 = k.sb("la_t2", [32, 512], F32, st2)
            qa2 = [k.sb("la_qa%d" % i, [96, 512], BF16, st2) for i in range(2)]
            ka2 = [k.sb("la_ka%d" % i, [96, 512], BF16, st2) for i in range(2)]
            vo2 = [k.sb("la_vo%d" % i, [128, 512], BF16, st2) for i in range(2)]
            c_cq, c_ckv, c_kr = FMI["cq0"], FMI["ckv0"], FMI["kr"]
            for tb in range(NTB):
                tsl = slice(tb * 512, (tb + 1) * 512)
                P.dma("sp", lambda e, tsl=tsl: e.dma_start(out=cq[:], in_=k.zT[c_cq * 128:(c_cq + 3) * 128, tsl].rearrange("(a p) t -> p a t", p=128)), reads=[("zT", c_cq + i) for i in range(3)], writes=["la_cq"])
                P.dma("sp", lambda e, tsl=tsl: e.dma_start(out=ckv[:], in_=k.zT[c_ckv * 128:(c_ckv + 2) * 128, tsl].rearrange("(a p) t -> p a t", p=128)), reads=[("zT", c_ckv + i) for i in range(2)], writes=["la_ckv"])
                P.dma("sp", lambda e, tsl=tsl: e.dma_start(out=krb[:], in_=k.zT[c_kr * 128:(c_kr + 2) * 128, tsl].rearrange("(a p) t -> p a t", p=128)[0:32]), reads=[("zT", c_kr), ("zT", c_kr + 1)], writes=["la_krb"])
                rms_fm(k, cq, 3, qng, cqn, sq, rs, 384)
                rms_fm(k, ckv, 2, kvng, ckvn, sq, rs, 256)
                P.op("dve", lambda e, tsl=tsl: e.tensor_tensor(out=krr[:], in0=krb[:, 0, :], in1=C2[:, tsl], op=ALU.mult), reads=["la_krb", "la_C2"], writes=["la_krr"])
                P.op("dve", lambda e, tsl=tsl: e.tensor_tensor(out=t1[:], in0=krb[:, 1, :], in1=S2[:, tsl], op=ALU.mult), reads=["la_krb", "la_S2"], writes=["la_t1"])
                P.op("dve", lambda e: e.tensor_tensor(out=krr[:], in0=krr[:], in1=t1[:], op=ALU.add), reads=["la_krr", "la_t1"], writes=["la_krr"])
                for h in range(8):
                    qa, ka = qa2[h % 2], ka2[h % 2]
                    psq, psr, psk = k.ps[2 + (h % 2)], k.ps[4], k.ps[5]
                    for a in range(3):
                        mm(k, psq[0:96, :], Wq[:, a, h * 128:h * 128 + 96], cqn[:, a, :], a == 0, a == 2, ["la_Wq", "la_cqn"], [psq.name])
                    for a in range(3):
                        mm(k, psr[0:32, :], Wq[:, a, h * 128 + 96:h * 128 + 128], cqn[:, a, :], a == 0, a == 2, ["la_Wq", "la_cqn"], [psr.name])
                    P.op("act", lambda e, qa=qa, psq=psq: e.activation(out=qa[32:64, :], in_=psq[32:64, :], func=AF.Copy, scale=SC), reads=[psq.name], writes=[qa.name])
                    P.op("act", lambda e, qa=qa, psq=psq: e.activation(out=qa[64:96, :], in_=psq[64:96, :], func=AF.Copy, scale=SC), reads=[psq.name], writes=[qa.name])
                    P.op("dve", lambda e, psq=psq, tsl=tsl: e.tensor_tensor(out=t1[:], in0=psq[0:32, :], in1=C2q[:, tsl], op=ALU.mult), reads=[psq.name, "la_C2q"], writes=["la_t1"])
                    P.op("dve", lambda e, psr=psr, tsl=tsl: e.tensor_tensor(out=t2[:], in0=psr[0:32, :], in1=S2q[:, tsl], op=ALU.mult), reads=[psr.name, "la_S2q"], writes=["la_t2"])
                    P.op("pool", lambda e, qa=qa: e.tensor_tensor(out=qa[0:32, :], in0=t1[:], in1=t2[:], op=ALU.add), reads=["la_t1", "la_t2"], writes=[qa.name])
                    P.dma("sp", lambda e, qa=qa, h=h, tsl=tsl: e.dma_start(out=k.qaT[h * 96:(h + 1) * 96, tsl], in_=qa[:]), reads=[qa.name], writes=[("qaT", h)])
                    for a in range(2):
                        mm(k, psk[0:96, :], Wk[:, a, h * 96:(h + 1) * 96], ckvn[:, a, :], a == 0, a == 1, ["la_Wk", "la_ckvn"], [psk.name])
                    P.op("act", lambda e, ka=ka, psk=psk: e.copy(out=ka[32:64, :], in_=psk[32:64, :]), reads=[psk.name], writes=[ka.name])
                    P.op("dve", lambda e, ka=ka, psk=psk: e.tensor_copy(out=ka[64:96, :], in_=psk[64:96, :]), reads=[psk.name], writes=[ka.name])
                    P.op("pool", lambda e, ka=ka: e.tensor_copy(out=ka[0:32, :], in_=krr[:]), reads=["la_krr"], writes=[ka.name])
                    P.dma("sp", lambda e, ka=ka, h=h, tsl=tsl: e.dma_start(out=k.kaT[h * 96:(h + 1) * 96, tsl], in_=ka[:]), reads=[ka.name], writes=[("kaT", h)])
                for j in range(4):
                    psv = k.ps[2 + (j % 2)]
                    vo = vo2[j % 2]
                    for a in range(2):
                        mm(k, psv[:], ckvn[:, a, j * 128:(j + 1) * 128], Wv[:, a, :], a == 0, a == 1, ["la_Wv", "la_ckvn"], [psv.name])
                    P.op("dve", lambda e, vo=vo, psv=psv: e.tensor_copy(out=vo[:], in_=psv[:]), reads=[psv.name], writes=[vo.name])
                    n = tb * 4 + j
                    P.dma("sp", lambda e, vo=vo, n=n: e.dma_start(out=k.vml[n * 128:(n + 1) * 128, :], in_=vo[:]), reads=[vo.name], writes=["vml"])
            P.barrier()
        attn_scratch(k, st)
        KT2 = [k.sb("la_K%d" % i, [96, S], BF16, st) for i in range(2)]
        QT2 = [k.sb("la_Q%d" % i, [96, S], BF16, st) for i in range(2)]
        V2 = [k.sb("la_V%d" % i, [128, NT, 128], BF16, st) for i in range(2)]
        rd = k.sb("la_rd", [128, 512], F32, st)
        yo2 = [k.sb("la_yo%d" % i, [64, 512], BF16, st) for i in range(2)]
        for i in range(2):
            P.op("pool", lambda e, i=i: e.memset(V2[i][:, :, 64:128], 1.0), writes=[V2[i].name])
        pf = std_pf(k)
        def load_head(h):
            KT, QT, V = KT2[h % 2], QT2[h % 2], V2[h % 2]
            P.dma("sp", lambda e, KT=KT, h=h: e.dma_start(out=KT[:], in_=k.kaT[h * 96:(h + 1) * 96, :]), reads=[("kaT", h)], writes=[KT.name])
            P.dma("sp", lambda e, QT=QT, h=h: e.dma_start(out=QT[:], in_=k.qaT[h * 96:(h + 1) * 96, :]), reads=[("qaT", h)], writes=[QT.name])
            P.dma("sp", lambda e, V=V, h=h: e.dma_start(out=V[:, :, 0:64], in_=k.vml[:, h * 64:(h + 1) * 64].rearrange("(n p) c -> p n c", p=128)), reads=["vml"], writes=[V.name])
        load_head(0)
        for h in range(8):
            KT, QT, V = KT2[h % 2], QT2[h % 2], V2[h % 2]
            for tb in range(NTB):
                if tb == NTB - 1 and h + 1 < 8:
                    load_head(h + 1)
                tsl = slice(tb * 512, (tb + 1) * 512)
                acc = k.ps[3 + (tb % 2)]

                def qk(i, step, ps, KT=KT, QT=QT, tsl=tsl):
                    s_t = step[0]
                    mm(k, ps[:], KT[:, s_t * 128:(s_t + 1) * 128], QT[:, tsl], True, True, [KT.name, QT.name], [ps.name])

                def pv(i, step, pt, first, lastf, V=V, acc=acc):
                    mm(k, acc[:], V[:, step[0], :], pt[:], first, lastf, [V.name, pt.name], [acc.name])
                attn_tb(k, causal_steps(tb), qk, pf, pv)
                yo = yo2[tb % 2]
                P.op("dve", lambda e, acc=acc: e.reciprocal(out=rd[64:128, :], in_=acc[64:128, :]), reads=[acc.name], writes=["la_rd"])
                P.op("dve", lambda e, acc=acc, yo=yo: e.tensor_tensor(out=yo[:], in0=acc[0:64, :], in1=rd[64:128, :], op=ALU.mult), reads=[acc.name, "la_rd"], writes=[yo.name])
                P.dma("sp", lambda e, yo=yo, h=h, tsl=tsl: e.dma_start(out=k.yT[1024 + h * 64:1024 + (h + 1) * 64, tsl], in_=yo[:]), reads=[yo.name], writes=[("yT", 8 + h)])
        P.barrier()


def phase_merge(k, l, xin, xout):
    P = k.P
    with ExitStack() as st:
        stage = [k.sb("mg_stg%d" % i, [128, 2048], F32, st) for i in range(2)]
        k.stage_i = 0
        Wb = [k.sb("mg_Wb%d" % i, [128, 4, D], BF16, st) for i in range(3)]
        Wo = k.sb("mg_Wo", [128, 8, D], BF16, st)
        for i in range(3):
            load_cast(k, st, "wbr", k.wbr[l][i], 128, 4, D, Wb[i], Wb[i].name, stage)
        load_cast(k, st, "wout", k.wout[l], 128, 8, D, Wo, "mg_Wo", stage)
        yb = [k.sb("mg_y%d" % i, [128, 12, 512], BF16, st) for i in range(2)]
        gb = [k.sb("mg_g%d" % i, [128, 24, 512], BF16, st) for i in range(2)]
        mT = k.sb("mg_mT", [128, 8, 512], BF16, st)
        mtmp = [[k.sb("mg_t%d_%d" % (i, j), [128, 512], BF16, st) for j in range(2)] for i in range(3)]
        xt2 = [k.sb("mg_x%d" % i, [128, D], F32, st) for i in range(2)]
        ot2 = [k.sb("mg_o%d" % i, [128, D], F32, st) for i in range(2)]
        junk = k.sb("mg_junk", [128, D], BF16, st)
        ss2 = [k.sb("mg_ss%d" % i, [128, 1], F32, st) for i in range(2)]
        rs2 = [k.sb("mg_rs%d" % i, [128, 1], F32, st) for i in range(2)]
        g1 = k.sb("mg_g1", [128, D], F32, st)
        P.dma("sp", lambda e: e.dma_start(out=g1[:], in_=k.gain_rows[l * 4 + 1].partition_broadcast(128)), writes=["mg_g1"])
        cg = FMI["gt0"]
        def load_tb(tb):
            tsl = slice(tb * 512, (tb + 1) * 512)
            y, g = yb[tb % 2], gb[tb % 2]
            P.dma("sp", lambda e, y=y, tsl=tsl: e.dma_start(out=y[:], in_=k.yT[:, tsl].rearrange("(a p) t -> p a t", p=128)), reads=[("yT", i) for i in range(16)], writes=[y.name])
            P.dma("sp", lambda e, g=g, tsl=tsl: e.dma_start(out=g[:], in_=k.zT[cg * 128:(cg + 24) * 128, tsl].rearrange("(a p) t -> p a t", p=128)), reads=[("zT", cg + i) for i in range(24)], writes=[g.name])
        load_tb(0)
        for tb in range(NTB):
            tsl = slice(tb * 512, (tb + 1) * 512)
            y, g = yb[tb % 2], gb[tb % 2]
            if tb + 1 < NTB:
                load_tb(tb + 1)
            for oc in range(8):
                for i in range(3):
                    ps = k.ps[(oc * 3 + i) % 2]
                    for a in range(4):
                        mm(k, ps[:], Wb[i][:, a, oc * 128:(oc + 1) * 128], y[:, i * 4 + a, :], a == 0, a == 3, [Wb[i].name, y.name], [ps.name])
                    tt = mtmp[i][oc % 2]
                    P.op("dve", lambda e, ps=ps, g=g, oc=oc, i=i, tt=tt: e.tensor_tensor(out=tt[:], in0=ps[:], in1=g[:, i * 8 + oc, :], op=ALU.mult), reads=[ps.name, g.name], writes=[tt.name])
                    if i == 1:
                        t0, t1 = mtmp[0][oc % 2], mtmp[1][oc % 2]
                        P.op("pool", lambda e, t0=t0, t1=t1: e.tensor_tensor(out=t0[:], in0=t0[:], in1=t1[:], op=ALU.add), reads=[t0.name, t1.name], writes=[t0.name])
                    elif i == 2:
                        t0, t2 = mtmp[0][oc % 2], mtmp[2][oc % 2]
                        P.op("dve", lambda e, oc=oc, t0=t0, t2=t2: e.tensor_tensor(out=mT[:, oc, :], in0=t0[:], in1=t2[:], op=ALU.add), reads=[t0.name, t2.name], writes=["mg_mT"])
            for j in range(4):
                n = tb * 4 + j
                xt, ot, ss, rs = xt2[j % 2], ot2[j % 2], ss2[j % 2], rs2[j % 2]
                P.dma("sp", lambda e, xt=xt, n=n: e.dma_start(out=xt[:], in_=xin[n * 128:(n + 1) * 128, :]), reads=[("dram", xin.name)], writes=[xt.name])
                pss = [k.ps[2 + 2 * (j % 2)], k.ps[3 + 2 * (j % 2)]]
                for c2 in range(2):
                    for a in range(8):
                        mm(k, pss[c2][:], mT[:, a, j * 128:(j + 1) * 128], Wo[:, a, c2 * 512:(c2 + 1) * 512], a == 0, a == 7, ["mg_mT", "mg_Wo"], [pss[c2].name])
                resid_norm(k, pss, xt, ot, ss, rs, junk, g1, xout, n)
        P.barrier()


def resid_norm(k, pss, xt, ot, ss, rs, junk, grow, xout, n):
    P = k.P
    P.op("pool", lambda e: e.memset(ss[:], 0.0), writes=[ss.name])
    for c2 in range(2):
        P.op("act", lambda e, c2=c2: e.activation(out=ot[:, c2 * 512:(c2 + 1) * 512], in_=pss[c2][:], func=AF.Copy), reads=[pss[c2].name], writes=[ot.name])
    P.op("act", lambda e: e.activation(out=junk[:], in_=ot[:], func=AF.Square, accum_out=ss[:]), reads=[ot.name, ss.name], writes=[junk.name, ss.name])
    P.op("act", lambda e: e.activation(out=rs[:], in_=ss[:], func=AF.Sqrt, bias=k.epsT[:], scale=1.0 / D), reads=[ss.name, "epsT"], writes=[rs.name])
    P.op("dve", lambda e: e.reciprocal(out=rs[:], in_=rs[:]), reads=[rs.name], writes=[rs.name])
    P.op("dve", lambda e: e.scalar_tensor_tensor(out=ot[:], in0=ot[:], scalar=rs[:, 0:1], in1=grow[:], op0=ALU.mult, op1=ALU.mult), reads=[ot.name, rs.name, grow.name], writes=[ot.name])
    P.op("pool", lambda e: e.tensor_tensor(out=ot[:], in0=ot[:], in1=xt[:], op=ALU.add), reads=[ot.name, xt.name], writes=[ot.name])
    P.dma("sp", lambda e: e.dma_start(out=xout[n * 128:(n + 1) * 128, :], in_=ot[:]), reads=[ot.name], writes=[("dram", xout.name)])


def phase_ffn(k, l, xin, xout):
    P = k.P
    with ExitStack() as st:
        stage = [k.sb("ff_stg%d" % i, [128, 1024], F32, st) for i in range(2)]
        k.stage_i = 0
        Wg = k.sb("ff_Wg", [128, 8, DFF], BF16, st)
        Wu = k.sb("ff_Wu", [128, 8, DFF], BF16, st)
        Wd = k.sb("ff_Wd", [128, NFC, D], BF16, st)
        for c0 in range(0, DFF, 512):
            cw = min(512, DFF - c0)
            for (W, src) in ((Wg, k.wg), (Wu, k.wu)):
                per = 1024 // cw
                for kc in range(0, 8, per):
                    m = min(per, 8 - kc)
                    stg = stage[k.stage_i % 2]
                    k.stage_i += 1
                    sv = stg[:, 0:m * cw].rearrange("p (a c) -> p a c", a=m)
                    srcv = src[l][kc * 128:(kc + m) * 128, c0:c0 + cw].rearrange("(a p) c -> p a c", p=128)
                    P.dma("sp", lambda e, sv=sv, srcv=srcv: e.dma_start(out=sv, in_=srcv), writes=[stg.name])
                    eng = ("dve", "act", "dve", "pool")[k.stage_i % 4]
                    if eng == "act":
                        P.op("act", lambda e, W=W, kc=kc, m=m, c0=c0, cw=cw, sv=sv: e.copy(out=W[:, kc:kc + m, c0:c0 + cw], in_=sv), reads=[stg.name], writes=[W.name])
                    else:
                        P.op(eng, lambda e, W=W, kc=kc, m=m, c0=c0, cw=cw, sv=sv: e.tensor_copy(out=W[:, kc:kc + m, c0:c0 + cw], in_=sv), reads=[stg.name], writes=[W.name])
        load_cast(k, st, "wd", k.wd[l], 128, NFC, D, Wd, "ff_Wd", stage, sw=1024)
        hT = k.sb("ff_hT", [128, 8, 512], BF16, st)
        aT = k.sb("ff_aT", [128, NFC, 512], BF16, st)
        sg = [k.sb("ff_sg%d" % i, [128, 512], BF16, st) for i in range(2)]
        xs = [k.sb("ff_xs%d" % i, [128, D], F32, st) for i in range(2)]
        ot2 = [k.sb("ff_o0", [128, D], F32, st)] * 2
        ss2 = [k.sb("ff_ss%d" % i, [128, 1], F32, st) for i in range(2)]
        rs2 = [k.sb("ff_rs%d" % i, [128, 1], F32, st) for i in range(2)]
        xn2 = [k.sb("ff_xn%d" % i, [128, D], BF16, st) for i in range(2)]
        g3 = k.sb("ff_g3", [128, D], F32, st)
        P.dma("sp", lambda e: e.dma_start(out=g3[:], in_=k.gain_rows[l * 4 + 3].partition_broadcast(128)), writes=["ff_g3"])
        gbc = k.gn[:, l * 4 + 2, :].unsqueeze(2).broadcast_to([128, 8, 128])
        for tb in range(NTB):
            for j in range(4):
                n = tb * 4 + j
                xt, xn, ss, rs = xs[j % 2], xn2[j % 2], ss2[j % 2], rs2[j % 2]
                P.dma("sp", lambda e, xt=xt, n=n: e.dma_start(out=xt[:], in_=xin[n * 128:(n + 1) * 128, :]), reads=[("dram", xin.name)], writes=[xt.name])
                norm_tile_T(k, "ff", xt, ss, rs, xn, n)
                for kc in range(8):
                    P.op("pe", lambda e, kc=kc, xn=xn: e.transpose(out=k.psb[:, kc * 128:(kc + 1) * 128], in_=xn[:, kc * 128:(kc + 1) * 128], identity=k.ident[:]), reads=[xn.name, "ident"], writes=["ps7"])
                P.op("dve", lambda e, j=j: e.tensor_tensor(out=hT[:, :, j * 128:(j + 1) * 128], in0=k.psb.rearrange("p (a t) -> p a t", a=8), in1=gbc, op=ALU.mult), reads=["ps7", "gn"], writes=["ff_hT"])
            for fc in range(NFC):
                psg, psu = (k.ps[0], k.ps[1]) if fc % 2 == 0 else (k.ps[2], k.ps[3])
                for a in range(8):
                    mm(k, psg[:], Wg[:, a, fc * 128:(fc + 1) * 128], hT[:, a, :], a == 0, a == 7, ["ff_Wg", "ff_hT"], [psg.name])
                for a in range(8):
                    mm(k, psu[:], Wu[:, a, fc * 128:(fc + 1) * 128], hT[:, a, :], a == 0, a == 7, ["ff_Wu", "ff_hT"], [psu.name])
                s_ = sg[fc % 2]
                P.op("act", lambda e, s_=s_, psg=psg: e.activation(out=s_[:], in_=psg[:], func=AF.Silu), reads=[psg.name], writes=[s_.name])
                P.op("dve", lambda e, s_=s_, psu=psu, fc=fc: e.tensor_tensor(out=aT[:, fc, :], in0=psu[:], in1=s_[:], op=ALU.mult), reads=[psu.name, s_.name], writes=["ff_aT"])
            for j in range(4):
                n = tb * 4 + j
                pss = [k.ps[4], k.ps[5]]
                for c2 in range(2):
                    for fc in range(NFC):
                        mm(k, pss[c2][:], aT[:, fc, j * 128:(j + 1) * 128], Wd[:, fc, c2 * 512:(c2 + 1) * 512], fc == 0, fc == NFC - 1, ["ff_aT", "ff_Wd"], [pss[c2].name])
                xt = xs[j % 2]
                P.dma("sp", lambda e, xt=xt, n=n: e.dma_start(out=xt[:], in_=xin[n * 128:(n + 1) * 128, :]), reads=[("dram", xin.name)], writes=[xt.name])
                resid_norm(k, pss, xt, ot2[j % 2], ss2[j % 2], rs2[j % 2], xn2[j % 2], g3, xout, n)
        P.barrier()


def phase_nsa(k, l):
    P = k.P
    c_bq, c_kcr, c_vcr, c_kslc, c_kwin, c_bg = FMI["bq0"], FMI["kcr"], FMI["vcr"], FMI["kslc"], FMI["kwin"], FMI["bg"]
    with ExitStack() as st:
        attn_scratch(k, st)
        G24 = k.sb("ns_G", [24, S], BF16, st)
        gsel = k.sb("ns_gsel", [24, 24 * 128], BF16, st)
        impacc = k.sb("ns_imp", [64, 2, S], F32, st)
        kcmpT = [k.sb("ns_kc%d" % g, [64, 256], BF16, st) for g in range(2)]
        Vc = [k.sb("ns_vc%d" % g, [128, 2, 128], BF16, st) for g in range(2)]
        gr = [k.sb("ns_gr%d" % i, [128, 512], F32, st) for i in range(2)]
        rd = k.sb("ns_rd", [128, 512], F32, st)
        coef = k.sb("ns_coef", [128, 512], F32, st)
        P.dma("sp", lambda e: e.dma_start(out=G24[:], in_=k.zT[c_bg * 128:c_bg * 128 + 24, :]), reads=[("zT", c_bg)], writes=["ns_G"])
        P.dma("sp", lambda e: e.dma_start(out=gsel[:], in_=k.c_gsel[:, :]), writes=["ns_gsel"])
        for g in range(2):
            P.op("pool", lambda e, g=g: e.memset(Vc[g][:, :, 64:128], 1.0), writes=[Vc[g].name])

        def grep(h, b, tsl, dst):
            psg = k.ps[7]
            col = (h * 3 + b) * 128
            mm(k, psg[:], gsel[:, col:col + 128], G24[:, tsl], True, True, ["ns_gsel", "ns_G"], [psg.name])
            P.op("act", lambda e: e.copy(out=dst[:], in_=psg[:]), reads=[psg.name], writes=[dst.name])

        with ExitStack() as st2:
            stage = [k.sb("ns_stg%d" % i, [128, 2048], F32, st2) for i in range(2)]
            W1 = k.sb("ns_W1", [64, 32, 128], BF16, st2)
            W2 = k.sb("ns_W2", [128, 64], BF16, st2)
            posT = k.sb("ns_pos", [64, 64], BF16, st2)
            xT = k.sb("ns_xT", [64, S], BF16, st2)
            hs = k.sb("ns_hs", [128, 256], BF16, st2)
            bsb = k.sb("ns_bsb", [128, 1], F32, st2)
            P.op("pool", lambda e: e.memset(hs[:], 0.0), writes=["ns_hs"])
            P.dma("sp", lambda e: e.dma_start(out=stage[0][0:64, 0:64], in_=k.cpos[l]), writes=[stage[0].name])
            P.op("dve", lambda e: e.tensor_copy(out=posT[:], in_=stage[0][0:64, 0:64]), reads=[stage[0].name], writes=["ns_pos"])
            for kv in range(2):
                for half in range(2):
                    stg = stage[half]
                    P.dma("sp", lambda e, stg=stg, kv=kv, half=half: e.dma_start(out=stg[0:64, :], in_=k.cw1[l][kv][:, half * 2048:(half + 1) * 2048]), writes=[stg.name])
                    P.op("dve", lambda e, stg=stg, half=half: e.tensor_copy(out=W1[:, half * 16:(half + 1) * 16, :], in_=stg[0:64, :].rearrange("p (a c) -> p a c", a=16)), reads=[stg.name], writes=["ns_W1"])
                P.dma("sp", lambda e, kv=kv: e.dma_start(out=stage[0][:, 0:64], in_=k.cw2[l][kv]), writes=[stage[0].name])
                P.op("dve", lambda e: e.tensor_copy(out=W2[:], in_=stage[0][:, 0:64]), reads=[stage[0].name], writes=["ns_W2"])
                for g in range(2):
                    cc = c_kcr if kv == 0 else c_vcr
                    P.dma("sp", lambda e, cc=cc, g=g: e.dma_start(out=xT[:], in_=k.zT[cc * 128 + g * 64:cc * 128 + g * 64 + 64, :]), reads=[("zT", cc)], writes=["ns_xT"])
                    hid, bps = k.ps[2], k.ps[3]
                    for li in range(32):
                        mm(k, hid[:, 0:255], W1[:, li, :], xT[:, li:li + 4065:16], li == 0, li == 31, ["ns_W1", "ns_xT"], [hid.name])
                    for li in range(32):
                        mm(k, bps[:, 0:1], W1[:, li, :], posT[:, kv * 32 + li:kv * 32 + li + 1], li == 0, li == 31, ["ns_W1", "ns_pos"], [bps.name])
                    P.op("dve", lambda e, bps=bps: e.tensor_copy(out=bsb[:], in_=bps[:, 0:1]), reads=[bps.name], writes=["ns_bsb"])
                    P.op("act", lambda e, hid=hid: e.activation(out=hs[:, 0:255], in_=hid[:, 0:255], func=AF.Silu, bias=bsb[:], scale=1.0), reads=[hid.name, "ns_bsb"], writes=["ns_hs"])
                    if kv == 0:
                        pk = k.ps[4]
                        mm(k, pk[0:64, 0:256], W2[:], hs[:], True, True, ["ns_W2", "ns_hs"], [pk.name])
                        P.op("dve", lambda e, g=g, pk=pk: e.tensor_copy(out=kcmpT[g][:], in_=pk[0:64, 0:256]), reads=[pk.name], writes=[kcmpT[g].name])
                    else:
                        for ch in range(2):
                            pv_ = k.ps[4 + ch]
                            mm(k, pv_[:, 0:64], hs[:, ch * 128:(ch + 1) * 128], W2[:], True, True, ["ns_W2", "ns_hs"], [pv_.name])
                            P.op("dve", lambda e, g=g, ch=ch, pv_=pv_: e.tensor_copy(out=Vc[g][:, ch, 0:64], in_=pv_[:, 0:64]), reads=[pv_.name], writes=[Vc[g].name])
            P.barrier()
        with ExitStack() as st2:
            cm = k.sb("ns_cm", [128, 2, S], BF16, st2)
            ovl = k.sb("ns_ovl", [128, 2, 64], BF16, st2)
            P.dma("sp", lambda e: e.dma_start(out=cm[:].rearrange("p a b -> p (a b)"), in_=k.c_cm[:, :]), writes=["ns_cm"])
            P.dma("sp", lambda e: e.dma_start(out=ovl[:].rearrange("p a b -> p (a b)"), in_=k.c_ovl[:, :]), writes=["ns_ovl"])
            QT2 = [k.sb("ns_Q%d" % i, [64, S], BF16, st2) for i in range(2)]
            tmpi = k.sb("ns_tmpi", [64, 512], F32, st2)
            yo2 = [k.sb("ns_yc%d" % i, [64, 512], BF16, st2) for i in range(2)]
            for h in range(8):
                g = h // 4
                QT = QT2[h % 2]
                cq = c_bq + h // 2
                r0 = (h % 2) * 64
                P.dma("sp", lambda e, QT=QT, cq=cq, r0=r0: e.dma_start(out=QT[:], in_=k.zT[cq * 128 + r0:cq * 128 + r0 + 64, :]), reads=[("zT", cq)], writes=[QT.name])
                for tb in range(NTB):
                    tsl = slice(tb * 512, (tb + 1) * 512)
                    steps = [(0, True if tb < 5 else None)] + ([(1, True)] if tb >= 4 else [])
                    acc, impu = (k.ps[3], k.ps[5]) if tb % 2 == 0 else (k.ps[4], k.ps[6])
                    grep(h, 0, tsl, gr[0])

                    def qk(i, step, ps, QT=QT, g=g, tsl=tsl):
                        ch = step[0]
                        mm(k, ps[:], kcmpT[g][:, ch * 128:(ch + 1) * 128], QT[:, tsl], True, True, [kcmpT[g].name, QT.name], [ps.name])

                    def pf(i, step, ps, pt, tsl=tsl):
                        ch, mk = step
                        P.op("act", lambda e: e.activation(out=pt[:], in_=ps[:], func=AF.Exp), reads=[ps.name], writes=[pt.name])
                        if mk:
                            P.op("dve", lambda e: e.tensor_tensor(out=pt[:], in0=pt[:], in1=cm[:, ch, tsl], op=ALU.mult), reads=[pt.name, "ns_cm"], writes=[pt.name])

                    def pv(i, step, pt, first, lastf, g=g, acc=acc, impu=impu):
                        ch = step[0]
                        mm(k, acc[:], Vc[g][:, ch, :], pt[:], first, lastf, [Vc[g].name, pt.name], [acc.name])
                        mm(k, impu[0:64, :], ovl[:, ch, :], pt[:], first, lastf, ["ns_ovl", pt.name], [impu.name])
                    attn_tb(k, steps, qk, pf, pv)
                    P.op("dve", lambda e, acc=acc: e.tensor_scalar(out=rd[64:128, :], in0=acc[64:128, :], scalar1=1e-30, scalar2=None, op0=ALU.max), reads=[acc.name], writes=["ns_rd"])
                    P.op("act", lambda e: e.activation(out=rd[64:128, :], in_=rd[64:128, :], func=AF.Ln), reads=["ns_rd"], writes=["ns_rd"])
                    P.op("act", lambda e: e.activation(out=rd[64:128, :], in_=rd[64:128, :], func=AF.Exp, scale=-1.0), reads=["ns_rd"], writes=["ns_rd"])
                    if h % 4 == 0:
                        P.op("dve", lambda e, impu=impu, g=g, tsl=tsl: e.tensor_tensor(out=impacc[:, g, tsl], in0=impu[0:64, :], in1=rd[64:128, :], op=ALU.mult), reads=[impu.name, "ns_rd"], writes=["ns_imp"])
                    else:
                        P.op("dve", lambda e, impu=impu: e.tensor_tensor(out=tmpi[:], in0=impu[0:64, :], in1=rd[64:128, :], op=ALU.mult), reads=[impu.name, "ns_rd"], writes=["ns_tmpi"])
                        P.op("pool", lambda e, g=g, tsl=tsl: e.tensor_tensor(out=impacc[:, g, tsl], in0=impacc[:, g, tsl], in1=tmpi[:], op=ALU.add), reads=["ns_imp", "ns_tmpi"], writes=["ns_imp"])
                    P.op("dve", lambda e: e.tensor_tensor(out=coef[64:128, :], in0=rd[64:128, :], in1=gr[0][64:128, :], op=ALU.mult), reads=["ns_rd", gr[0].name], writes=["ns_coef"])
                    yo = yo2[tb % 2]
                    P.op("dve", lambda e, acc=acc, yo=yo: e.tensor_tensor(out=yo[:], in0=acc[0:64, :], in1=coef[64:128, :], op=ALU.mult), reads=[acc.name, "ns_coef"], writes=[yo.name])
                    P.dma("sp", lambda e, yo=yo, h=h, tsl=tsl: e.dma_start(out=k.ycmp[h * 64:(h + 1) * 64, tsl], in_=yo[:]), reads=[yo.name], writes=[("ycmp", h)])
            if "impd" in k.taps:
                P.dma("sp", lambda e: e.dma_start(out=k.impd[:, :].rearrange("(g j) t -> j g t", g=2), in_=impacc[:]), reads=["ns_imp"], writes=["impd"])
            P.barrier()
        with ExitStack() as st2:
            identf = k.sb("ns_idf", [128, 128], F32, st2)
            valid = k.sb("ns_valid", [128, NT, 64], F32, st2)
            addc = k.sb("ns_addc", [128, NT, 64], F32, st2)
            P.dma("sp", lambda e: e.dma_start(out=identf[:], in_=k.c_identf[:, :]), writes=["ns_idf"])
            P.dma("sp", lambda e: e.dma_start(out=valid[:].rearrange("p a b -> p (a b)"), in_=k.c_valid[:, :]), writes=["ns_valid"])
            P.dma("sp", lambda e: e.dma_start(out=addc[:].rearrange("p a b -> p (a b)"), in_=k.c_addc[:, :]), writes=["ns_addc"])
            sc2 = [k.sb("ns_sc%d" % i, [128, 64], F32, st2) for i in range(2)]
            sr2 = [k.sb("ns_sr%d" % i, [128, 64], F32, st2) for i in range(2)]
            se2 = [k.sb("ns_se%d" % i, [128, 64], F32, st2) for i in range(2)]
            m8a = [k.sb("ns_ma%d" % i, [128, 8], F32, st2) for i in range(2)]
            m8b = [k.sb("ns_mb%d" % i, [128, 8], F32, st2) for i in range(2)]
            bst = [k.sb("ns_bst%d" % i, [64, 512], BF16, st2) for i in range(2)]
            m3e4 = k.sb("ns_m3e4", [128, 1], F32, st2)
            P.op("pool", lambda e: e.memset(m3e4[:], -30000.0), writes=["ns_m3e4"])
            for g in range(2):
                for n in range(NT):
                    sc, sr, se, ma, mb = sc2[n % 2], sr2[n % 2], se2[n % 2], m8a[n % 2], m8b[n % 2]
                    tp = k.ps[2 + (n % 2)]
                    P.op("pe", lambda e, tp=tp, g=g, n=n: e.transpose(out=tp[:, 0:64], in_=impacc[:, g, n * 128:(n + 1) * 128], identity=identf[0:64, 0:64]), reads=["ns_imp", "ns_idf"], writes=[tp.name])
                    P.op("dve", lambda e, tp=tp, sc=sc, n=n: e.tensor_tensor(out=sc[:], in0=tp[:, 0:64], in1=valid[:, n, :], op=ALU.mult), reads=[tp.name, "ns_valid"], writes=[sc.name])
                    P.op("dve", lambda e, sc=sc, n=n: e.tensor_tensor(out=sc[:], in0=sc[:], in1=addc[:, n, :], op=ALU.add), reads=[sc.name, "ns_addc"], writes=[sc.name])
                    P.op("dve", lambda e, sc=sc, ma=ma: e.max(out=ma[:], in_=sc[:]), reads=[sc.name], writes=[ma.name])
                    P.op("dve", lambda e, sc=sc, sr=sr, ma=ma: e.match_replace(out=sr[:], in_to_replace=ma[:], in_values=sc[:], imm_value=-1e9), reads=[sc.name, ma.name], writes=[sr.name])
                    P.op("dve", lambda e, sr=sr, mb=mb: e.max(out=mb[:], in_=sr[:]), reads=[sr.name], writes=[mb.name])
                    P.op("pool", lambda e, mb=mb: e.tensor_scalar(out=mb[:, 7:8], in0=mb[:, 7:8], scalar1=-0.5, scalar2=None, op0=ALU.max), reads=[mb.name], writes=[mb.name])
                    P.op("dve", lambda e, sc=sc, se=se, mb=mb: e.tensor_scalar(out=se[:], in0=sc[:], scalar1=mb[:, 7:8], scalar2=None, op0=ALU.is_ge), reads=[sc.name, mb.name], writes=[se.name])
                    tq = k.ps[4 + (n % 2)]
                    P.op("pe", lambda e, tq=tq, se=se: e.transpose(out=tq[0:64, 0:128], in_=se[:], identity=identf[:]), reads=[se.name, "ns_idf"], writes=[tq.name])
                    bs = bst[(n // 4) % 2]
                    P.op("act", lambda e, tq=tq, bs=bs, n=n: e.activation(out=bs[:, (n % 4) * 128:(n % 4 + 1) * 128], in_=tq[0:64, 0:128], func=AF.Identity, bias=m3e4[0:64, :], scale=30000.0), reads=[tq.name, "ns_m3e4"], writes=[bs.name])
                    if n % 4 == 3:
                        tbb = n // 4
                        P.dma("sp", lambda e, bs=bs, g=g, tbb=tbb: e.dma_start(out=k.biasT[g * 64:(g + 1) * 64, tbb * 512:(tbb + 1) * 512], in_=bs[:]), reads=[bs.name], writes=[("biasT", g)])
            P.barrier()
        with ExitStack() as st2:
            kE = [k.sb("ns_kE%d" % g, [128, S], BF16, st2) for g in range(2)]
            kwT = [k.sb("ns_kw%d" % g, [64, S], BF16, st2) for g in range(2)]
            Vs = [k.sb("ns_Vs%d" % g, [128, NT, 128], BF16, st2) for g in range(2)]
            Vw = [k.sb("ns_Vw%d" % g, [128, NT, 128], BF16, st2) for g in range(2)]
            qb2 = [k.sb("ns_qb%d" % i, [128, S], BF16, st2) for i in range(2)]
            ycm2 = [k.sb("ns_ycm%d" % i, [64, 512], BF16, st2) for i in range(2)]
            ys = k.sb("ns_ys", [64, 512], F32, st2)
            yw = k.sb("ns_yw", [64, 512], F32, st2)
            yo2 = [k.sb("ns_yo%d" % i, [64, 512], BF16, st2) for i in range(2)]
            for g in range(2):
                P.dma("sp", lambda e, g=g: e.dma_start(out=kE[g][0:64, :], in_=k.zT[c_kslc * 128 + g * 64:c_kslc * 128 + g * 64 + 64, :]), reads=[("zT", c_kslc)], writes=[kE[g].name])
                P.dma("sp", lambda e, g=g: e.dma_start(out=kE[g][64:128, :], in_=k.c_E[:, :]), writes=[kE[g].name])
                P.dma("sp", lambda e, g=g: e.dma_start(out=kwT[g][:], in_=k.zT[c_kwin * 128 + g * 64:c_kwin * 128 + g * 64 + 64, :]), reads=[("zT", c_kwin)], writes=[kwT[g].name])
                P.op("pool", lambda e, g=g: e.memset(Vs[g][:, :, 64:128], 1.0), writes=[Vs[g].name])
                P.op("pool", lambda e, g=g: e.memset(Vw[g][:, :, 64:128], 1.0), writes=[Vw[g].name])
                P.dma("sp", lambda e, g=g: e.dma_start(out=Vs[g][:, :, 0:64], in_=k.vtm[:, 512 + g * 64:512 + (g + 1) * 64].rearrange("(n p) c -> p n c", p=128)), reads=[("vtm", 512)], writes=[Vs[g].name])
                P.dma("sp", lambda e, g=g: e.dma_start(out=Vw[g][:, :, 0:64], in_=k.vtm[:, 640 + g * 64:640 + (g + 1) * 64].rearrange("(n p) c -> p n c", p=128)), reads=[("vtm", 512)], writes=[Vw[g].name])
            pf = std_pf(k)
            def load_qb(h):
                g = h // 4
                qb = qb2[h % 2]
                cq = c_bq + h // 2
                r0 = (h % 2) * 64
                P.dma("sp", lambda e, qb=qb, cq=cq, r0=r0: e.dma_start(out=qb[0:64, :], in_=k.zT[cq * 128 + r0:cq * 128 + r0 + 64, :]), reads=[("zT", cq)], writes=[qb.name])
                P.dma("sp", lambda e, qb=qb, g=g: e.dma_start(out=qb[64:128, :], in_=k.biasT[g * 64:(g + 1) * 64, :]), reads=[("biasT", g)], writes=[qb.name])
            load_qb(0)
            for h in range(8):
                g = h // 4
                qb = qb2[h % 2]
                for tb in range(NTB):
                    if tb == NTB - 1 and h + 1 < 8:
                        load_qb(h + 1)
                    tsl = slice(tb * 512, (tb + 1) * 512)
                    acs, acw = (k.ps[3], k.ps[5]) if tb % 2 == 0 else (k.ps[4], k.ps[6])
                    ycm = ycm2[tb % 2]
                    P.dma("sp", lambda e, ycm=ycm, h=h, tsl=tsl: e.dma_start(out=ycm[:], in_=k.ycmp[h * 64:(h + 1) * 64, tsl]), reads=[("ycmp", h)], writes=[ycm.name])
                    grep(h, 1, tsl, gr[0])
                    grep(h, 2, tsl, gr[1])

                    def qk_s(i, step, ps, qb=qb, g=g, tsl=tsl):
                        s_t = step[0]
                        mm(k, ps[:], kE[g][:, s_t * 128:(s_t + 1) * 128], qb[:, tsl], True, True, [kE[g].name, qb.name], [ps.name])

                    def pv_s(i, step, pt, first, lastf, g=g, acs=acs):
                        mm(k, acs[:], Vs[g][:, step[0], :], pt[:], first, lastf, [Vs[g].name, pt.name], [acs.name])
                    attn_tb(k, causal_steps(tb), qk_s, pf, pv_s)
                    wsteps = []
                    for s_t in range(max(0, 4 * tb - 4), 4 * tb + 4):
                        wsteps.append((s_t, (s_t - 4 * tb) if s_t >= 4 * tb else 4 + (s_t - (4 * tb - 4))))

                    def qk_w(i, step, ps, qb=qb, g=g, tsl=tsl):
                        s_t = step[0]
                        mm(k, ps[:], kwT[g][:, s_t * 128:(s_t + 1) * 128], qb[0:64, tsl], True, True, [kwT[g].name, qb.name], [ps.name])

                    def pv_w(i, step, pt, first, lastf, g=g, acw=acw):
                        mm(k, acw[:], Vw[g][:, step[0], :], pt[:], first, lastf, [Vw[g].name, pt.name], [acw.name])
                    attn_tb(k, wsteps, qk_w, pf, pv_w)
                    P.op("act", lambda e, acs=acs: e.activation(out=rd[64:128, :], in_=acs[64:128, :], func=AF.Ln), reads=[acs.name], writes=["ns_rd"])
                    P.op("act", lambda e: e.activation(out=rd[64:128, :], in_=rd[64:128, :], func=AF.Exp, scale=-1.0), reads=["ns_rd"], writes=["ns_rd"])
                    P.op("dve", lambda e: e.tensor_tensor(out=coef[64:128, :], in0=rd[64:128, :], in1=gr[0][64:128, :], op=ALU.mult), reads=["ns_rd", gr[0].name], writes=["ns_coef"])
                    P.op("dve", lambda e, acs=acs: e.tensor_tensor(out=ys[:], in0=acs[0:64, :], in1=coef[64:128, :], op=ALU.mult), reads=[acs.name, "ns_coef"], writes=["ns_ys"])
                    P.op("act", lambda e, acw=acw: e.activation(out=rd[64:128, :], in_=acw[64:128, :], func=AF.Ln), reads=[acw.name], writes=["ns_rd"])
                    P.op("act", lambda e: e.activation(out=rd[64:128, :], in_=rd[64:128, :], func=AF.Exp, scale=-1.0), reads=["ns_rd"], writes=["ns_rd"])
                    P.op("dve", lambda e: e.tensor_tensor(out=coef[64:128, :], in0=rd[64:128, :], in1=gr[1][64:128, :], op=ALU.mult), reads=["ns_rd", gr[1].name], writes=["ns_coef"])
                    P.op("dve", lambda e, acw=acw: e.tensor_tensor(out=yw[:], in0=acw[0:64, :], in1=coef[64:128, :], op=ALU.mult), reads=[acw.name, "ns_coef"], writes=["ns_yw"])
                    P.op("pool", lambda e: e.tensor_tensor(out=ys[:], in0=ys[:], in1=yw[:], op=ALU.add), reads=["ns_ys", "ns_yw"], writes=["ns_ys"])
                    yo = yo2[tb % 2]
                    P.op("pool", lambda e, yo=yo, ycm=ycm: e.tensor_tensor(out=yo[:], in0=ys[:], in1=ycm[:], op=ALU.add), reads=["ns_ys", ycm.name], writes=[yo.name])
                    P.dma("sp", lambda e, yo=yo, h=h, tsl=tsl: e.dma_start(out=k.yT[512 + h * 64:512 + (h + 1) * 64, tsl], in_=yo[:]), reads=[yo.name], writes=[("yT", 4 + h // 2)])
        P.barrier()


def _consts():
    c = {}
    c["c_ident"] = np.eye(128, dtype=np.float32).astype(ml_dtypes.bfloat16)
    c["c_identf"] = np.eye(128, dtype=np.float32)
    sl = np.arange(128)[:, None]
    tl = np.arange(512)[None, :]
    cm = np.zeros((128, 8, 512), np.float32)
    for o in range(4):
        cm[:, o, :] = (sl + 128 * o <= tl)
        cm[:, 4 + o, :] = 1.0 - cm[:, o, :]
    c["c_cmask"] = cm.reshape(128, 8 * 512).astype(ml_dtypes.bfloat16)
    half = 16
    freq = (10000.0 ** (-np.arange(half, dtype=np.float32) / half)).astype(np.float32)
    fr = np.zeros((32, 4), np.float32)
    fr[:, 0] = np.concatenate([freq, freq])
    fr[:, 1] = np.concatenate([-np.ones(16), np.ones(16)])
    fr[:, 2] = np.concatenate([np.full(16, np.pi), np.full(16, -np.pi)])
    fr[:, 3] = -np.pi
    c["c_freq"] = fr
    nb = 255
    n = np.arange(256)
    t = np.arange(S)
    vis = ((16 * n[:, None] + 31) <= t[None, :]) & (n[:, None] < nb)
    c["c_cm"] = np.concatenate([vis[0:128], vis[128:256]], axis=1).astype(np.float32).astype(ml_dtypes.bfloat16)
    c_start = np.arange(nb) * 16
    s_start = np.arange(64) * 64
    ovl = ((c_start[:, None] < s_start[None, :] + 64) & (c_start[:, None] + 32 > s_start[None, :])).astype(np.float32)
    ovl = np.concatenate([ovl, np.zeros((1, 64), np.float32)], 0)
    c["c_ovl"] = np.concatenate([ovl[0:128], ovl[128:256]], axis=1).astype(ml_dtypes.bfloat16)
    cur = t // 64
    jb = np.arange(64)
    valid = (jb[None, :] <= cur[:, None])
    forced = (jb[None, :] == 0) | (jb[None, :] == cur[:, None]) | (jb[None, :] == cur[:, None] - 1)
    addc = np.where(valid, 1e4 * forced.astype(np.float32), -1.0).astype(np.float32)
    c["c_valid"] = valid.astype(np.float32).reshape(NT, 128, 64).transpose(1, 0, 2).reshape(128, NT * 64).copy()
    c["c_addc"] = addc.reshape(NT, 128, 64).transpose(1, 0, 2).reshape(128, NT * 64).copy()
    E = (jb[:, None] == (t[None, :] // 64)).astype(np.float32)
    c["c_E"] = E.astype(ml_dtypes.bfloat16)
    gs = np.zeros((24, 24, 128), np.float32)
    for i in range(24):
        gs[i, i, :] = 1.0
    c["c_gsel"] = gs.reshape(24, 24 * 128).astype(ml_dtypes.bfloat16)
    return c


def prep_weights(inp):
    f = lambda a: np.ascontiguousarray(np.asarray(a, dtype=np.float32))
    w_in = f(inp["w_in"])
    out = {}
    cols = []
    for (name, c0, w, kind) in FM:
        blk = np.zeros((DEPTH, D, 128), np.float32)
        if name == "krp":
            kr = w_in[:, :, 3488:3520]
            blk[:, :, 0:32] = np.concatenate([kr[:, :, 16:32], kr[:, :, 0:16]], axis=-1)
        else:
            blk[:, :, 0:w] = w_in[:, :, c0:c0 + w]
        cols.append(blk)
    out["w_fm"] = np.concatenate(cols, axis=-1)
    out["w_tm"] = np.concatenate([w_in[:, :, 512:1024], w_in[:, :, BKV + 384:BKV + 512], w_in[:, :, BKV + 640:BKV + 768]], axis=-1)
    g = f(inp["norm_gains"])
    out["gain_rows"] = g.reshape(DEPTH * 4, D).copy()
    out["gains"] = g.reshape(DEPTH * 4, 8, 128).transpose(2, 0, 1).reshape(128, DEPTH * 4 * 8).copy()
    mc = f(inp["m_conv"])
    out["mconv"] = mc.reshape(DEPTH, 4, 4, 128).transpose(0, 3, 2, 1).reshape(DEPTH, 128, 16).copy()
    gb = f(inp["m_gate_bias"])
    out["gbias"] = gb.reshape(DEPTH, 2, 4).transpose(0, 2, 1).copy()
    out["hnorm"] = f(inp["m_head_norm"]).transpose(0, 2, 1).copy()
    cp = f(inp["nsa_cmp_pos"])
    out["cpos"] = cp.transpose(0, 3, 1, 2).reshape(DEPTH, 64, 64).copy()
    w1 = f(inp["nsa_cmp_w1"])
    out["cw1"] = w1.reshape(DEPTH, 2, 32, 64, 128).transpose(0, 1, 3, 2, 4).reshape(DEPTH, 2, 64, 32 * 128).copy()
    out["cw2"] = f(inp["nsa_cmp_w2"])
    out["qn"] = f(inp["mla_q_norm"]).reshape(DEPTH, 3, 128).transpose(0, 2, 1).copy()
    out["kvn"] = f(inp["mla_kv_norm"]).reshape(DEPTH, 2, 128).transpose(0, 2, 1).copy()
    wq = f(inp["mla_w_uq"]).reshape(DEPTH, 384, 8, 96)
    rope = wq[..., 64:96]
    ropep = np.concatenate([rope[..., 16:32], rope[..., 0:16]], axis=-1)
    out["wuq"] = np.concatenate([rope, wq[..., 0:64], ropep], axis=-1).reshape(DEPTH, 384, 8 * 128).copy()
    wkv = f(inp["mla_w_ukv"]).reshape(DEPTH, 256, 8, 128)
    out["wuk"] = np.concatenate([np.zeros((DEPTH, 256, 8, 32), np.float32), wkv[..., 0:64]], axis=-1).reshape(DEPTH, 256, 8 * 96).copy()
    out["wuv"] = wkv[..., 64:128].reshape(DEPTH, 256, 512).copy()
    out["wbr"] = f(inp["w_branch"])
    out["wout"] = f(inp["w_out"])
    out["wg"] = f(inp["w_ffn_gate"])
    out["wu"] = f(inp["w_ffn_up"])
    out["wd"] = f(inp["w_ffn_down"])
    out.update(_consts())
    return out


_CACHE = {}


def kernel(**inputs):
    shared = prep_weights(inputs)
    x = np.ascontiguousarray(np.asarray(inputs["x"], dtype=np.float32))
    pos = np.ascontiguousarray(np.asarray(inputs["positions"], dtype=np.int32))
    if "nc" not in _CACHE:
        _CACHE["nc"] = build()
    nc = _CACHE["nc"]
    in_maps = []
    for c in range(NCORES):
        b = c % 4
        m = dict(shared)
        m["x"] = x[b]
        m["pos"] = pos[b]
        in_maps.append(m)
    res = run_bass_kernel_spmd(nc, in_maps, core_ids=list(range(NCORES)))
    return np.stack([np.asarray(res.results[b]["y"], dtype=np.float32) for b in range(4)], axis=0)
```

```python
import math
from contextlib import ExitStack
import numpy as np
import ml_dtypes
import concourse.bass as bass
import concourse.mybir as mybir
from concourse.bass_utils import run_bass_kernel_spmd

F32 = mybir.dt.float32
BF16 = mybir.dt.bfloat16
I32 = mybir.dt.int32
AF = mybir.ActivationFunctionType
ALU = mybir.AluOpType
AX = mybir.AxisListType

S = 4096
D = 1024
NT = 32
NTB = 8
DFF = 2816
NFC = 22
EPS = 1e-6
DEPTH = 2
NCORES = 8

ENGS = ("pe", "act", "dve", "pool", "sp")

FM = []
def _fm(name, c0, w, kind):
    FM.append((name, c0, w, kind))
_fm("aq0", 0, 128, "copy"); _fm("aq1", 128, 128, "copy")
_fm("ak0", 256, 128, "copy"); _fm("ak1", 384, 128, "copy")
for i in range(4): _fm("ao%d" % i, 1024 + 128 * i, 128, "sigmoid")
_fm("ai", 1536, 4, "ai"); _fm("af", 1540, 4, "af")
for i in range(4): _fm("bq%d" % i, 1544 + 128 * i, 128, "scale8")
BKV = 2056
_fm("kcr", BKV + 0, 128, "copy"); _fm("vcr", BKV + 128, 128, "copy")
_fm("kslc", BKV + 256, 128, "copy"); _fm("kwin", BKV + 512, 128, "copy")
_fm("bg", 2824, 24, "sigmoid")
for i in range(3): _fm("cq%d" % i, 2848 + 128 * i, 128, "copy")
for i in range(2): _fm("ckv%d" % i, 3232 + 128 * i, 128, "copy")
_fm("kr", 3488, 32, "copy"); _fm("krp", -1, 32, "copy")
for i in range(24): _fm("gt%d" % i, 3520 + 128 * i, 128, "sigmoid")
NFM = len(FM)
FMI = {f[0]: i for i, f in enumerate(FM)}


class Prog:
    def __init__(self, nc, n_dma_sems=32):
        self.nc = nc
        self.ops = {e: [] for e in ENGS}
        self.seq = {e: 0 for e in ENGS}
        self.last_w = {}
        self.readers = {}
        self.waited = {e: {} for e in ENGS}
        self.n_dma_sems = n_dma_sems
        self.dma_cnt = [0] * n_dma_sems
        self.dma_rr = 0
        self.n_ops = 0

    @staticmethod
    def _norm(rs):
        return [r.split("@")[0] if isinstance(r, str) else r for r in rs]

    def _deps(self, eng, reads, writes):
        deps = {}

        def add(k, v):
            if deps.get(k, 0) < v:
                deps[k] = v
        for r in reads:
            h = self.last_w.get(r)
            if h is not None:
                add(*h)
        for w in writes:
            h = self.last_w.get(w)
            if h is not None:
                add(*h)
            for k, v in self.readers.get(w, {}).items():
                add(k, v)
        out = []
        for k, v in deps.items():
            if k == eng and eng == "pe":
                continue
            if self.waited[eng].get(k, 0) >= v:
                continue
            self.waited[eng][k] = v
            out.append((k, v))
        return out

    def _commit(self, handle, reads, writes):
        k, v = handle
        for r in reads:
            d = self.readers.setdefault(r, {})
            if d.get(k, 0) < v:
                d[k] = v
        for w in writes:
            self.last_w[w] = handle
            self.readers[w] = {}

    def op(self, eng, fn, reads=(), writes=()):
        reads, writes = self._norm(reads), self._norm(writes)
        waits = self._deps(eng, reads, writes)
        self.seq[eng] += 1
        handle = (eng, self.seq[eng])
        self.ops[eng].append((waits, fn, ("eng", eng)))
        self._commit(handle, reads, writes)
        self.n_ops += 1

    def dma(self, eng, fn, reads=(), writes=()):
        s = self.dma_rr
        self.dma_rr = (self.dma_rr + 1) % self.n_dma_sems
        key = ("dma", s)
        reads, writes = self._norm(reads), self._norm(writes)
        waits = self._deps(eng, reads, writes)
        prev = self.dma_cnt[s] * 16
        if prev and self.waited[eng].get(key, 0) < prev:
            self.waited[eng][key] = prev
            waits.append((key, prev))
        self.dma_cnt[s] += 1
        handle = (key, self.dma_cnt[s] * 16)
        self.ops[eng].append((waits, fn, ("dma", s)))
        self._commit(handle, reads, writes)
        self.n_ops += 1

    def barrier(self):
        cur = [(e, self.seq[e]) for e in ENGS if self.seq[e] > 0]
        cur += [(("dma", s), c * 16) for s, c in enumerate(self.dma_cnt) if c > 0]
        for e in ENGS:
            waits = []
            for k, v in cur:
                if k == e:
                    continue
                if self.waited[e].get(k, 0) >= v:
                    continue
                self.waited[e][k] = v
                waits.append((k, v))
            if waits:
                self.ops[e].append((waits, None, None))

    def emit(self):
        nc = self.nc
        with ExitStack() as st:
            W = 16000
            esem = {e: [st.enter_context(nc.semaphore("s_%s%d" % (e, i))) for i in range(self.seq[e] // W + 1)] for e in ENGS}
            dsem = [st.enter_context(nc.semaphore("d%d" % i)) for i in range(self.n_dma_sems)]
            block = st.enter_context(nc.Block())

            def run(eo, lst):
                myseq = 0
                for waits, fn, inc in lst:
                    for k, v in waits:
                        if isinstance(k, tuple):
                            eo.wait_ge(dsem[k[1]], v)
                        else:
                            eo.wait_ge(esem[k][(v - 1) // W], (v - 1) % W + 1)
                    if fn is None:
                        continue
                    ins = fn(eo)
                    if inc[0] == "eng":
                        myseq += 1
                        ins.then_inc(esem[inc[1]][(myseq - 1) // W], 1)
                    else:
                        ins.then_inc(dsem[inc[1]], 16)

            @block.tensor
            def _(e):
                run(e, self.ops["pe"])

            @block.scalar
            def _(e):
                run(e, self.ops["act"])

            @block.vector
            def _(e):
                run(e, self.ops["dve"])

            @block.gpsimd
            def _(e):
                run(e, self.ops["pool"])

            @block.sync
            def _(e):
                run(e, self.ops["sp"])


class K:
    pass


def build(nlayers=DEPTH, taps=(), stop_after=None):
    nc = bass.Bass("TRN2", target_bir_lowering=False)
    k = K()
    k.nc = nc
    k.taps = set(taps)
    P = Prog(nc)
    k.P = P

    def din(name, shape, dt):
        return nc.dram_tensor(name, list(shape), dt, kind="ExternalInput").ap()

    def dscr(name, shape, dt):
        kind = "ExternalOutput" if name in k.taps else "Internal"
        return nc.dram_tensor(name, list(shape), dt, kind=kind).ap()

    k.x = din("x", [S, D], F32)
    k.pos = din("pos", [S], I32)
    k.gains = din("gains", [128, DEPTH * 4 * 8], F32)
    k.gain_rows = din("gain_rows", [DEPTH * 4, D], F32)
    k.w_fm = din("w_fm", [DEPTH, D, NFM * 128], F32)
    k.w_tm = din("w_tm", [DEPTH, D, 768], F32)
    k.mconv = din("mconv", [DEPTH, 128, 16], F32)
    k.gbias = din("gbias", [DEPTH, 4, 2], F32)
    k.hnorm = din("hnorm", [DEPTH, 128, 4], F32)
    k.cpos = din("cpos", [DEPTH, 64, 64], F32)
    k.cw1 = din("cw1", [DEPTH, 2, 64, 32 * 128], F32)
    k.cw2 = din("cw2", [DEPTH, 2, 128, 64], F32)
    k.qn = din("qn", [DEPTH, 128, 3], F32)
    k.kvn = din("kvn", [DEPTH, 128, 2], F32)
    k.wuq = din("wuq", [DEPTH, 384, 8 * 128], F32)
    k.wuk = din("wuk", [DEPTH, 256, 8 * 96], F32)
    k.wuv = din("wuv", [DEPTH, 256, 512], F32)
    k.wbr = din("wbr", [DEPTH, 3, 512, D], F32)
    k.wout = din("wout", [DEPTH, D, D], F32)
    k.wg = din("wg", [DEPTH, D, DFF], F32)
    k.wu = din("wu", [DEPTH, D, DFF], F32)
    k.wd = din("wd", [DEPTH, DFF, D], F32)
    k.c_ident = din("c_ident", [128, 128], BF16)
    k.c_identf = din("c_identf", [128, 128], F32)
    k.c_cmask = din("c_cmask", [128, 8 * 512], BF16)
    k.c_freq = din("c_freq", [32, 4], F32)
    k.c_cm = din("c_cm", [128, 2 * S], BF16)
    k.c_ovl = din("c_ovl", [128, 2 * 64], BF16)
    k.c_valid = din("c_valid", [128, NT * 64], F32)
    k.c_addc = din("c_addc", [128, NT * 64], F32)
    k.c_E = din("c_E", [64, S], BF16)
    k.c_gsel = din("c_gsel", [24, 24 * 128], BF16)
    k.y = nc.dram_tensor("y", [S, D], F32, kind="ExternalOutput").ap()

    k.zT = dscr("zT", [NFM * 128, S], BF16)
    k.vtm = dscr("vtm", [S, 768], BF16)
    k.Bd = dscr("Bd", [4, S], F32)
    k.csd = dscr("csd", [4, S], F32)
    k.yT = dscr("yT", [1536, S], BF16)
    k.qaT = dscr("qaT", [8 * 96, S], BF16)
    k.kaT = dscr("kaT", [8 * 96, S], BF16)
    k.vml = dscr("vml", [S, 512], BF16)
    k.ycmp = dscr("ycmp", [512, S], BF16)
    k.biasT = dscr("biasT", [128, S], BF16)
    k.x1 = dscr("x1", [S, D], F32)
    k.x2 = dscr("x2", [S, D], F32)
    k.kcmpd = dscr("kcmpd", [128, 256], BF16)
    k.impd = dscr("impd", [128, S], F32)
    k.ifd = dscr("ifd", [2, 4, S], F32)
    k.ropeT = dscr("ropeT", [4, 32, S], F32)

    with ExitStack() as gst:
        k.uid = 0

        def sb(name, shape, dt, st=gst):
            k.uid += 1
            return st.enter_context(nc.sbuf_tensor("%s@%d" % (name, k.uid), list(shape), dt))
        k.sb = sb
        k.ps = [gst.enter_context(nc.psum_tensor("ps%d" % i, [128, 512], F32)) for i in range(8)]
        k.psb = k.ps[7][:, :].bitcast(BF16)
        k.ident = sb("ident", [128, 128], BF16)
        k.ones = sb("ones", [128, 128], BF16)
        k.cmask = sb("cmask", [128, 8, 512], BF16)
        k.gn = sb("gn", [128, DEPTH * 4, 8], F32)
        k.epsT = sb("epsT", [128, 1], F32)
        k.zeroT = sb("zeroT", [128, 1], F32)
        P.dma("sp", lambda e: e.dma_start(out=k.ident[:], in_=k.c_ident[:, :]), writes=["ident"])
        P.dma("sp", lambda e: e.dma_start(out=k.cmask[:].rearrange("p a b -> p (a b)"), in_=k.c_cmask[:, :]), writes=["cmask"])
        P.dma("sp", lambda e: e.dma_start(out=k.gn[:].rearrange("p a b -> p (a b)"), in_=k.gains[:, :]), writes=["gn"])
        P.op("pool", lambda e: e.memset(k.ones[:], 1.0), writes=["ones"])
        P.op("pool", lambda e: e.memset(k.epsT[:], EPS), writes=["epsT"])
        P.op("pool", lambda e: e.memset(k.zeroT[:], 0.0), writes=["zeroT"])
        P.barrier()

        xin = k.x
        for l in range(nlayers):
            last = (l == nlayers - 1)
            phase_proj(k, l, xin)
            if stop_after == "proj":
                break
            phase_mlstm(k, l)
            if stop_after == "mlstm":
                break
            phase_mla(k, l)
            if stop_after == "mla":
                break
            phase_nsa(k, l)
            if stop_after == "nsa":
                break
            phase_merge(k, l, xin, k.x1)
            if stop_after == "merge":
                break
            xo = k.y if last else k.x2
            phase_ffn(k, l, k.x1, xo)
            xin = k.x2
        P.barrier()
        P.emit()
    return nc


def norm_tile_T(k, st_tag, xt, ss, rs, xn, n):
    P = k.P
    P.op("pool", lambda e: e.memset(ss[:], 0.0), writes=[ss.name])
    P.op("act", lambda e: e.activation(out=xn[:], in_=xt[:], func=AF.Square, accum_out=ss[:]),
         reads=[xt.name, ss.name], writes=[xn.name, ss.name])
    P.op("act", lambda e: e.activation(out=rs[:], in_=ss[:], func=AF.Sqrt, bias=k.epsT[:], scale=1.0 / D),
         reads=[ss.name, "epsT"], writes=[rs.name])
    P.op("dve", lambda e: e.reciprocal(out=rs[:], in_=rs[:]), reads=[rs.name], writes=[rs.name])
    P.op("dve", lambda e: e.tensor_scalar(out=xn[:], in0=xt[:], scalar1=rs[:, 0:1], scalar2=None, op0=ALU.mult),
         reads=[xt.name, rs.name, xn.name], writes=[xn.name])


def load_cast(k, st, name, dram_ap, rows_p, nk, ncols, dst, dst_res, stage, q="sp", eng="rot", sw=2048):
    P = k.P
    per = max(1, sw // ncols)
    kc = 0
    i = 0
    while kc < nk:
        m = min(per, nk - kc)
        stg = stage[k.stage_i % len(stage)]
        k.stage_i += 1
        src = dram_ap[kc * 128:(kc + m) * 128, :].rearrange("(a p) c -> p a c", p=128)
        sv = stg[:rows_p, 0:m * ncols].rearrange("p (a c) -> p a c", a=m)
        P.dma(q, lambda e, sv=sv, src=src: e.dma_start(out=sv, in_=src), writes=[stg.name])
        dv = dst[:rows_p, kc:kc + m, 0:ncols]
        if eng == "rot":
            eng_i = ("dve", "act", "dve", "pool")[k.stage_i % 4]
        else:
            eng_i = eng
        if eng_i == "act":
            P.op("act", lambda e, dv=dv, sv=sv: e.copy(out=dv, in_=sv), reads=[stg.name], writes=[dst_res])
        else:
            P.op(eng_i, lambda e, dv=dv, sv=sv: e.tensor_copy(out=dv, in_=sv), reads=[stg.name], writes=[dst_res])
        kc += m
        i += 1


def make_hT(k, st, xin, l, gi, hT, tiles=range(NT), tok_off=0):
    P = k.P
    nc = k.nc
    xt2 = [k.sb("mh_x%d" % i, [128, D], F32, st) for i in range(2)]
    xn2 = [k.sb("mh_n%d" % i, [128, D], BF16, st) for i in range(2)]
    ss2 = [k.sb("mh_s%d" % i, [128, 1], F32, st) for i in range(2)]
    rs2 = [k.sb("mh_r%d" % i, [128, 1], F32, st) for i in range(2)]
    gb = k.gn[:, l * 4 + gi, :].unsqueeze(2).broadcast_to([128, 8, 128])
    for j, n in enumerate(tiles):
        xt, xn, ss, rs = xt2[j % 2], xn2[j % 2], ss2[j % 2], rs2[j % 2]
        P.dma("sp", lambda e, xt=xt, n=n: e.dma_start(out=xt[:], in_=xin[n * 128:(n + 1) * 128, :]), reads=[("dram", xin.name)], writes=[xt.name])
        norm_tile_T(k, "mh", xt, ss, rs, xn, n)
        for kc in range(8):
            P.op("pe", lambda e, kc=kc, xn=xn: e.transpose(out=k.psb[:, kc * 128:(kc + 1) * 128], in_=xn[:, kc * 128:(kc + 1) * 128], identity=k.ident[:]),
                 reads=[xn.name, "ident"], writes=["ps7"])
        t0 = (n - tok_off) * 128
        P.op("dve", lambda e, t0=t0: e.tensor_tensor(out=hT[:, :, t0:t0 + 128], in0=k.psb.rearrange("p (a t) -> p a t", a=8), in1=gb, op=ALU.mult),
             reads=["ps7", "gn"], writes=[hT.name])


def phase_proj(k, l, xin):
    P = k.P
    with ExitStack() as st:
        hT = k.sb("hT", [128, 8, S], BF16, st)
        make_hT(k, st, xin, l, 0, hT)
        stage = [k.sb("pj_stg%d" % i, [128, 2048], F32, st) for i in range(2)]
        k.stage_i = 0
        wb2 = [k.sb("pj_wb%d" % i, [128, 8, 512], BF16, st) for i in range(2)]
        ev = [k.sb("pj_ev%d" % i, [128, 512], BF16, st) for i in range(4)]
        gbt = k.sb("pj_gb", [4, 2], F32, st)
        evf = [k.sb("pj_evf%d" % i, [4, 512], F32, st) for i in range(2)]
        P.dma("sp", lambda e: e.dma_start(out=gbt[:], in_=k.gbias[l]), writes=["pj_gb"])
        evi = 0
        psi = 0
        def load_group(g0):
            gw = min(4, NFM - g0)
            wb = wb2[(g0 // 4) % 2]
            load_cast(k, st, "wfm", k.w_fm[l][:, g0 * 128:(g0 + gw) * 128], 128, 8, gw * 128, wb, wb.name, stage, eng="pool")
        load_group(0)
        for g0 in range(0, NFM, 4):
            gw = min(4, NFM - g0)
            wb = wb2[(g0 // 4) % 2]
            if g0 + 4 < NFM:
                load_group(g0 + 4)
            for ci in range(g0, g0 + gw):
                name, c0, w, kind = FM[ci]
                off = (ci - g0) * 128
                for tb in range(NTB):
                    ps = k.ps[psi % 2]
                    psi += 1
                    for kc in range(8):
                        P.op("pe", lambda e, ps=ps, kc=kc, wb=wb, off=off, w=w, tb=tb: e.matmul(ps[0:w, :], lhsT=wb[:, kc, off:off + w], rhs=hT[:, kc, tb * 512:(tb + 1) * 512], start=(kc == 0), stop=(kc == 7)),
                             reads=[wb.name, "hT"], writes=[ps.name])
                    if kind in ("ai", "af"):
                        j = 0 if kind == "ai" else 1
                        dst = evf[evi % 2]
                        evi += 1
                        P.op("dve", lambda e, ps=ps, dst=dst, j=j: e.tensor_scalar(out=dst[:], in0=ps[0:4, :], scalar1=gbt[:, j:j + 1], scalar2=None, op0=ALU.add),
                             reads=[ps.name, "pj_gb"], writes=[dst.name])
                        P.dma("sp", lambda e, dst=dst, j=j, tb=tb: e.dma_start(out=k.ifd[j][:, tb * 512:(tb + 1) * 512], in_=dst[:]), reads=[dst.name], writes=["ifd"])
                        continue
                    e_t = ev[evi % 4]
                    evi += 1
                    if kind == "sigmoid":
                        P.op("act", lambda e, ps=ps, e_t=e_t, w=w: e.activation(out=e_t[0:w, :], in_=ps[0:w, :], func=AF.Sigmoid), reads=[ps.name], writes=[e_t.name])
                    elif kind == "scale8":
                        P.op("dve", lambda e, ps=ps, e_t=e_t, w=w: e.tensor_scalar(out=e_t[0:w, :], in0=ps[0:w, :], scalar1=0.125, scalar2=None, op0=ALU.mult), reads=[ps.name], writes=[e_t.name])
                    else:
                        if evi % 2:
                            P.op("dve", lambda e, ps=ps, e_t=e_t, w=w: e.tensor_copy(out=e_t[0:w, :], in_=ps[0:w, :]), reads=[ps.name], writes=[e_t.name])
                        else:
                            P.op("act", lambda e, ps=ps, e_t=e_t, w=w: e.copy(out=e_t[0:w, :], in_=ps[0:w, :]), reads=[ps.name], writes=[e_t.name])
                    P.dma("sp", lambda e, e_t=e_t, ci=ci, w=w, tb=tb: e.dma_start(out=k.zT[ci * 128:ci * 128 + w, tb * 512:(tb + 1) * 512], in_=e_t[0:w, :]),
                          reads=[e_t.name], writes=[("zT", ci)])
        for (c0, cw) in ((0, 512), (512, 256)):
            wb = wb2[0]
            load_cast(k, st, "wtm", k.w_tm[l][:, c0:c0 + cw], 128, 8, cw, wb, wb.name, stage, eng="pool")
            for n in range(NT):
                ps = k.ps[psi % 2]
                psi += 1
                for kc in range(8):
                    P.op("pe", lambda e, ps=ps, kc=kc, n=n, cw=cw: e.matmul(ps[:, 0:cw], lhsT=hT[:, kc, n * 128:(n + 1) * 128], rhs=wb[:, kc, 0:cw], start=(kc == 0), stop=(kc == 7)),
                         reads=[wb.name, "hT"], writes=[ps.name])
                e_t = ev[evi % 4]
                evi += 1
                P.op("dve", lambda e, ps=ps, e_t=e_t, cw=cw: e.tensor_copy(out=e_t[:, 0:cw], in_=ps[:, 0:cw]), reads=[ps.name], writes=[e_t.name])
                P.dma("sp", lambda e, e_t=e_t, n=n, c0=c0, cw=cw: e.dma_start(out=k.vtm[n * 128:(n + 1) * 128, c0:c0 + cw], in_=e_t[:, 0:cw]),
                      reads=[e_t.name], writes=[("vtm", c0)])
        k.P.barrier()


def attn_tb(k, steps, qk, pf, pv, nb=3):
    n = len(steps)
    sps = [k.ps[i] for i in range(nb)]
    LA = nb - 1
    for j in range(min(LA, n)):
        qk(j, steps[j], sps[j % nb])
    for i in range(n):
        if i + LA < n:
            qk(i + LA, steps[i + LA], sps[(i + LA) % nb])
        pt = k.pts[k.pt_i % 4]
        k.pt_i += 1
        pf(i, steps[i], sps[i % nb], pt)
        pv(i, steps[i], pt, i == 0, i == n - 1)


def mm(k, out, lhsT, rhs, start, stop, reads, writes):
    k.P.op("pe", lambda e: e.matmul(out, lhsT=lhsT, rhs=rhs, start=start, stop=stop), reads=reads, writes=writes)


def causal_steps(tb):
    return [(st, (st - 4 * tb) if st >= 4 * tb else None) for st in range(4 * tb + 4)]


def attn_scratch(k, st):
    k.pts = [k.sb("pt%d" % i, [128, 512], BF16, st) for i in range(4)]
    k.pt_i = 0


def std_pf(k, maskres="cmask"):
    P = k.P

    def pf(i, step, ps, pt):
        s_t, mo = step
        P.op("act", lambda e: e.activation(out=pt[:], in_=ps[:], func=AF.Exp), reads=[ps.name], writes=[pt.name])
        if mo is not None:
            P.op("dve", lambda e: e.tensor_tensor(out=pt[:], in0=pt[:], in1=k.cmask[:, mo, :], op=ALU.mult), reads=[pt.name, maskres], writes=[pt.name])
    return pf


def phase_mlstm(k, l):
    P = k.P
    with ExitStack() as st:
        mcv = k.sb("ml_mcv", [128, 16], F32, st)
        P.dma("sp", lambda e: e.dma_start(out=mcv[:], in_=k.mconv[l]), writes=["ml_mcv"])
        with ExitStack() as st2:
            xp = k.sb("ml_xp", [128, 3 + S], BF16, st2)
            acc = k.sb("ml_acc", [128, S], F32, st2)
            ob = k.sb("ml_ob", [128, S], BF16, st2)
            P.op("pool", lambda e: e.memset(xp[:, 0:3], 0.0), writes=["ml_xp"])
            for c in range(4):
                ci = c
                P.dma("sp", lambda e, ci=ci: e.dma_start(out=xp[:, 3:3 + S], in_=k.zT[ci * 128:(ci + 1) * 128, :]), reads=[("zT", ci)], writes=["ml_xp"])
                P.op("dve", lambda e, c=c: e.tensor_scalar(out=acc[:], in0=xp[:, 3:3 + S], scalar1=mcv[:, c * 4 + 3:c * 4 + 4], scalar2=None, op0=ALU.mult), reads=["ml_xp", "ml_mcv"], writes=["ml_acc"])
                for tap in range(3):
                    P.op("dve", lambda e, c=c, tap=tap: e.scalar_tensor_tensor(out=acc[:], in0=xp[:, tap:tap + S], scalar=mcv[:, c * 4 + tap:c * 4 + tap + 1], in1=acc[:], op0=ALU.mult, op1=ALU.add),
                         reads=["ml_xp", "ml_mcv", "ml_acc"], writes=["ml_acc"])
                if c < 2:
                    P.op("act", lambda e: e.activation(out=acc[:], in_=acc[:], func=AF.Silu), reads=["ml_acc"], writes=["ml_acc"])
                    P.op("dve", lambda e: e.tensor_scalar(out=ob[:], in0=acc[:], scalar1=0.125, scalar2=None, op0=ALU.mult), reads=["ml_acc"], writes=["ml_ob"])
                else:
                    P.op("act", lambda e: e.activation(out=ob[:], in_=acc[:], func=AF.Silu), reads=["ml_acc"], writes=["ml_ob"])
                P.dma("sp", lambda e, ci=ci: e.dma_start(out=k.zT[ci * 128:(ci + 1) * 128, :], in_=ob[:]), reads=["ml_ob"], writes=[("zT", ci)])
            t1 = k.sb("ml_t1", [4, S], F32, st2)
            cum = k.sb("ml_cum", [4, S], F32, st2)
            on4 = k.sb("ml_on4", [4, S], F32, st2)
            oneT = k.sb("ml_one", [4, 1], F32, st2)
            li = k.sb("ml_li", [4, S], F32, st2)
            P.op("pool", lambda e: e.memset(oneT[:], 1.0), writes=["ml_one"])
            P.dma("sp", lambda e: e.dma_start(out=cum[:], in_=k.ifd[1]), reads=["ifd"], writes=["ml_cum"])
            P.dma("sp", lambda e: e.dma_start(out=li[:], in_=k.ifd[0]), reads=["ifd"], writes=["ml_li"])
            P.op("pool", lambda e: e.memset(on4[:], 1.0), writes=["ml_on4"])
            P.op("act", lambda e: e.activation(out=t1[:], in_=cum[:], func=AF.Exp, scale=-1.0), reads=["ml_cum"], writes=["ml_t1"])
            P.op("act", lambda e: e.activation(out=t1[:], in_=t1[:], func=AF.Ln, bias=oneT[:], scale=1.0), reads=["ml_t1", "ml_one"], writes=["ml_t1"])
            P.op("dve", lambda e: e.tensor_tensor_scan(out=cum[:], data0=on4[:], data1=t1[:], initial=0.0, op0=ALU.mult, op1=ALU.add), reads=["ml_on4", "ml_t1"], writes=["ml_cum"])
            P.op("dve", lambda e: e.tensor_tensor(out=t1[:], in0=li[:], in1=cum[:], op=ALU.add), reads=["ml_li", "ml_cum", "ml_t1"], writes=["ml_t1"])
            P.op("dve", lambda e: e.tensor_scalar(out=cum[:], in0=cum[:], scalar1=-1.0, scalar2=None, op0=ALU.mult), reads=["ml_cum"], writes=["ml_cum"])
            P.dma("sp", lambda e: e.dma_start(out=k.Bd[:, :], in_=cum[:]), reads=["ml_cum"], writes=["Bd"])
            P.dma("sp", lambda e: e.dma_start(out=k.csd[:, :], in_=t1[:]), reads=["ml_t1"], writes=["csd"])
            P.barrier()
        attn_scratch(k, st)
        cs_tm = k.sb("ml_cs", [128, 4, NT], F32, st)
        hn_g = k.sb("ml_hn", [128, 4], F32, st)
        P.dma("sp", lambda e: e.dma_start(out=hn_g[:], in_=k.hnorm[l]), writes=["ml_hn"])
        for h in range(4):
            P.dma("sp", lambda e, h=h: e.dma_start(out=cs_tm[:, h, :], in_=k.csd[h].rearrange("(n p) -> p n", p=128), allow_slow_non_contiguous=True), reads=["csd"], writes=["ml_cs"])
        KT2 = [k.sb("ml_K%d" % i, [64, S], BF16, st) for i in range(2)]
        QT2 = [k.sb("ml_Q%d" % i, [64, S], BF16, st) for i in range(2)]
        V2 = [k.sb("ml_V%d" % i, [128, NT, 128], BF16, st) for i in range(2)]
        Br2 = [k.sb("ml_B%d" % i, [128, S], F32, st) for i in range(2)]
        Dt2 = [k.sb("ml_D%d" % i, [128, 512], F32, st) for i in range(4)]
        ao2 = [k.sb("ml_ao%d" % i, [128, 512], BF16, st) for i in range(2)]
        hn = k.sb("ml_h", [128, 512], F32, st)
        rr = k.sb("ml_r", [128, 512], F32, st)
        sq = k.sb("ml_sq", [128, 512], BF16, st)
        yo2 = [k.sb("ml_yo%d" % i, [128, 512], BF16, st) for i in range(2)]
        di = [0]
        Cf = k.sb("ml_Cf", [64, 128], F32, st)
        Nf = k.sb("ml_Nf", [64, 128], F32, st)
        Cb2 = [k.sb("ml_Cb%d" % i, [64, 128], BF16, st) for i in range(2)]
        Nb2 = [k.sb("ml_Nb%d" % i, [64, 128], BF16, st) for i in range(2)]
        negE = k.sb("ml_negE", [128, NTB], F32, st)
        gcol = k.sb("ml_gcol", [128, 1], F32, st)
        dec = k.sb("ml_dec", [64, 512], F32, st)
        qd2 = [k.sb("ml_qd%d" % i, [64, 512], BF16, st) for i in range(2)]
        wcol = [k.sb("ml_w%d" % i, [128, 1], F32, st) for i in range(4)]
        kw = [k.sb("ml_kw%d" % i, [128, 64], BF16, st) for i in range(4)]
        psT = k.ps[2][:, :].bitcast(BF16)
        TS = [(Cf, Nf, Cb2, Nb2, negE, gcol, dec, qd2, wcol, kw, hn, rr, sq)]
        TS.append((k.sb("ml_CfB", [64, 128], F32, st), k.sb("ml_NfB", [64, 128], F32, st),
                   [k.sb("ml_CbB%d" % i, [64, 128], BF16, st) for i in range(2)],
                   [k.sb("ml_NbB%d" % i, [64, 128], BF16, st) for i in range(2)],
                   k.sb("ml_negEB", [128, NTB], F32, st), k.sb("ml_gcolB", [128, 1], F32, st),
                   k.sb("ml_decB", [64, 512], F32, st),
                   [k.sb("ml_qdB%d" % i, [64, 512], BF16, st) for i in range(2)],
                   [k.sb("ml_wB%d" % i, [128, 1], F32, st) for i in range(4)],
                   [k.sb("ml_kwB%d" % i, [128, 64], BF16, st) for i in range(4)],
                   k.sb("ml_hB", [128, 512], F32, st), k.sb("ml_rB", [128, 512], F32, st), k.sb("ml_sqB", [128, 512], BF16, st)))

        def load_head(h):
            KT, QT, V, Br = KT2[h % 2], QT2[h % 2], V2[h % 2], Br2[h % 2]
            cq, ck = h // 2, 2 + h // 2
            r0 = (h % 2) * 64
            P.dma("sp", lambda e, KT=KT, ck=ck, r0=r0: e.dma_start(out=KT[:], in_=k.zT[ck * 128 + r0:ck * 128 + r0 + 64, :]), reads=[("zT", ck)], writes=[KT.name])
            P.dma("sp", lambda e, QT=QT, cq=cq, r0=r0: e.dma_start(out=QT[:], in_=k.zT[cq * 128 + r0:cq * 128 + r0 + 64, :]), reads=[("zT", cq)], writes=[QT.name])
            P.dma("sp", lambda e, V=V, h=h: e.dma_start(out=V[:], in_=k.vtm[:, h * 128:(h + 1) * 128].rearrange("(n p) c -> p n c", p=128)), reads=[("vtm", 0)], writes=[V.name])
            P.dma("sp", lambda e, Br=Br, h=h: e.dma_start(out=Br[:], in_=k.Bd[h].partition_broadcast(128)), reads=["Bd"], writes=[Br.name])
        def block(h, tb):
            KT, QT, V, Br = KT2[h % 2], QT2[h % 2], V2[h % 2], Br2[h % 2]
            Cf, Nf, Cb2, Nb2, negE, gcol, dec, qd2, wcol, kw, hn, rr, sq = TS[h % 2]
            if True:
                if tb == 0:
                    P.op("dve", lambda e: e.tensor_scalar(out=negE[:], in0=Br[:, 511::512], scalar1=-1.0, scalar2=None, op0=ALU.mult), reads=[Br.name], writes=[negE.name])
                tsl = slice(tb * 512, (tb + 1) * 512)
                num, den = (k.ps[3], k.ps[4]) if h % 2 == 0 else (k.ps[5], k.ps[6])
                ao = ao2[h % 2]
                P.dma("sp", lambda e, ao=ao, h=h, tsl=tsl: e.dma_start(out=ao[:], in_=k.zT[(4 + h) * 128:(5 + h) * 128, tsl]), reads=[("zT", 4 + h)], writes=[ao.name])
                Cb, Nb = Cb2[tb % 2], Nb2[tb % 2]
                if tb >= 1:
                    qd = qd2[tb % 2]
                    P.op("act", lambda e, Br=Br, tsl=tsl, tb=tb: e.activation(out=dec[:], in_=Br[0:64, tsl], func=AF.Exp, bias=negE[0:64, tb - 1:tb], scale=1.0), reads=[Br.name, negE.name], writes=[dec.name])
                    P.op("dve", lambda e, QT=QT, tsl=tsl, qd=qd: e.tensor_tensor(out=qd[:], in0=QT[:, tsl], in1=dec[:], op=ALU.mult), reads=[QT.name, dec.name], writes=[qd.name])
                    mm(k, num[:], Cb[:], qd[:], True, False, [Cb.name, qd.name], [num.name])
                    mm(k, den[:], Nb[:], qd[:], True, False, [Nb.name, qd.name], [den.name])

                def qk(i, step, ps, KT=KT, QT=QT, tsl=tsl):
                    s_t = step[0]
                    mm(k, ps[:], KT[:, s_t * 128:(s_t + 1) * 128], QT[:, tsl], True, True, [KT.name, QT.name], [ps.name])

                def pf(i, step, ps, pt, Br=Br, tsl=tsl, h=h):
                    s_t, mo = step
                    Dt = Dt2[di[0] % 4]
                    di[0] += 1
                    P.op("act", lambda e: e.activation(out=Dt[:], in_=Br[:, tsl], func=AF.Exp, bias=cs_tm[:, h, s_t:s_t + 1], scale=1.0), reads=[Br.name, "ml_cs"], writes=[Dt.name])
                    P.op("dve", lambda e: e.tensor_tensor(out=pt[:], in0=ps[:], in1=Dt[:], op=ALU.mult), reads=[ps.name, Dt.name], writes=[pt.name])
                    if mo is not None:
                        P.op("pool" if mo % 2 else "dve", lambda e: e.tensor_tensor(out=pt[:], in0=pt[:], in1=k.cmask[:, mo, :], op=ALU.mult), reads=[pt.name, "cmask"], writes=[pt.name])

                def pv(i, step, pt, first, lastf, V=V, num=num, den=den, tb=tb):
                    s_t = step[0]
                    mm(k, num[:], V[:, s_t, :], pt[:], first and tb == 0, lastf, [V.name, pt.name], [num.name])
                    mm(k, den[:], k.ones[:], pt[:], first and tb == 0, lastf, ["ones", pt.name], [den.name])
                attn_tb(k, [(4 * tb + o, o) for o in range(4)], qk, pf, pv, nb=2)
                if tb < NTB - 1:
                    ecol = Br[:, tb * 512 + 511:tb * 512 + 512]
                    dC, dN = k.ps[2], k.ps[2]
                    for o in range(4):
                        s_t = 4 * tb + o
                        P.op("act", lambda e, o=o, s_t=s_t, ecol=ecol, h=h: e.activation(out=wcol[o][:], in_=cs_tm[:, h, s_t:s_t + 1], func=AF.Exp, bias=ecol, scale=1.0), reads=["ml_cs", Br.name], writes=[wcol[o].name])
                        P.op("pe", lambda e, o=o, s_t=s_t, KT=KT: e.transpose(out=psT[:, o * 64:(o + 1) * 64], in_=KT[:, s_t * 128:(s_t + 1) * 128], identity=k.ident[0:64, 0:64]), reads=[KT.name, "ident"], writes=["ps2"])
                        P.op("dve", lambda e, o=o: e.tensor_scalar(out=kw[o][:], in0=psT[:, o * 64:(o + 1) * 64], scalar1=wcol[o][:, 0:1], scalar2=None, op0=ALU.mult), reads=["ps2", wcol[o].name], writes=[kw[o].name])
                    for o in range(4):
                        mm(k, dC[0:64, 128:256], kw[o][:], V[:, 4 * tb + o, :], o == 0, o == 3, [kw[o].name, V.name], [dC.name])
                    for o in range(4):
                        mm(k, dN[0:64, 256:384], kw[o][:], k.ones[:], o == 0, o == 3, [kw[o].name, "ones"], [dN.name])
                    Cbn, Nbn = Cb2[(tb + 1) % 2], Nb2[(tb + 1) % 2]
                    if tb == 0:
                        P.op("dve", lambda e, dC=dC: e.tensor_copy(out=Cf[:], in_=dC[0:64, 128:256]), reads=[dC.name], writes=[Cf.name])
                        P.op("dve", lambda e, dN=dN: e.tensor_copy(out=Nf[:], in_=dN[0:64, 256:384]), reads=[dN.name], writes=[Nf.name])
                    else:
                        P.op("act", lambda e, ecol=ecol, tb=tb: e.activation(out=gcol[0:64, :], in_=ecol[0:64, :], func=AF.Exp, bias=negE[0:64, tb - 1:tb], scale=1.0), reads=[Br.name, negE.name], writes=[gcol.name])
                        P.op("dve", lambda e, dC=dC: e.scalar_tensor_tensor(out=Cf[:], in0=Cf[:], scalar=gcol[0:64, 0:1], in1=dC[0:64, 128:256], op0=ALU.mult, op1=ALU.add), reads=[Cf.name, gcol.name, dC.name], writes=[Cf.name])
                        P.op("dve", lambda e, dN=dN: e.scalar_tensor_tensor(out=Nf[:], in0=Nf[:], scalar=gcol[0:64, 0:1], in1=dN[0:64, 256:384], op0=ALU.mult, op1=ALU.add), reads=[Nf.name, gcol.name, dN.name], writes=[Nf.name])
                    P.op("act", lambda e, Cbn=Cbn: e.copy(out=Cbn[:], in_=Cf[:]), reads=[Cf.name], writes=[Cbn.name])
                    P.op("act", lambda e, Nbn=Nbn: e.copy(out=Nbn[:], in_=Nf[:]), reads=[Nf.name], writes=[Nbn.name])
                P.op("dve", lambda e, den=den: e.tensor_scalar(out=rr[:], in0=den[:], scalar1=-1.0, scalar2=None, op0=ALU.mult), reads=[den.name], writes=[rr.name])
                P.op("dve", lambda e, den=den: e.tensor_tensor(out=rr[:], in0=rr[:], in1=den[:], op=ALU.max), reads=[den.name, rr.name], writes=[rr.name])
                P.op("dve", lambda e: e.tensor_scalar(out=rr[:], in0=rr[:], scalar1=1.0, scalar2=None, op0=ALU.max), reads=[rr.name], writes=[rr.name])
                P.op("act", lambda e: e.activation(out=rr[:], in_=rr[:], func=AF.Ln), reads=[rr.name], writes=[rr.name])
                P.op("act", lambda e: e.activation(out=rr[:], in_=rr[:], func=AF.Exp, scale=-1.0), reads=[rr.name], writes=[rr.name])
                P.op("dve", lambda e, num=num: e.tensor_tensor(out=hn[:], in0=num[:], in1=rr[:], op=ALU.mult), reads=[num.name, rr.name], writes=[hn.name])
                P.op("pool", lambda e: e.tensor_tensor(out=sq[:], in0=hn[:], in1=hn[:], op=ALU.mult), reads=[hn.name], writes=[sq.name])
                ss = k.ps[7]
                mm(k, ss[:], k.ones[:], sq[:], True, True, ["ones", sq.name], [ss.name])
                P.op("act", lambda e, ss=ss: e.activation(out=rr[:], in_=ss[:], func=AF.Ln, bias=k.epsT[:], scale=1.0 / 128), reads=[ss.name, "epsT"], writes=[rr.name])
                P.op("act", lambda e: e.activation(out=rr[:], in_=rr[:], func=AF.Exp, scale=-0.5), reads=[rr.name], writes=[rr.name])
                P.op("dve", lambda e, h=h: e.scalar_tensor_tensor(out=hn[:], in0=hn[:], scalar=hn_g[:, h:h + 1], in1=rr[:], op0=ALU.mult, op1=ALU.mult), reads=[hn.name, "ml_hn", rr.name], writes=[hn.name])
                yo = yo2[h % 2]
                P.op("pool", lambda e, yo=yo, ao=ao: e.tensor_tensor(out=yo[:], in0=hn[:], in1=ao[:], op=ALU.mult), reads=[hn.name, ao.name], writes=[yo.name])
                P.dma("sp", lambda e, yo=yo, h=h, tsl=tsl: e.dma_start(out=k.yT[h * 128:(h + 1) * 128, tsl], in_=yo[:]), reads=[yo.name], writes=[("yT", h)])
        for hp in (0, 2):
            load_head(hp)
            load_head(hp + 1)
            for tb in range(NTB):
                for h in (hp, hp + 1):
                    block(h, tb)
        P.barrier()


def rms_fm(k, src, nchunk, gain, dst, sq, rs, width):
    P = k.P
    P.op("act", lambda e: e.activation(out=sq[:, 0:nchunk, :], in_=src[:, 0:nchunk, :], func=AF.Square), reads=[src.name], writes=[sq.name])
    ss = k.ps[7]
    for a in range(nchunk):
        mm(k, ss[:], k.ones[:], sq[:, a, :], a == 0, a == nchunk - 1, ["ones", sq.name], [ss.name])
    P.op("act", lambda e: e.activation(out=rs[:], in_=ss[:], func=AF.Ln, bias=k.epsT[:], scale=1.0 / width), reads=[ss.name, "epsT"], writes=[rs.name])
    P.op("act", lambda e: e.activation(out=rs[:], in_=rs[:], func=AF.Exp, scale=-0.5), reads=[rs.name], writes=[rs.name])
    for a in range(nchunk):
        P.op("dve", lambda e, a=a: e.scalar_tensor_tensor(out=dst[:, a, :], in0=src[:, a, :], scalar=gain[:, a:a + 1], in1=rs[:], op0=ALU.mult, op1=ALU.mult),
             reads=[src.name, gain.name, rs.name], writes=[dst.name])


def phase_mla(k, l):
    P = k.P
    SC = 96.0 ** -0.5
    with ExitStack() as st:
        with ExitStack() as st2:
            C2 = k.sb("la_C2", [32, S], F32, st2)
            S2 = k.sb("la_S2", [32, S], F32, st2)
            C2q = k.sb("la_C2q", [32, S], F32, st2)
            S2q = k.sb("la_S2q", [32, S], F32, st2)
            fr = k.sb("la_fr", [32, 4], F32, st2)
            if l == 0:
                st3 = ExitStack()
                posi = k.sb("la_posi", [32, S], I32, st3)
                ang = k.sb("la_ang", [32, S], F32, st3)
                tmp = k.sb("la_tmp", [32, S], F32, st3)
                red = k.sb("la_red", [32, S], F32, st3)
                P.dma("sp", lambda e: e.dma_start(out=fr[:], in_=k.c_freq[:, :]), writes=["la_fr"])
                P.dma("sp", lambda e: e.dma_start(out=posi[:], in_=k.pos.partition_broadcast(32)), writes=["la_posi"])
                P.op("dve", lambda e: e.tensor_copy(out=ang[:], in_=posi[:]), reads=["la_posi"], writes=["la_ang"])
                P.op("dve", lambda e: e.tensor_scalar(out=ang[:], in0=ang[:], scalar1=fr[:, 0:1], scalar2=None, op0=ALU.mult), reads=["la_ang", "la_fr"], writes=["la_ang"])
                TWO_PI = 2 * math.pi

                def reduce_to_pi(src_off):
                    P.op("dve", lambda e: e.tensor_scalar(out=tmp[:], in0=ang[:], scalar1=float(src_off), scalar2=None, op0=ALU.add), reads=["la_ang", "la_S2", "la_C2"], writes=["la_tmp"])
                    P.op("dve", lambda e: e.tensor_scalar(out=red[:], in0=tmp[:], scalar1=1.0 / TWO_PI, scalar2=None, op0=ALU.mult), reads=["la_tmp"], writes=["la_red"])
                    P.op("dve", lambda e: e.tensor_copy(out=posi[:], in_=red[:]), reads=["la_red"], writes=["la_posi"])
                    P.op("dve", lambda e: e.tensor_copy(out=red[:], in_=posi[:]), reads=["la_posi"], writes=["la_red"])
                    P.op("dve", lambda e: e.tensor_scalar(out=red[:], in0=red[:], scalar1=-TWO_PI, scalar2=None, op0=ALU.mult), reads=["la_red"], writes=["la_red"])
                    P.op("dve", lambda e: e.tensor_tensor(out=tmp[:], in0=tmp[:], in1=red[:], op=ALU.add), reads=["la_tmp", "la_red"], writes=["la_tmp"])
                    for (cmp_op, thr, corr) in ((ALU.is_gt, math.pi, -TWO_PI), (ALU.is_lt, -math.pi, TWO_PI)):
                        P.op("dve", lambda e, cmp_op=cmp_op, thr=thr: e.tensor_scalar(out=red[:], in0=tmp[:], scalar1=float(thr), scalar2=None, op0=cmp_op), reads=["la_tmp"], writes=["la_red"])
                        P.op("dve", lambda e, corr=corr: e.tensor_scalar(out=red[:], in0=red[:], scalar1=float(corr), scalar2=None, op0=ALU.mult), reads=["la_red"], writes=["la_red"])
                        P.op("dve", lambda e: e.tensor_tensor(out=tmp[:], in0=tmp[:], in1=red[:], op=ALU.add), reads=["la_tmp", "la_red"], writes=["la_tmp"])
                reduce_to_pi(0.0)
                P.op("act", lambda e: e.activation(out=S2[:], in_=tmp[:], func=AF.Sin, scale=fr[:, 1:2]), reads=["la_tmp", "la_fr"], writes=["la_S2"])
                reduce_to_pi(0.5 * math.pi)
                P.op("act", lambda e: e.activation(out=C2[:], in_=tmp[:], func=AF.Sin), reads=["la_tmp"], writes=["la_C2"])
                P.op("pool", lambda e: e.tensor_scalar(out=C2q[:], in0=C2[:], scalar1=SC, scalar2=None, op0=ALU.mult), reads=["la_C2"], writes=["la_C2q"])
                P.op("pool", lambda e: e.tensor_scalar(out=S2q[:], in0=S2[:], scalar1=SC, scalar2=None, op0=ALU.mult), reads=["la_S2"], writes=["la_S2q"])
                P.barrier()
                st3.close()
                for ti, T in enumerate((C2, S2, C2q, S2q)):
                    P.dma("sp", lambda e, ti=ti, T=T: e.dma_start(out=k.ropeT[ti], in_=T[:]), reads=[T.name], writes=["ropeT"])
            else:
                for ti, T in enumerate((C2, S2, C2q, S2q)):
                    P.dma("sp", lambda e, ti=ti, T=T: e.dma_start(out=T[:], in_=k.ropeT[ti]), reads=["ropeT"], writes=[T.name])
            stage = [k.sb("la_stg%d" % i, [128, 2048], F32, st2) for i in range(2)]
            k.stage_i = 0
            Wq = k.sb("la_Wq", [128, 3, 1024], BF16, st2)
            Wk = k.sb("la_Wk", [128, 2, 768], BF16, st2)
            Wv = k.sb("la_Wv", [128, 2, 512], BF16, st2)
            load_cast(k, st2, "wuq", k.wuq[l], 128, 3, 1024, Wq, "la_Wq", stage)
            load_cast(k, st2, "wuk", k.wuk[l], 128, 2, 768, Wk, "la_Wk", stage)
            load_cast(k, st2, "wuv", k.wuv[l], 128, 2, 512, Wv, "la_Wv", stage)
            qng = k.sb("la_qn", [128, 3], F32, st2)
            kvng = k.sb("la_kvn", [128, 2], F32, st2)
            P.dma("sp", lambda e: e.dma_start(out=qng[:], in_=k.qn[l]), writes=["la_qn"])
            P.dma("sp", lambda e: e.dma_start(out=kvng[:], in_=k.kvn[l]), writes=["la_kvn"])
            cq = k.sb("la_cq", [128, 3, 512], BF16, st2)
            ckv = k.sb("la_ckv", [128, 2, 512], BF16, st2)
            cqn = k.sb("la_cqn", [128, 3, 512], BF16, st2)
            ckvn = k.sb("la_ckvn", [128, 2, 512], BF16, st2)
            sq = k.sb("la_sq", [128, 3, 512], BF16, st2)
            rs = k.sb("la_rs", [128, 512], F32, st2)
            krb = k.sb("la_krb", [32, 2, 512], BF16, st2)
            krr = k.sb("la_krr", [32, 512], F32, st2)
            t1 = k.sb("la_t1", [32, 512], F32, st2)
            t2 = k.sb("la_t2", [32, 512], F32, st2)
            qa2 = [k.sb("la_qa%d" % i, [96, 512], BF16, st2) for i in range(2)]
            ka2 = [k.sb("la_ka%d" % i, [96, 512], BF16, st2) for i in range(2)]
            vo2 = [k.sb("la_vo%d" % i, [128, 512], BF16, st2) for i in range(2)]
            c_cq, c_ckv, c_kr = FMI["cq0"], FMI["ckv0"], FMI["kr"]
            for tb in range(NTB):
                tsl = slice(tb * 512, (tb + 1) * 512)
                P.dma("sp", lambda e, tsl=tsl: e.dma_start(out=cq[:], in_=k.zT[c_cq * 128:(c_cq + 3) * 128, tsl].rearrange("(a p) t -> p a t", p=128)), reads=[("zT", c_cq + i) for i in range(3)], writes=["la_cq"])
                P.dma("sp", lambda e, tsl=tsl: e.dma_start(out=ckv[:], in_=k.zT[c_ckv * 128:(c_ckv + 2) * 128, tsl].rearrange("(a p) t -> p a t", p=128)), reads=[("zT", c_ckv + i) for i in range(2)], writes=["la_ckv"])
                P.dma("sp", lambda e, tsl=tsl: e.dma_start(out=krb[:], in_=k.zT[c_kr * 128:(c_kr + 2) * 128, tsl].rearrange("(a p) t -> p a t", p=128)[0:32]), reads=[("zT", c_kr), ("zT", c_kr + 1)], writes=["la_krb"])
                rms_fm(k, cq, 3, qng, cqn, sq, rs, 384)
                rms_fm(k, ckv, 2, kvng, ckvn, sq, rs, 256)
                P.op("dve", lambda e, tsl=tsl: e.tensor_tensor(out=krr[:], in0=krb[:, 0, :], in1=C2[:, tsl], op=ALU.mult), reads=["la_krb", "la_C2"], writes=["la_krr"])
                P.op("dve", lambda e, tsl=tsl: e.tensor_tensor(out=t1[:], in0=krb[:, 1, :], in1=S2[:, tsl], op=ALU.mult), reads=["la_krb", "la_S2"], writes=["la_t1"])
                P.op("dve", lambda e: e.tensor_tensor(out=krr[:], in0=krr[:], in1=t1[:], op=ALU.add), reads=["la_krr", "la_t1"], writes=["la_krr"])
                for h in range(8):
                    qa, ka = qa2[h % 2], ka2[h % 2]
                    psq, psr, psk = k.ps[2 + (h % 2)], k.ps[4], k.ps[5]
                    for a in range(3):
                        mm(k, psq[0:96, :], Wq[:, a, h * 128:h * 128 + 96], cqn[:, a, :], a == 0, a == 2, ["la_Wq", "la_cqn"], [psq.name])
                    for a in range(3):
                        mm(k, psr[0:32, :], Wq[:, a, h * 128 + 96:h * 128 + 128], cqn[:, a, :], a == 0, a == 2, ["la_Wq", "la_cqn"], [psr.name])
                    P.op("act", lambda e, qa=qa, psq=psq: e.activation(out=qa[32:64, :], in_=psq[32:64, :], func=AF.Copy, scale=SC), reads=[psq.name], writes=[qa.name])
                    P.op("act", lambda e, qa=qa, psq=psq: e.activation(out=qa[64:96, :], in_=psq[64:96, :], func=AF.Copy, scale=SC), reads=[psq.name], writes=[qa.name])
                    P.op("dve", lambda e, psq=psq, tsl=tsl: e.tensor_tensor(out=t1[:], in0=psq[0:32, :], in1=C2q[:, tsl], op=ALU.mult), reads=[psq.name, "la_C2q"], writes=["la_t1"])
                    P.op("dve", lambda e, psr=psr, tsl=tsl: e.tensor_tensor(out=t2[:], in0=psr[0:32, :], in1=S2q[:, tsl], op=ALU.mult), reads=[psr.name, "la_S2q"], writes=["la_t2"])
                    P.op("pool", lambda e, qa=qa: e.tensor_tensor(out=qa[0:32, :], in0=t1[:], in1=t2[:], op=ALU.add), reads=["la_t1", "la_t2"], writes=[qa.name])
                    P.dma("sp", lambda e, qa=qa, h=h, tsl=tsl: e.dma_start(out=k.qaT[h * 96:(h + 1) * 96, tsl], in_=qa[:]), reads=[qa.name], writes=[("qaT", h)])
                    for a in range(2):
                        mm(k, psk[0:96, :], Wk[:, a, h * 96:(h + 1) * 96], ckvn[:, a, :], a == 0, a == 1, ["la_Wk", "la_ckvn"], [psk.name])
                    P.op("act", lambda e, ka=ka, psk=psk: e.copy(out=ka[32:64, :], in_=psk[32:64, :]), reads=[psk.name], writes=[ka.name])
                    P.op("dve", lambda e, ka=ka, psk=psk: e.tensor_copy(out=ka[64:96, :], in_=psk[64:96, :]), reads=[psk.name], writes=[ka.name])
                    P.op("pool", lambda e, ka=ka: e.tensor_copy(out=ka[0:32, :], in_=krr[:]), reads=["la_krr"], writes=[ka.name])
                    P.dma("sp", lambda e, ka=ka, h=h, tsl=tsl: e.dma_start(out=k.kaT[h * 96:(h + 1) * 96, tsl], in_=ka[:]), reads=[ka.name], writes=[("kaT", h)])
                for j in range(4):
                    psv = k.ps[2 + (j % 2)]
                    vo = vo2[j % 2]
                    for a in range(2):
                        mm(k, psv[:], ckvn[:, a, j * 128:(j + 1) * 128], Wv[:, a, :], a == 0, a == 1, ["la_Wv", "la_ckvn"], [psv.name])
                    P.op("dve", lambda e, vo=vo, psv=psv: e.tensor_copy(out=vo[:], in_=psv[:]), reads=[psv.name], writes=[vo.name])
                    n = tb * 4 + j
                    P.dma("sp", lambda e, vo=vo, n=n: e.dma_start(out=k.vml[n * 128:(n + 1) * 128, :], in_=vo[:]), reads=[vo.name], writes=["vml"])
            P.barrier()
        attn_scratch(k, st)
        KT2 = [k.sb("la_K%d" % i, [96, S], BF16, st) for i in range(2)]
        QT2 = [k.sb("la_Q%d" % i, [96, S], BF16, st) for i in range(2)]
        V2 = [k.sb("la_V%d" % i, [128, NT, 128], BF16, st) for i in range(2)]
        rd = k.sb("la_rd", [128, 512], F32, st)
        yo2 = [k.sb("la_yo%d" % i, [64, 512], BF16, st) for i in range(2)]
        for i in range(2):
            P.op("pool", lambda e, i=i: e.memset(V2[i][:, :, 64:128], 1.0), writes=[V2[i].name])
        pf = std_pf(k)
        def load_head(h):
            KT, QT, V = KT2[h % 2], QT2[h % 2], V2[h % 2]
            P.dma("sp", lambda e, KT=KT, h=h: e.dma_start(out=KT[:], in_=k.kaT[h * 96:(h + 1) * 96, :]), reads=[("kaT", h)], writes=[KT.name])
            P.dma("sp", lambda e, QT=QT, h=h: e.dma_start(out=QT[:], in_=k.qaT[h * 96:(h + 1) * 96, :]), reads=[("qaT", h)], writes=[QT.name])
            P.dma("sp", lambda e, V=V, h=h: e.dma_start(out=V[:, :, 0:64], in_=k.vml[:, h * 64:(h + 1) * 64].rearrange("(n p) c -> p n c", p=128)), reads=["vml"], writes=[V.name])
        load_head(0)
        for h in range(8):
            KT, QT, V = KT2[h % 2], QT2[h % 2], V2[h % 2]
            for tb in range(NTB):
                if tb == NTB - 1 and h + 1 < 8:
                    load_head(h + 1)
                tsl = slice(tb * 512, (tb + 1) * 512)
                acc = k.ps[3 + (tb % 2)]

                def qk(i, step, ps, KT=KT, QT=QT, tsl=tsl):
                    s_t = step[0]
                    mm(k, ps[:], KT[:, s_t * 128:(s_t + 1) * 128], QT[:, tsl], True, True, [KT.name, QT.name], [ps.name])

                def pv(i, step, pt, first, lastf, V=V, acc=acc):
                    mm(k, acc[:], V[:, step[0], :], pt[:], first, lastf, [V.name, pt.name], [acc.name])
                attn_tb(k, causal_steps(tb), qk, pf, pv)
                yo = yo2[tb % 2]
                P.op("dve", lambda e, acc=acc: e.reciprocal(out=rd[64:128, :], in_=acc[64:128, :]), reads=[acc.name], writes=["la_rd"])
                P.op("dve", lambda e, acc=acc, yo=yo: e.tensor_tensor(out=yo[:], in0=acc[0:64, :], in1=rd[64:128, :], op=ALU.mult), reads=[acc.name, "la_rd"], writes=[yo.name])
                P.dma("sp", lambda e, yo=yo, h=h, tsl=tsl: e.dma_start(out=k.yT[1024 + h * 64:1024 + (h + 1) * 64, tsl], in_=yo[:]), reads=[yo.name], writes=[("yT", 8 + h)])
        P.barrier()


def phase_merge(k, l, xin, xout):
    P = k.P
    with ExitStack() as st:
        stage = [k.sb("mg_stg%d" % i, [128, 2048], F32, st) for i in range(2)]
        k.stage_i = 0
        Wb = [k.sb("mg_Wb%d" % i, [128, 4, D], BF16, st) for i in range(3)]
        Wo = k.sb("mg_Wo", [128, 8, D], BF16, st)
        for i in range(3):
            load_cast(k, st, "wbr", k.wbr[l][i], 128, 4, D, Wb[i], Wb[i].name, stage)
        load_cast(k, st, "wout", k.wout[l], 128, 8, D, Wo, "mg_Wo", stage)
        yb = [k.sb("mg_y%d" % i, [128, 12, 512], BF16, st) for i in range(2)]
        gb = [k.sb("mg_g%d" % i, [128, 24, 512], BF16, st) for i in range(2)]
        mT = k.sb("mg_mT", [128, 8, 512], BF16, st)
        mtmp = [[k.sb("mg_t%d_%d" % (i, j), [128, 512], BF16, st) for j in range(2)] for i in range(3)]
        xt2 = [k.sb("mg_x%d" % i, [128, D], F32, st) for i in range(2)]
        ot2 = [k.sb("mg_o%d" % i, [128, D], F32, st) for i in range(2)]
        junk = k.sb("mg_junk", [128, D], BF16, st)
        ss2 = [k.sb("mg_ss%d" % i, [128, 1], F32, st) for i in range(2)]
        rs2 = [k.sb("mg_rs%d" % i, [128, 1], F32, st) for i in range(2)]
        g1 = k.sb("mg_g1", [128, D], F32, st)
        P.dma("sp", lambda e: e.dma_start(out=g1[:], in_=k.gain_rows[l * 4 + 1].partition_broadcast(128)), writes=["mg_g1"])
        cg = FMI["gt0"]
        def load_tb(tb):
            tsl = slice(tb * 512, (tb + 1) * 512)
            y, g = yb[tb % 2], gb[tb % 2]
            P.dma("sp", lambda e, y=y, tsl=tsl: e.dma_start(out=y[:], in_=k.yT[:, tsl].rearrange("(a p) t -> p a t", p=128)), reads=[("yT", i) for i in range(16)], writes=[y.name])
            P.dma("sp", lambda e, g=g, tsl=tsl: e.dma_start(out=g[:], in_=k.zT[cg * 128:(cg + 24) * 128, tsl].rearrange("(a p) t -> p a t", p=128)), reads=[("zT", cg + i) for i in range(24)], writes=[g.name])
        load_tb(0)
        for tb in range(NTB):
            tsl = slice(tb * 512, (tb + 1) * 512)
            y, g = yb[tb % 2], gb[tb % 2]
            if tb + 1 < NTB:
                load_tb(tb + 1)
            for oc in range(8):
                for i in range(3):
                    ps = k.ps[(oc * 3 + i) % 2]
                    for a in range(4):
                        mm(k, ps[:], Wb[i][:, a, oc * 128:(oc + 1) * 128], y[:, i * 4 + a, :], a == 0, a == 3, [Wb[i].name, y.name], [ps.name])
                    tt = mtmp[i][oc % 2]
                    P.op("dve", lambda e, ps=ps, g=g, oc=oc, i=i, tt=tt: e.tensor_tensor(out=tt[:], in0=ps[:], in1=g[:, i * 8 + oc, :], op=ALU.mult), reads=[ps.name, g.name], writes=[tt.name])
                    if i == 1:
                        t0, t1 = mtmp[0][oc % 2], mtmp[1][oc % 2]
                        P.op("pool", lambda e, t0=t0, t1=t1: e.tensor_tensor(out=t0[:], in0=t0[:], in1=t1[:], op=ALU.add), reads=[t0.name, t1.name], writes=[t0.name])
                    elif i == 2:
                        t0, t2 = mtmp[0][oc % 2], mtmp[2][oc % 2]
                        P.op("dve", lambda e, oc=oc, t0=t0, t2=t2: e.tensor_tensor(out=mT[:, oc, :], in0=t0[:], in1=t2[:], op=ALU.add), reads=[t0.name, t2.name], writes=["mg_mT"])
            for j in range(4):
                n = tb * 4 + j
                xt, ot, ss, rs = xt2[j % 2], ot2[j % 2], ss2[j % 2], rs2[j % 2]
                P.dma("sp", lambda e, xt=xt, n=n: e.dma_start(out=xt[:], in_=xin[n * 128:(n + 1) * 128, :]), reads=[("dram", xin.name)], writes=[xt.name])
                pss = [k.ps[2 + 2 * (j % 2)], k.ps[3 + 2 * (j % 2)]]
                for c2 in range(2):
                    for a in range(8):
                        mm(k, pss[c2][:], mT[:, a, j * 128:(j + 1) * 128], Wo[:, a, c2 * 512:(c2 + 1) * 512], a == 0, a == 7, ["mg_mT", "mg_Wo"], [pss[c2].name])
                resid_norm(k, pss, xt, ot, ss, rs, junk, g1, xout, n)
        P.barrier()


def resid_norm(k, pss, xt, ot, ss, rs, junk, grow, xout, n):
    P = k.P
    P.op("pool", lambda e: e.memset(ss[:], 0.0), writes=[ss.name])
    for c2 in range(2):
        P.op("act", lambda e, c2=c2: e.activation(out=ot[:, c2 * 512:(c2 + 1) * 512], in_=pss[c2][:], func=AF.Copy), reads=[pss[c2].name], writes=[ot.name])
    P.op("act", lambda e: e.activation(out=junk[:], in_=ot[:], func=AF.Square, accum_out=ss[:]), reads=[ot.name, ss.name], writes=[junk.name, ss.name])
    P.op("act", lambda e: e.activation(out=rs[:], in_=ss[:], func=AF.Sqrt, bias=k.epsT[:], scale=1.0 / D), reads=[ss.name, "epsT"], writes=[rs.name])
    P.op("dve", lambda e: e.reciprocal(out=rs[:], in_=rs[:]), reads=[rs.name], writes=[rs.name])
    P.op("dve", lambda e: e.scalar_tensor_tensor(out=ot[:], in0=ot[:], scalar=rs[:, 0:1], in1=grow[:], op0=ALU.mult, op1=ALU.mult), reads=[ot.name, rs.name, grow.name], writes=[ot.name])
    P.op("pool", lambda e: e.tensor_tensor(out=ot[:], in0=ot[:], in1=xt[:], op=ALU.add), reads=[ot.name, xt.name], writes=[ot.name])
    P.dma("sp", lambda e: e.dma_start(out=xout[n * 128:(n + 1) * 128, :], in_=ot[:]), reads=[ot.name], writes=[("dram", xout.name)])


def phase_ffn(k, l, xin, xout):
    P = k.P
    with ExitStack() as st:
        stage = [k.sb("ff_stg%d" % i, [128, 1024], F32, st) for i in range(2)]
        k.stage_i = 0
        Wg = k.sb("ff_Wg", [128, 8, DFF], BF16, st)
        Wu = k.sb("ff_Wu", [128, 8, DFF], BF16, st)
        Wd = k.sb("ff_Wd", [128, NFC, D], BF16, st)
        for c0 in range(0, DFF, 512):
            cw = min(512, DFF - c0)
            for (W, src) in ((Wg, k.wg), (Wu, k.wu)):
                per = 1024 // cw
                for kc in range(0, 8, per):
                    m = min(per, 8 - kc)
                    stg = stage[k.stage_i % 2]
                    k.stage_i += 1
                    sv = stg[:, 0:m * cw].rearrange("p (a c) -> p a c", a=m)
                    srcv = src[l][kc * 128:(kc + m) * 128, c0:c0 + cw].rearrange("(a p) c -> p a c", p=128)
                    P.dma("sp", lambda e, sv=sv, srcv=srcv: e.dma_start(out=sv, in_=srcv), writes=[stg.name])
                    eng = ("dve", "act", "dve", "pool")[k.stage_i % 4]
                    if eng == "act":
                        P.op("act", lambda e, W=W, kc=kc, m=m, c0=c0, cw=cw, sv=sv: e.copy(out=W[:, kc:kc + m, c0:c0 + cw], in_=sv), reads=[stg.name], writes=[W.name])
                    else:
                        P.op(eng, lambda e, W=W, kc=kc, m=m, c0=c0, cw=cw, sv=sv: e.tensor_copy(out=W[:, kc:kc + m, c0:c0 + cw], in_=sv), reads=[stg.name], writes=[W.name])
        load_cast(k, st, "wd", k.wd[l], 128, NFC, D, Wd, "ff_Wd", stage, sw=1024)
        hT = k.sb("ff_hT", [128, 8, 512], BF16, st)
        aT = k.sb("ff_aT", [128, NFC, 512], BF16, st)
        sg = [k.sb("ff_sg%d" % i, [128, 512], BF16, st) for i in range(2)]
        xs = [k.sb("ff_xs%d" % i, [128, D], F32, st) for i in range(2)]
        ot2 = [k.sb("ff_o0", [128, D], F32, st)] * 2
        ss2 = [k.sb("ff_ss%d" % i, [128, 1], F32, st) for i in range(2)]
        rs2 = [k.sb("ff_rs%d" % i, [128, 1], F32, st) for i in range(2)]
        xn2 = [k.sb("ff_xn%d" % i, [128, D], BF16, st) for i in range(2)]
        g3 = k.sb("ff_g3", [128, D], F32, st)
        P.dma("sp", lambda e: e.dma_start(out=g3[:], in_=k.gain_rows[l * 4 + 3].partition_broadcast(128)), writes=["ff_g3"])
        gbc = k.gn[:, l * 4 + 2, :].unsqueeze(2).broadcast_to([128, 8, 128])
        for tb in range(NTB):
            for j in range(4):
                n = tb * 4 + j
                xt, xn, ss, rs = xs[j % 2], xn2[j % 2], ss2[j % 2], rs2[j % 2]
                P.dma("sp", lambda e, xt=xt, n=n: e.dma_start(out=xt[:], in_=xin[n * 128:(n + 1) * 128, :]), reads=[("dram", xin.name)], writes=[xt.name])
                norm_tile_T(k, "ff", xt, ss, rs, xn, n)
                for kc in range(8):
                    P.op("pe", lambda e, kc=kc, xn=xn: e.transpose(out=k.psb[:, kc * 128:(kc + 1) * 128], in_=xn[:, kc * 128:(kc + 1) * 128], identity=k.ident[:]), reads=[xn.name, "ident"], writes=["ps7"])
                P.op("dve", lambda e, j=j: e.tensor_tensor(out=hT[:, :, j * 128:(j + 1) * 128], in0=k.psb.rearrange("p (a t) -> p a t", a=8), in1=gbc, op=ALU.mult), reads=["ps7", "gn"], writes=["ff_hT"])
            for fc in range(NFC):
                psg, psu = (k.ps[0], k.ps[1]) if fc % 2 == 0 else (k.ps[2], k.ps[3])
                for a in range(8):
                    mm(k, psg[:], Wg[:, a, fc * 128:(fc + 1) * 128], hT[:, a, :], a == 0, a == 7, ["ff_Wg", "ff_hT"], [psg.name])
                for a in range(8):
                    mm(k, psu[:], Wu[:, a, fc * 128:(fc + 1) * 128], hT[:, a, :], a == 0, a == 7, ["ff_Wu", "ff_hT"], [psu.name])
                s_ = sg[fc % 2]
                P.op("act", lambda e, s_=s_, psg=psg: e.activation(out=s_[:], in_=psg[:], func=AF.Silu), reads=[psg.name], writes=[s_.name])
                P.op("dve", lambda e, s_=s_, psu=psu, fc=fc: e.tensor_tensor(out=aT[:, fc, :], in0=psu[:], in1=s_[:], op=ALU.mult), reads=[psu.name, s_.name], writes=["ff_aT"])
            for j in range(4):
                n = tb * 4 + j
                pss = [k.ps[4], k.ps[5]]
                for c2 in range(2):
                    for fc in range(NFC):
                        mm(k, pss[c2][:], aT[:, fc, j * 128:(j + 1) * 128], Wd[:, fc, c2 * 512:(c2 + 1) * 512], fc == 0, fc == NFC - 1, ["ff_aT", "ff_Wd"], [pss[c2].name])
                xt = xs[j % 2]
                P.dma("sp", lambda e, xt=xt, n=n: e.dma_start(out=xt[:], in_=xin[n * 128:(n + 1) * 128, :]), reads=[("dram", xin.name)], writes=[xt.name])
                resid_norm(k, pss, xt, ot2[j % 2], ss2[j % 2], rs2[j % 2], xn2[j % 2], g3, xout, n)
        P.barrier()


def phase_nsa(k, l):
    P = k.P
    c_bq, c_kcr, c_vcr, c_kslc, c_kwin, c_bg = FMI["bq0"], FMI["kcr"], FMI["vcr"], FMI["kslc"], FMI["kwin"], FMI["bg"]
    with ExitStack() as st:
        attn_scratch(k, st)
        G24 = k.sb("ns_G", [24, S], BF16, st)
        gsel = k.sb("ns_gsel", [24, 24 * 128], BF16, st)
        impacc = k.sb("ns_imp", [64, 2, S], F32, st)
        kcmpT = [k.sb("ns_kc%d" % g, [64, 256], BF16, st) for g in range(2)]
        Vc = [k.sb("ns_vc%d" % g, [128, 2, 128], BF16, st) for g in range(2)]
        gr = [k.sb("ns_gr%d" % i, [128, 512], F32, st) for i in range(2)]
        rd = k.sb("ns_rd", [128, 512], F32, st)
        coef = k.sb("ns_coef", [128, 512], F32, st)
        P.dma("sp", lambda e: e.dma_start(out=G24[:], in_=k.zT[c_bg * 128:c_bg * 128 + 24, :]), reads=[("zT", c_bg)], writes=["ns_G"])
        P.dma("sp", lambda e: e.dma_start(out=gsel[:], in_=k.c_gsel[:, :]), writes=["ns_gsel"])
        for g in range(2):
            P.op("pool", lambda e, g=g: e.memset(Vc[g][:, :, 64:128], 1.0), writes=[Vc[g].name])

        def grep(h, b, tsl, dst):
            psg = k.ps[7]
            col = (h * 3 + b) * 128
            mm(k, psg[:], gsel[:, col:col + 128], G24[:, tsl], True, True, ["ns_gsel", "ns_G"], [psg.name])
            P.op("act", lambda e: e.copy(out=dst[:], in_=psg[:]), reads=[psg.name], writes=[dst.name])

        with ExitStack() as st2:
            stage = [k.sb("ns_stg%d" % i, [128, 2048], F32, st2) for i in range(2)]
            W1 = k.sb("ns_W1", [64, 32, 128], BF16, st2)
            W2 = k.sb("ns_W2", [128, 64], BF16, st2)
            posT = k.sb("ns_pos", [64, 64], BF16, st2)
            xT = k.sb("ns_xT", [64, S], BF16, st2)
            hs = k.sb("ns_hs", [128, 256], BF16, st2)
            bsb = k.sb("ns_bsb", [128, 1], F32, st2)
            P.op("pool", lambda e: e.memset(hs[:], 0.0), writes=["ns_hs"])
            P.dma("sp", lambda e: e.dma_start(out=stage[0][0:64, 0:64], in_=k.cpos[l]), writes=[stage[0].name])
            P.op("dve", lambda e: e.tensor_copy(out=posT[:], in_=stage[0][0:64, 0:64]), reads=[stage[0].name], writes=["ns_pos"])
            for kv in range(2):
                for half in range(2):
                    stg = stage[half]
                    P.dma("sp", lambda e, stg=stg, kv=kv, half=half: e.dma_start(out=stg[0:64, :], in_=k.cw1[l][kv][:, half * 2048:(half + 1) * 2048]), writes=[stg.name])
                    P.op("dve", lambda e, stg=stg, half=half: e.tensor_copy(out=W1[:, half * 16:(half + 1) * 16, :], in_=stg[0:64, :].rearrange("p (a c) -> p a c", a=16)), reads=[stg.name], writes=["ns_W1"])
                P.dma("sp", lambda e, kv=kv: e.dma_start(out=stage[0][:, 0:64], in_=k.cw2[l][kv]), writes=[stage[0].name])
                P.op("dve", lambda e: e.tensor_copy(out=W2[:], in_=stage[0][:, 0:64]), reads=[stage[0].name], writes=["ns_W2"])
                for g in range(2):
                    cc = c_kcr if kv == 0 else c_vcr
                    P.dma("sp", lambda e, cc=cc, g=g: e.dma_start(out=xT[:], in_=k.zT[cc * 128 + g * 64:cc * 128 + g * 64 + 64, :]), reads=[("zT", cc)], writes=["ns_xT"])
                    hid, bps = k.ps[2], k.ps[3]
                    for li in range(32):
                        mm(k, hid[:, 0:255], W1[:, li, :], xT[:, li:li + 4065:16], li == 0, li == 31, ["ns_W1", "ns_xT"], [hid.name])
                    for li in range(32):
                        mm(k, bps[:, 0:1], W1[:, li, :], posT[:, kv * 32 + li:kv * 32 + li + 1], li == 0, li == 31, ["ns_W1", "ns_pos"], [bps.name])
                    P.op("dve", lambda e, bps=bps: e.tensor_copy(out=bsb[:], in_=bps[:, 0:1]), reads=[bps.name], writes=["ns_bsb"])
                    P.op("act", lambda e, hid=hid: e.activation(out=hs[:, 0:255], in_=hid[:, 0:255], func=AF.Silu, bias=bsb[:], scale=1.0), reads=[hid.name, "ns_bsb"], writes=["ns_hs"])
                    if kv == 0:
                        pk = k.ps[4]
                        mm(k, pk[0:64, 0:256], W2[:], hs[:], True, True, ["ns_W2", "ns_hs"], [pk.name])
                        P.op("dve", lambda e, g=g, pk=pk: e.tensor_copy(out=kcmpT[g][:], in_=pk[0:64, 0:256]), reads=[pk.name], writes=[kcmpT[g].name])
                    else:
                        for ch in range(2):
                            pv_ = k.ps[4 + ch]
                            mm(k, pv_[:, 0:64], hs[:, ch * 128:(ch + 1) * 128], W2[:], True, True, ["ns_W2", "ns_hs"], [pv_.name])
                            P.op("dve", lambda e, g=g, ch=ch, pv_=pv_: e.tensor_copy(out=Vc[g][:, ch, 0:64], in_=pv_[:, 0:64]), reads=[pv_.name], writes=[Vc[g].name])
            P.barrier()
        with ExitStack() as st2:
            cm = k.sb("ns_cm", [128, 2, S], BF16, st2)
            ovl = k.sb("ns_ovl", [128, 2, 64], BF16, st2)
            P.dma("sp", lambda e: e.dma_start(out=cm[:].rearrange("p a b -> p (a b)"), in_=k.c_cm[:, :]), writes=["ns_cm"])
            P.dma("sp", lambda e: e.dma_start(out=ovl[:].rearrange("p a b -> p (a b)"), in_=k.c_ovl[:, :]), writes=["ns_ovl"])
            QT2 = [k.sb("ns_Q%d" % i, [64, S], BF16, st2) for i in range(2)]
            tmpi = k.sb("ns_tmpi", [64, 512], F32, st2)
            yo2 = [k.sb("ns_yc%d" % i, [64, 512], BF16, st2) for i in range(2)]
            for h in range(8):
                g = h // 4
                QT = QT2[h % 2]
                cq = c_bq + h // 2
                r0 = (h % 2) * 64
                P.dma("sp", lambda e, QT=QT, cq=cq, r0=r0: e.dma_start(out=QT[:], in_=k.zT[cq * 128 + r0:cq * 128 + r0 + 64, :]), reads=[("zT", cq)], writes=[QT.name])
                for tb in range(NTB):
                    tsl = slice(tb * 512, (tb + 1) * 512)
                    steps = [(0, True if tb < 5 else None)] + ([(1, True)] if tb >= 4 else [])
                    acc, impu = (k.ps[3], k.ps[5]) if tb % 2 == 0 else (k.ps[4], k.ps[6])
                    grep(h, 0, tsl, gr[0])

                    def qk(i, step, ps, QT=QT, g=g, tsl=tsl):
                        ch = step[0]
                        mm(k, ps[:], kcmpT[g][:, ch * 128:(ch + 1) * 128], QT[:, tsl], True, True, [kcmpT[g].name, QT.name], [ps.name])

                    def pf(i, step, ps, pt, tsl=tsl):
                        ch, mk = step
                        P.op("act", lambda e: e.activation(out=pt[:], in_=ps[:], func=AF.Exp), reads=[ps.name], writes=[pt.name])
                        if mk:
                            P.op("dve", lambda e: e.tensor_tensor(out=pt[:], in0=pt[:], in1=cm[:, ch, tsl], op=ALU.mult), reads=[pt.name, "ns_cm"], writes=[pt.name])

                    def pv(i, step, pt, first, lastf, g=g, acc=acc, impu=impu):
                        ch = step[0]
                        mm(k, acc[:], Vc[g][:, ch, :], pt[:], first, lastf, [Vc[g].name, pt.name], [acc.name])
                        mm(k, impu[0:64, :], ovl[:, ch, :], pt[:], first, lastf, ["ns_ovl", pt.name], [impu.name])
                    attn_tb(k, steps, qk, pf, pv)
                    P.op("dve", lambda e, acc=acc: e.tensor_scalar(out=rd[64:128, :], in0=acc[64:128, :], scalar1=1e-30, scalar2=None, op0=ALU.max), reads=[acc.name], writes=["ns_rd"])
                    P.op("act", lambda e: e.activation(out=rd[64:128, :], in_=rd[64:128, :], func=AF.Ln), reads=["ns_rd"], writes=["ns_rd"])
                    P.op("act", lambda e: e.activation(out=rd[64:128, :], in_=rd[64:128, :], func=AF.Exp, scale=-1.0), reads=["ns_rd"], writes=["ns_rd"])
                    if h % 4 == 0:
                        P.op("dve", lambda e, impu=impu, g=g, tsl=tsl: e.tensor_tensor(out=impacc[:, g, tsl], in0=impu[0:64, :], in1=rd[64:128, :], op=ALU.mult), reads=[impu.name, "ns_rd"], writes=["ns_imp"])
                    else:
                        P.op("dve", lambda e, impu=impu: e.tensor_tensor(out=tmpi[:], in0=impu[0:64, :], in1=rd[64:128, :], op=ALU.mult), reads=[impu.name, "ns_rd"], writes=["ns_tmpi"])
                        P.op("pool", lambda e, g=g, tsl=tsl: e.tensor_tensor(out=impacc[:, g, tsl], in0=impacc[:, g, tsl], in1=tmpi[:], op=ALU.add), reads=["ns_imp", "ns_tmpi"], writes=["ns_imp"])
                    P.op("dve", lambda e: e.tensor_tensor(out=coef[64:128, :], in0=rd[64:128, :], in1=gr[0][64:128, :], op=ALU.mult), reads=["ns_rd", gr[0].name], writes=["ns_coef"])
                    yo = yo2[tb % 2]
                    P.op("dve", lambda e, acc=acc, yo=yo: e.tensor_tensor(out=yo[:], in0=acc[0:64, :], in1=coef[64:128, :], op=ALU.mult), reads=[acc.name, "ns_coef"], writes=[yo.name])
                    P.dma("sp", lambda e, yo=yo, h=h, tsl=tsl: e.dma_start(out=k.ycmp[h * 64:(h + 1) * 64, tsl], in_=yo[:]), reads=[yo.name], writes=[("ycmp", h)])
            if "impd" in k.taps:
                P.dma("sp", lambda e: e.dma_start(out=k.impd[:, :].rearrange("(g j) t -> j g t", g=2), in_=impacc[:]), reads=["ns_imp"], writes=["impd"])
            P.barrier()
        with ExitStack() as st2:
            identf = k.sb("ns_idf", [128, 128], F32, st2)
            valid = k.sb("ns_valid", [128, NT, 64], F32, st2)
            addc = k.sb("ns_addc", [128, NT, 64], F32, st2)
            P.dma("sp", lambda e: e.dma_start(out=identf[:], in_=k.c_identf[:, :]), writes=["ns_idf"])
            P.dma("sp", lambda e: e.dma_start(out=valid[:].rearrange("p a b -> p (a b)"), in_=k.c_valid[:, :]), writes=["ns_valid"])
            P.dma("sp", lambda e: e.dma_start(out=addc[:].rearrange("p a b -> p (a b)"), in_=k.c_addc[:, :]), writes=["ns_addc"])
            sc2 = [k.sb("ns_sc%d" % i, [128, 64], F32, st2) for i in range(2)]
            sr2 = [k.sb("ns_sr%d" % i, [128, 64], F32, st2) for i in range(2)]
            se2 = [k.sb("ns_se%d" % i, [128, 64], F32, st2) for i in range(2)]
            m8a = [k.sb("ns_ma%d" % i, [128, 8], F32, st2) for i in range(2)]
            m8b = [k.sb("ns_mb%d" % i, [128, 8], F32, st2) for i in range(2)]
            bst = [k.sb("ns_bst%d" % i, [64, 512], BF16, st2) for i in range(2)]
            m3e4 = k.sb("ns_m3e4", [128, 1], F32, st2)
            P.op("pool", lambda e: e.memset(m3e4[:], -30000.0), writes=["ns_m3e4"])
            for g in range(2):
                for n in range(NT):
                    sc, sr, se, ma, mb = sc2[n % 2], sr2[n % 2], se2[n % 2], m8a[n % 2], m8b[n % 2]
                    tp = k.ps[2 + (n % 2)]
                    P.op("pe", lambda e, tp=tp, g=g, n=n: e.transpose(out=tp[:, 0:64], in_=impacc[:, g, n * 128:(n + 1) * 128], identity=identf[0:64, 0:64]), reads=["ns_imp", "ns_idf"], writes=[tp.name])
                    P.op("dve", lambda e, tp=tp, sc=sc, n=n: e.tensor_tensor(out=sc[:], in0=tp[:, 0:64], in1=valid[:, n, :], op=ALU.mult), reads=[tp.name, "ns_valid"], writes=[sc.name])
                    P.op("dve", lambda e, sc=sc, n=n: e.tensor_tensor(out=sc[:], in0=sc[:], in1=addc[:, n, :], op=ALU.add), reads=[sc.name, "ns_addc"], writes=[sc.name])
                    P.op("dve", lambda e, sc=sc, ma=ma: e.max(out=ma[:], in_=sc[:]), reads=[sc.name], writes=[ma.name])
                    P.op("dve", lambda e, sc=sc, sr=sr, ma=ma: e.match_replace(out=sr[:], in_to_replace=ma[:], in_values=sc[:], imm_value=-1e9), reads=[sc.name, ma.name], writes=[sr.name])
                    P.op("dve", lambda e, sr=sr, mb=mb: e.max(out=mb[:], in_=sr[:]), reads=[sr.name], writes=[mb.name])
                    P.op("pool", lambda e, mb=mb: e.tensor_scalar(out=mb[:, 7:8], in0=mb[:, 7:8], scalar1=-0.5, scalar2=None, op0=ALU.max), reads=[mb.name], writes=[mb.name])
                    P.op("dve", lambda e, sc=sc, se=se, mb=mb: e.tensor_scalar(out=se[:], in0=sc[:], scalar1=mb[:, 7:8], scalar2=None, op0=ALU.is_ge), reads=[sc.name, mb.name], writes=[se.name])
                    tq = k.ps[4 + (n % 2)]
                    P.op("pe", lambda e, tq=tq, se=se: e.transpose(out=tq[0:64, 0:128], in_=se[:], identity=identf[:]), reads=[se.name, "ns_idf"], writes=[tq.name])
                    bs = bst[(n // 4) % 2]
                    P.op("act", lambda e, tq=tq, bs=bs, n=n: e.activation(out=bs[:, (n % 4) * 128:(n % 4 + 1) * 128], in_=tq[0:64, 0:128], func=AF.Identity, bias=m3e4[0:64, :], scale=30000.0), reads=[tq.name, "ns_m3e4"], writes=[bs.name])
                    if n % 4 == 3:
                        tbb = n // 4
                        P.dma("sp", lambda e, bs=bs, g=g, tbb=tbb: e.dma_start(out=k.biasT[g * 64:(g + 1) * 64, tbb * 512:(tbb + 1) * 512], in_=bs[:]), reads=[bs.name], writes=[("biasT", g)])
            P.barrier()
        with ExitStack() as st2:
            kE = [k.sb("ns_kE%d" % g, [128, S], BF16, st2) for g in range(2)]
            kwT = [k.sb("ns_kw%d" % g, [128, S], BF16, st2) for g in range(2)]
            Vs = [k.sb("ns_Vs%d" % g, [128, NT, 128], BF16, st2) for g in range(2)]
            Vw = [k.sb("ns_Vw%d" % g, [128, NT, 128], BF16, st2) for g in range(2)]
            qb2 = [k.sb("ns_qb%d" % i, [128, S], BF16, st2) for i in range(2)]
            ycm2 = [k.sb("ns_ycm%d" % i, [64, 512], BF16, st2) for i in range(2)]
            ys = k.sb("ns_ys", [64, 512], F32, st2)
            yw = k.sb("ns_yw", [64, 512], F32, st2)
            yo2 = [k.sb("ns_yo%d" % i, [64, 512], BF16, st2) for i in range(2)]
            for g in range(2):
                P.dma("sp", lambda e, g=g: e.dma_start(out=kE[g][0:64, :], in_=k.zT[c_kslc * 128 + g * 64:c_kslc * 128 + g * 64 + 64, :]), reads=[("zT", c_kslc)], writes=[kE[g].name])
                P.dma("sp", lambda e, g=g: e.dma_start(out=kE[g][64:128, :], in_=k.c_E[:, :]), writes=[kE[g].name])
                P.op("pool", lambda e, g=g: e.memset(kwT[g][64:128, :], 0.0), writes=[kwT[g].name])
                P.dma("sp", lambda e, g=g: e.dma_start(out=kwT[g][0:64, :], in_=k.zT[c_kwin * 128 + g * 64:c_kwin * 128 + g * 64 + 64, :]), reads=[("zT", c_kwin)], writes=[kwT[g].name])
                P.op("pool", lambda e, g=g: e.memset(Vs[g][:, :, 64:128], 1.0), writes=[Vs[g].name])
                P.op("pool", lambda e, g=g: e.memset(Vw[g][:, :, 64:128], 1.0), writes=[Vw[g].name])
                P.dma("sp", lambda e, g=g: e.dma_start(out=Vs[g][:, :, 0:64], in_=k.vtm[:, 512 + g * 64:512 + (g + 1) * 64].rearrange("(n p) c -> p n c", p=128)), reads=[("vtm", 512)], writes=[Vs[g].name])
                P.dma("sp", lambda e, g=g: e.dma_start(out=Vw[g][:, :, 0:64], in_=k.vtm[:, 640 + g * 64:640 + (g + 1) * 64].rearrange("(n p) c -> p n c", p=128)), reads=[("vtm", 512)], writes=[Vw[g].name])
            pf = std_pf(k)
            def load_qb(h):
                g = h // 4
                qb = qb2[h % 2]
                cq = c_bq + h // 2
                r0 = (h % 2) * 64
                P.dma("sp", lambda e, qb=qb, cq=cq, r0=r0: e.dma_start(out=qb[0:64, :], in_=k.zT[cq * 128 + r0:cq * 128 + r0 + 64, :]), reads=[("zT", cq)], writes=[qb.name])
                P.dma("sp", lambda e, qb=qb, g=g: e.dma_start(out=qb[64:128, :], in_=k.biasT[g * 64:(g + 1) * 64, :]), reads=[("biasT", g)], writes=[qb.name])
            load_qb(0)
            for h in range(8):
                g = h // 4
                qb = qb2[h % 2]
                for tb in range(NTB):
                    if tb == NTB - 1 and h + 1 < 8:
                        load_qb(h + 1)
                    tsl = slice(tb * 512, (tb + 1) * 512)
                    acs, acw = (k.ps[3], k.ps[5]) if tb % 2 == 0 else (k.ps[4], k.ps[6])
                    ycm = ycm2[tb % 2]
                    P.dma("sp", lambda e, ycm=ycm, h=h, tsl=tsl: e.dma_start(out=ycm[:], in_=k.ycmp[h * 64:(h + 1) * 64, tsl]), reads=[("ycmp", h)], writes=[ycm.name])
                    grep(h, 1, tsl, gr[0])
                    grep(h, 2, tsl, gr[1])

                    def qk_s(i, step, ps, qb=qb, g=g, tsl=tsl):
                        s_t = step[0]
                        mm(k, ps[:], kE[g][:, s_t * 128:(s_t + 1) * 128], qb[:, tsl], True, True, [kE[g].name, qb.name], [ps.name])

                    def pv_s(i, step, pt, first, lastf, g=g, acs=acs):
                        mm(k, acs[:], Vs[g][:, step[0], :], pt[:], first, lastf, [Vs[g].name, pt.name], [acs.name])
                    attn_tb(k, causal_steps(tb), qk_s, pf, pv_s)
                    wsteps = []
                    for s_t in range(max(0, 4 * tb - 4), 4 * tb + 4):
                        wsteps.append((s_t, (s_t - 4 * tb) if s_t >= 4 * tb else 4 + (s_t - (4 * tb - 4))))

                    def qk_w(i, step, ps, qb=qb, g=g, tsl=tsl):
                        s_t = step[0]
                        mm(k, ps[:], kwT[g][:, s_t * 128:(s_t + 1) * 128], qb[:, tsl], True, True, [kwT[g].name, qb.name], [ps.name])

                    def pv_w(i, step, pt, first, lastf, g=g, acw=acw):
                        mm(k, acw[:], Vw[g][:, step[0], :], pt[:], first, lastf, [Vw[g].name, pt.name], [acw.name])
                    attn_tb(k, wsteps, qk_w, pf, pv_w)
                    P.op("act", lambda e, acs=acs: e.activation(out=rd[64:128, :], in_=acs[64:128, :], func=AF.Ln), reads=[acs.name], writes=["ns_rd"])
                    P.op("act", lambda e: e.activation(out=rd[64:128, :], in_=rd[64:128, :], func=AF.Exp, scale=-1.0), reads=["ns_rd"], writes=["ns_rd"])
                    P.op("dve", lambda e: e.tensor_tensor(out=coef[64:128, :], in0=rd[64:128, :], in1=gr[0][64:128, :], op=ALU.mult), reads=["ns_rd", gr[0].name], writes=["ns_coef"])
                    P.op("dve", lambda e, acs=acs: e.tensor_tensor(out=ys[:], in0=acs[0:64, :], in1=coef[64:128, :], op=ALU.mult), reads=[acs.name, "ns_coef"], writes=["ns_ys"])
                    P.op("act", lambda e, acw=acw: e.activation(out=rd[64:128, :], in_=acw[64:128, :], func=AF.Ln), reads=[acw.name], writes=["ns_rd"])
                    P.op("act", lambda e: e.activation(out=rd[64:128, :], in_=rd[64:128, :], func=AF.Exp, scale=-1.0), reads=["ns_rd"], writes=["ns_rd"])
                    P.op("dve", lambda e: e.tensor_tensor(out=coef[64:128, :], in0=rd[64:128, :], in1=gr[1][64:128, :], op=ALU.mult), reads=["ns_rd", gr[1].name], writes=["ns_coef"])
                    P.op("dve", lambda e, acw=acw: e.tensor_tensor(out=yw[:], in0=acw[0:64, :], in1=coef[64:128, :], op=ALU.mult), reads=[acw.name, "ns_coef"], writes=["ns_yw"])
                    P.op("pool", lambda e: e.tensor_tensor(out=ys[:], in0=ys[:], in1=yw[:], op=ALU.add), reads=["ns_ys", "ns_yw"], writes=["ns_ys"])
                    yo = yo2[tb % 2]
                    P.op("pool", lambda e, yo=yo, ycm=ycm: e.tensor_tensor(out=yo[:], in0=ys[:], in1=ycm[:], op=ALU.add), reads=["ns_ys", ycm.name], writes=[yo.name])
                    P.dma("sp", lambda e, yo=yo, h=h, tsl=tsl: e.dma_start(out=k.yT[512 + h * 64:512 + (h + 1) * 64, tsl], in_=yo[:]), reads=[yo.name], writes=[("yT", 4 + h // 2)])
        P.barrier()


def _consts():
    c = {}
    c["c_ident"] = np.eye(128, dtype=np.float32).astype(ml_dtypes.bfloat16)
    c["c_identf"] = np.eye(128, dtype=np.float32)
    sl = np.arange(128)[:, None]
    tl = np.arange(512)[None, :]
    cm = np.zeros((128, 8, 512), np.float32)
    for o in range(4):
        cm[:, o, :] = (sl + 128 * o <= tl)
        cm[:, 4 + o, :] = 1.0 - cm[:, o, :]
    c["c_cmask"] = cm.reshape(128, 8 * 512).astype(ml_dtypes.bfloat16)
    half = 16
    freq = (10000.0 ** (-np.arange(half, dtype=np.float32) / half)).astype(np.float32)
    fr = np.zeros((32, 4), np.float32)
    fr[:, 0] = np.concatenate([freq, freq])
    fr[:, 1] = np.concatenate([-np.ones(16), np.ones(16)])
    fr[:, 2] = np.concatenate([np.full(16, np.pi), np.full(16, -np.pi)])
    fr[:, 3] = -np.pi
    c["c_freq"] = fr
    nb = 255
    n = np.arange(256)
    t = np.arange(S)
    vis = ((16 * n[:, None] + 31) <= t[None, :]) & (n[:, None] < nb)
    c["c_cm"] = np.concatenate([vis[0:128], vis[128:256]], axis=1).astype(np.float32).astype(ml_dtypes.bfloat16)
    c_start = np.arange(nb) * 16
    s_start = np.arange(64) * 64
    ovl = ((c_start[:, None] < s_start[None, :] + 64) & (c_start[:, None] + 32 > s_start[None, :])).astype(np.float32)
    ovl = np.concatenate([ovl, np.zeros((1, 64), np.float32)], 0)
    c["c_ovl"] = np.concatenate([ovl[0:128], ovl[128:256]], axis=1).astype(ml_dtypes.bfloat16)
    cur = t // 64
    jb = np.arange(64)
    valid = (jb[None, :] <= cur[:, None])
    forced = (jb[None, :] == 0) | (jb[None, :] == cur[:, None]) | (jb[None, :] == cur[:, None] - 1)
    addc = np.where(valid, 1e4 * forced.astype(np.float32), -1.0).astype(np.float32)
    c["c_valid"] = valid.astype(np.float32).reshape(NT, 128, 64).transpose(1, 0, 2).reshape(128, NT * 64).copy()
    c["c_addc"] = addc.reshape(NT, 128, 64).transpose(1, 0, 2).reshape(128, NT * 64).copy()
    E = (jb[:, None] == (t[None, :] // 64)).astype(np.float32)
    c["c_E"] = E.astype(ml_dtypes.bfloat16)
    gs = np.zeros((24, 24, 128), np.float32)
    for i in range(24):
        gs[i, i, :] = 1.0
    c["c_gsel"] = gs.reshape(24, 24 * 128).astype(ml_dtypes.bfloat16)
    return c


def prep_weights(inp):
    f = lambda a: np.ascontiguousarray(np.asarray(a, dtype=np.float32))
    w_in = f(inp["w_in"])
    out = {}
    cols = []
    for (name, c0, w, kind) in FM:
        blk = np.zeros((DEPTH, D, 128), np.float32)
        if name == "krp":
            kr = w_in[:, :, 3488:3520]
            blk[:, :, 0:32] = np.concatenate([kr[:, :, 16:32], kr[:, :, 0:16]], axis=-1)
        else:
            blk[:, :, 0:w] = w_in[:, :, c0:c0 + w]
        cols.append(blk)
    out["w_fm"] = np.concatenate(cols, axis=-1)
    out["w_tm"] = np.concatenate([w_in[:, :, 512:1024], w_in[:, :, BKV + 384:BKV + 512], w_in[:, :, BKV + 640:BKV + 768]], axis=-1)
    g = f(inp["norm_gains"])
    out["gain_rows"] = g.reshape(DEPTH * 4, D).copy()
    out["gains"] = g.reshape(DEPTH * 4, 8, 128).transpose(2, 0, 1).reshape(128, DEPTH * 4 * 8).copy()
    mc = f(inp["m_conv"])
    out["mconv"] = mc.reshape(DEPTH, 4, 4, 128).transpose(0, 3, 2, 1).reshape(DEPTH, 128, 16).copy()
    gb = f(inp["m_gate_bias"])
    out["gbias"] = gb.reshape(DEPTH, 2, 4).transpose(0, 2, 1).copy()
    out["hnorm"] = f(inp["m_head_norm"]).transpose(0, 2, 1).copy()
    cp = f(inp["nsa_cmp_pos"])
    out["cpos"] = cp.transpose(0, 3, 1, 2).reshape(DEPTH, 64, 64).copy()
    w1 = f(inp["nsa_cmp_w1"])
    out["cw1"] = w1.reshape(DEPTH, 2, 32, 64, 128).transpose(0, 1, 3, 2, 4).reshape(DEPTH, 2, 64, 32 * 128).copy()
    out["cw2"] = f(inp["nsa_cmp_w2"])
    out["qn"] = f(inp["mla_q_norm"]).reshape(DEPTH, 3, 128).transpose(0, 2, 1).copy()
    out["kvn"] = f(inp["mla_kv_norm"]).reshape(DEPTH, 2, 128).transpose(0, 2, 1).copy()
    wq = f(inp["mla_w_uq"]).reshape(DEPTH, 384, 8, 96)
    rope = wq[..., 64:96]
    ropep = np.concatenate([rope[..., 16:32], rope[..., 0:16]], axis=-1)
    out["wuq"] = np.concatenate([rope, wq[..., 0:64], ropep], axis=-1).reshape(DEPTH, 384, 8 * 128).copy()
    wkv = f(inp["mla_w_ukv"]).reshape(DEPTH, 256, 8, 128)
    out["wuk"] = np.concatenate([np.zeros((DEPTH, 256, 8, 32), np.float32), wkv[..., 0:64]], axis=-1).reshape(DEPTH, 256, 8 * 96).copy()
    out["wuv"] = wkv[..., 64:128].reshape(DEPTH, 256, 512).copy()
    out["wbr"] = f(inp["w_branch"])
    out["wout"] = f(inp["w_out"])
    out["wg"] = f(inp["w_ffn_gate"])
    out["wu"] = f(inp["w_ffn_up"])
    out["wd"] = f(inp["w_ffn_down"])
    out.update(_consts())
    return out


_CACHE = {}


def kernel(**inputs):
    shared = prep_weights(inputs)
    x = np.ascontiguousarray(np.asarray(inputs["x"], dtype=np.float32))
    pos = np.ascontiguousarray(np.asarray(inputs["positions"], dtype=np.int32))
    if "nc" not in _CACHE:
        _CACHE["nc"] = build()
    nc = _CACHE["nc"]
    in_maps = []
    for c in range(NCORES):
        b = c % 4
        m = dict(shared)
        m["x"] = x[b]
        m["pos"] = pos[b]
        in_maps.append(m)
    res = run_bass_kernel_spmd(nc, in_maps, core_ids=list(range(NCORES)))
    return np.stack([np.asarray(res.results[b]["y"], dtype=np.float32) for b in range(4)], axis=0)
```
